# Optimizing a Trainium2 kernel written in Bass

```python
import math
import jax, jax.numpy as jnp
from jax import lax
import numpy as np

D_MODEL = 1024
BATCH = 16
SEQ = 4096
DEPTH = 4

HEAD_DIM = 64
N_BRANCHES = 4
BRANCH_WIDTH = 256

RWKV_HEADS = 4
RWKV_WIDTH = RWKV_HEADS * HEAD_DIM
RWKV_DECAY_RANK = 64
RWKV_ICL_RANK = 64
RWKV_GATE_RANK = 128
RWKV_DECAY_SCALE = 0.606531
RWKV_LN_EPS = 64e-5
RWKV_COLS = 3 * RWKV_WIDTH + RWKV_DECAY_RANK + RWKV_ICL_RANK + RWKV_GATE_RANK

DIL_PATTERNS = ((128, 1), (512, 4), (2048, 16))
DIL_GROUPS = 3
DIL_HEADS_PER_GROUP = 4
DIL_HEADS = DIL_GROUPS * DIL_HEADS_PER_GROUP
DIL_QKV = DIL_HEADS * HEAD_DIM
DIL_COLS = 3 * DIL_QKV

DIFF_HEADS = 4
DIFF_QK_DIM = 32
DIFF_V_DIM = 2 * DIFF_QK_DIM
DIFF_Q_COLS = DIFF_HEADS * 2 * DIFF_QK_DIM
DIFF_V_COLS = DIFF_HEADS * DIFF_V_DIM
DIFF_COLS = 2 * DIFF_Q_COLS + DIFF_V_COLS
DIFF_QBLOCK = 128
DIFF_SUBLN_EPS = 1e-5

CONV_CHANNELS = 256
CONV_WIDTH = 31
CONV_COLS = 2 * CONV_CHANNELS
CONV_LN_EPS = 1e-5

GATE_COLS = N_BRANCHES * D_MODEL
IN_COLS = RWKV_COLS + DIL_COLS + DIFF_COLS + CONV_COLS + GATE_COLS

D_FF = ((8 * D_MODEL + 3 * 256 - 1) // (3 * 256)) * 256

NORM_EPS = 1e-6
NEG_INF = -1e30

kernel_name = 'hybrid_rwkv7_dilated_diffattn_conformer_block'


def _split(t, sizes):
    out, start = [], 0
    for n in sizes:
        out.append(t[..., start:start + n])
        start += n
    return out


def rms_norm(x, gain, eps=NORM_EPS):
    xf = x.astype(jnp.float32)
    y = xf * lax.rsqrt(jnp.mean(xf * xf, axis=-1, keepdims=True) + eps)
    return (y * gain.astype(jnp.float32)).astype(x.dtype)


def layer_norm_f32(x, gain, bias, eps):
    xf = x.astype(jnp.float32)
    mu = jnp.mean(xf, axis=-1, keepdims=True)
    var = jnp.mean(jnp.square(xf - mu), axis=-1, keepdims=True)
    return (xf - mu) * lax.rsqrt(var + eps) * gain.astype(jnp.float32) + bias.astype(jnp.float32)


def alibi_slopes(n):
    return jnp.asarray(2.0 ** (-8.0 * np.arange(1, n + 1) / n), dtype=jnp.float32)


def rwkv7_time_mix(p, mu, w0, w_up, a0, a_up, g_up, k_k, k_a, r_k, ln_g, ln_b):
    B, S, _ = p.shape
    H, N = RWKV_HEADS, HEAD_DIM
    f32 = jnp.float32
    p_prev = jnp.pad(p[:, :-1], ((0, 0), (1, 0), (0, 0)))
    p = p + (p_prev - p) * mu
    r, k, v, zw, za, zg = _split(p, (RWKV_WIDTH, RWKV_WIDTH, RWKV_WIDTH, RWKV_DECAY_RANK, RWKV_ICL_RANK, RWKV_GATE_RANK))
    log_w = -RWKV_DECAY_SCALE * jax.nn.sigmoid((w0 + jnp.tanh(zw) @ w_up).astype(f32))
    a = jax.nn.sigmoid((a0 + za @ a_up).astype(f32))
    g = (jax.nn.sigmoid(zg) @ g_up).astype(f32)
    heads = lambda t: t.astype(f32).reshape(B, S, H, N)
    r, k, v, log_w, a = heads(r), heads(k), heads(v), heads(log_w), heads(a)
    kk = k * k_k.astype(f32).reshape(H, N)
    kk = kk / jnp.maximum(jnp.sqrt(jnp.sum(kk * kk, axis=-1, keepdims=True)), 1e-12)
    k = k * (1.0 + (a - 1.0) * k_a.astype(f32).reshape(H, N))
    w = jnp.exp(log_w)

    def step(state, inp):
        r_t, w_t, k_t, v_t, kk_t, a_t = inp
        sa = jnp.einsum('bhvk,bhk->bhv', state, -kk_t)
        state = (state * w_t[:, :, None, :]
                 + sa[..., None] * (kk_t * a_t)[:, :, None, :]
                 + v_t[..., None] * k_t[:, :, None, :])
        return state, jnp.einsum('bhvk,bhk->bhv', state, r_t)

    xs = tuple(t.transpose(1, 0, 2, 3) for t in (r, w, k, v, kk, a))
    _, o = lax.scan(step, jnp.zeros((B, H, N, N), f32), xs)
    o = o.transpose(1, 0, 2, 3)
    o = layer_norm_f32(o, ln_g.reshape(H, N), ln_b.reshape(H, N), RWKV_LN_EPS)
    o = o + jnp.sum(r * k * r_k.astype(f32), axis=-1, keepdims=True) * v
    return (o.reshape(B, S, H * N) * g).astype(p.dtype)


def _dilated_group(q, k, v, window, dilation, slopes):
    B, S, H, Dh = q.shape
    n = window // dilation
    L = S // dilation
    nb = -(-L // n)
    Lp = nb * n

    def to_sub(t):
        t = t.reshape(B, L, dilation, H, Dh).transpose(0, 2, 1, 3, 4)
        return jnp.pad(t, ((0, 0), (0, 0), (0, Lp - L), (0, 0), (0, 0)))

    def band(t):
        t = jnp.pad(t, ((0, 0), (0, 0), (n, 0), (0, 0), (0, 0))).reshape(B, dilation, nb + 1, n, H, Dh)
        return jnp.concatenate([t[:, :, :-1], t[:, :, 1:]], axis=3)

    qb = to_sub(q).reshape(B, dilation, nb, n, H, Dh)
    kb, vb = band(to_sub(k)), band(to_sub(v))
    s = jnp.einsum('brcqhd,brckhd->brchqk', qb, kb, preferred_element_type=jnp.float32) * (Dh ** -0.5)
    kj = jnp.arange(2 * n)
    rel = n + jnp.arange(n)[:, None] - kj[None, :]
    front_pad = (jnp.arange(nb)[:, None, None] == 0) & (kj[None, None, :] < n)
    valid = ((rel >= 0) & (rel <= n))[None] & ~front_pad
    bias = -slopes[:, None, None] * (dilation * rel).astype(jnp.float32)[None]
    s = jnp.where(valid[None, None, :, None], s + bias[None, None, None], NEG_INF)
    lse = jax.nn.logsumexp(s, axis=-1)
    pr = jnp.exp(s - lse[..., None])
    o = jnp.einsum('brchqk,brckhd->brcqhd', pr, vb.astype(jnp.float32))
    o = o.reshape(B, dilation, Lp, H, Dh)[:, :, :L].transpose(0, 2, 1, 3, 4).reshape(B, S, H, Dh)
    lse = lse.transpose(0, 1, 2, 4, 3).reshape(B, dilation, Lp, H)[:, :, :L].transpose(0, 2, 1, 3).reshape(B, S, H)
    return o, lse


def dilated_attention(q, k, v, slopes):
    outs, lses = [], []
    for gi, (window, dilation) in enumerate(DIL_PATTERNS):
        o, lse = _dilated_group(q[:, :, gi], k[:, :, gi], v[:, :, gi], window, dilation, slopes[gi])
        outs.append(o)
        lses.append(lse)
    wts = jax.nn.softmax(jnp.stack(lses, axis=0), axis=0)
    return jnp.sum(wts[..., None] * jnp.stack(outs, axis=0), axis=0)


def diff_attention(q, k, v, lam, slopes, subln_g, lambda_init):
    B, S, H, _, dq = q.shape
    nq = S // DIFF_QBLOCK
    qb = q.reshape(B, nq, DIFF_QBLOCK, H, 2, dq).transpose(1, 0, 2, 3, 4, 5)
    kpos = jnp.arange(S)
    vf = v.astype(jnp.float32)

    def block(args):
        c, q_blk = args
        s = jnp.einsum('bqhmd,bkhmd->bhmqk', q_blk, k, preferred_element_type=jnp.float32) * (dq ** -0.5)
        rel = (c * DIFF_QBLOCK + jnp.arange(DIFF_QBLOCK))[:, None] - kpos[None, :]
        s = s - slopes[:, None, None, None] * rel.astype(jnp.float32)
        s = jnp.where(rel >= 0, s, NEG_INF)
        pr = jax.nn.softmax(s, axis=-1)
        att = pr[:, :, 0] - lam * pr[:, :, 1]
        return jnp.einsum('bhqk,bkhd->bqhd', att, vf)

    o = lax.map(block, (jnp.arange(nq), qb))
    o = o.transpose(1, 0, 2, 3, 4).reshape(B, S, H, -1)
    o = o * lax.rsqrt(jnp.mean(o * o, axis=-1, keepdims=True) + DIFF_SUBLN_EPS) * subln_g.astype(jnp.float32)
    return (o * (1.0 - lambda_init)).reshape(B, S, -1).astype(q.dtype)


def conformer_conv(p, dw_w, dw_b, ln_g, ln_b):
    a, b = _split(p, (CONV_CHANNELS, CONV_CHANNELS))
    u = a * jax.nn.sigmoid(b)
    y = lax.conv_general_dilated(u, dw_w, window_strides=(1,), padding=((CONV_WIDTH - 1, 0),),
                                 dimension_numbers=('NWC', 'WIO', 'NWC'),
                                 feature_group_count=CONV_CHANNELS) + dw_b
    y = layer_norm_f32(y, ln_g, ln_b, CONV_LN_EPS)
    return jax.nn.silu(y).astype(p.dtype)


def setup_inputs(seed: int = 0) -> dict:
    key = jax.random.key(seed)
    ks = iter(jax.random.split(key, 40))
    L, D = DEPTH, D_MODEL
    nrm = lambda shape, scale: scale * jax.random.normal(next(ks), shape, jnp.float32)
    gain = lambda shape: 1.0 + nrm(shape, 0.02)
    return {
        'x': nrm((BATCH, SEQ, D), 1.0),
        'norm_mix_pre': gain((L, D)),
        'norm_mix_post': gain((L, D)),
        'norm_ffn_pre': gain((L, D)),
        'norm_ffn_post': gain((L, D)),
        'w_in': nrm((L, D, IN_COLS), D ** -0.5),
        'gate_bias': nrm((L, GATE_COLS), 0.01),
        'rwkv_mu': jax.random.uniform(next(ks), (L, RWKV_COLS), jnp.float32),
        'rwkv_w0': nrm((L, RWKV_WIDTH), 1.0),
        'rwkv_w_up': nrm((L, RWKV_DECAY_RANK, RWKV_WIDTH), 0.5 * RWKV_DECAY_RANK ** -0.5),
        'rwkv_a0': nrm((L, RWKV_WIDTH), 0.1),
        'rwkv_a_up': nrm((L, RWKV_ICL_RANK, RWKV_WIDTH), 0.5 * RWKV_ICL_RANK ** -0.5),
        'rwkv_g_up': nrm((L, RWKV_GATE_RANK, RWKV_WIDTH), RWKV_GATE_RANK ** -0.5),
        'rwkv_k_k': 0.85 + nrm((L, RWKV_WIDTH), 0.02),
        'rwkv_k_a': gain((L, RWKV_WIDTH)),
        'rwkv_r_k': nrm((L, RWKV_HEADS, HEAD_DIM), 0.1),
        'rwkv_ln_g': gain((L, RWKV_WIDTH)),
        'rwkv_ln_b': nrm((L, RWKV_WIDTH), 0.01),
        'diff_lam_q1': nrm((L, DIFF_QK_DIM), 0.1),
        'diff_lam_k1': nrm((L, DIFF_QK_DIM), 0.1),
        'diff_lam_q2': nrm((L, DIFF_QK_DIM), 0.1),
        'diff_lam_k2': nrm((L, DIFF_QK_DIM), 0.1),
        'diff_subln_g': gain((L, DIFF_V_DIM)),
        'conv_dw_w': nrm((L, CONV_WIDTH, 1, CONV_CHANNELS), CONV_WIDTH ** -0.5),
        'conv_dw_b': nrm((L, CONV_CHANNELS), 0.01),
        'conv_ln_g': gain((L, CONV_CHANNELS)),
        'conv_ln_b': nrm((L, CONV_CHANNELS), 0.01),
        'w_branch': nrm((L, N_BRANCHES, BRANCH_WIDTH, D), BRANCH_WIDTH ** -0.5),
        'w_out': nrm((L, D, D), D ** -0.5),
        'ffn_w_gate': nrm((L, D, D_FF), D ** -0.5),
        'ffn_w_up': nrm((L, D, D_FF), D ** -0.5),
        'ffn_w_down': nrm((L, D_FF, D), D_FF ** -0.5),
    }


def reference(x, norm_mix_pre, norm_mix_post, norm_ffn_pre, norm_ffn_post, w_in, gate_bias,
              rwkv_mu, rwkv_w0, rwkv_w_up, rwkv_a0, rwkv_a_up, rwkv_g_up, rwkv_k_k, rwkv_k_a,
              rwkv_r_k, rwkv_ln_g, rwkv_ln_b, diff_lam_q1, diff_lam_k1, diff_lam_q2, diff_lam_k2,
              diff_subln_g, conv_dw_w, conv_dw_b, conv_ln_g, conv_ln_b, w_branch, w_out,
              ffn_w_gate, ffn_w_up, ffn_w_down):
    B, S, D = x.shape
    dil_slopes = alibi_slopes(DIL_HEADS).reshape(DIL_GROUPS, DIL_HEADS_PER_GROUP)
    diff_slopes = alibi_slopes(DIFF_HEADS)
    for l in range(DEPTH):
        h = rms_norm(x, norm_mix_pre[l])
        proj = h @ w_in[l]
        p_rwkv, p_dil, p_diff, p_conv, p_gate = _split(proj, (RWKV_COLS, DIL_COLS, DIFF_COLS, CONV_COLS, GATE_COLS))

        y_a = rwkv7_time_mix(p_rwkv, rwkv_mu[l], rwkv_w0[l], rwkv_w_up[l], rwkv_a0[l], rwkv_a_up[l],
                             rwkv_g_up[l], rwkv_k_k[l], rwkv_k_a[l], rwkv_r_k[l], rwkv_ln_g[l], rwkv_ln_b[l])

        dq, dk, dv = _split(p_dil, (DIL_QKV, DIL_QKV, DIL_QKV))
        grp = lambda t: t.reshape(B, S, DIL_GROUPS, DIL_HEADS_PER_GROUP, HEAD_DIM)
        y_b = dilated_attention(grp(dq), grp(dk), grp(dv), dil_slopes).reshape(B, S, BRANCH_WIDTH).astype(x.dtype)

        cq, ck, cv = _split(p_diff, (DIFF_Q_COLS, DIFF_Q_COLS, DIFF_V_COLS))
        lambda_init = 0.8 - 0.6 * math.exp(-0.3 * l)
        lam = (jnp.exp(jnp.sum(diff_lam_q1[l].astype(jnp.float32) * diff_lam_k1[l].astype(jnp.float32)))
               - jnp.exp(jnp.sum(diff_lam_q2[l].astype(jnp.float32) * diff_lam_k2[l].astype(jnp.float32)))
               + lambda_init)
        y_c = diff_attention(cq.reshape(B, S, DIFF_HEADS, 2, DIFF_QK_DIM),
                             ck.reshape(B, S, DIFF_HEADS, 2, DIFF_QK_DIM),
                             cv.reshape(B, S, DIFF_HEADS, DIFF_V_DIM),
                             lam, diff_slopes, diff_subln_g[l], lambda_init)

        y_d = conformer_conv(p_conv, conv_dw_w[l], conv_dw_b[l], conv_ln_g[l], conv_ln_b[l])

        ys = jnp.stack([y_a, y_b, y_c, y_d], axis=2)
        gates = jax.nn.sigmoid(p_gate + gate_bias[l]).reshape(B, S, N_BRANCHES, D)
        merged = jnp.einsum('bsnd,bsnd->bsd', gates, jnp.einsum('bsnc,ncd->bsnd', ys, w_branch[l]))
        x = x + rms_norm(merged @ w_out[l], norm_mix_post[l])

        h = rms_norm(x, norm_ffn_pre[l])
        f = (jax.nn.silu(h @ ffn_w_gate[l]) * (h @ ffn_w_up[l])) @ ffn_w_down[l]
        x = x + rms_norm(f, norm_ffn_post[l])
    return x
```

```python
import numpy as np
import concourse.bass as bass
import concourse.mybir as mybir
from concourse.bass_utils import run_bass_kernel_spmd

F32 = mybir.dt.float32
BF16 = mybir.dt.bfloat16
AF = mybir.ActivationFunctionType
ALU = mybir.AluOpType
AX = mybir.AxisListType

D = 1024
S = 4096
DEPTH = 4
KC = 8
IN_COLS = 8704
D_FF = 2816
FC = 22
RW0, DIL0, DIF0, CONV0, GATE0 = 0, 1024, 3328, 4096, 4608
SEM_LIMIT = 30000
NCM = 768


class T:
    def __init__(self, h, const=False):
        self.h = h
        self.w = None
        self.rd = {}
        self.const = const

    def __getitem__(self, idx):
        return self.h[idx]


class Eng:
    def __init__(self, kb, name, e, is_pe=False):
        self.kb, self.name, self.e, self.is_pe = kb, name, e, is_pe
        self.gen = 0
        self.sem = kb.nc.alloc_semaphore(f"s_{name}_0")
        self.cnt = 0
        self.seen = {}
        self.last = None
        self.dsems = []
        self.di = 0

    def signal(self, ins):
        if self.cnt >= SEM_LIMIT:
            self.gen += 1
            self.sem = self.kb.nc.alloc_semaphore(f"s_{self.name}_{self.gen}")
            self.cnt = 0
        self.cnt += 1
        ins.then_inc(self.sem, 1)
        ev = (self.sem, self.cnt, self)
        self.last = ev
        return ev


class KB:
    def __init__(self, nc, ndma_sems=12):
        self.nc = nc
        self.eng = {
            'pe': Eng(self, 'pe', nc.tensor, True),
            'act': Eng(self, 'act', nc.scalar),
            'dve': Eng(self, 'dve', nc.vector),
            'pool': Eng(self, 'pool', nc.gpsimd),
            'sp': Eng(self, 'sp', nc.sync),
        }
        self.ndma = ndma_sems
        self.dma_latest = {}
        self.n_ins = 0
        self.uid = 0

    def sb(self, name, shape, dt, const=False):
        self.uid += 1
        return T(self.nc.alloc_sbuf_tensor(f"{name}_{self.uid}", list(shape), dt), const)

    def ps(self, name, shape=(128, 512), dt=F32):
        self.uid += 1
        return T(self.nc.alloc_psum_tensor(f"{name}_{self.uid}", list(shape), dt))

    def _wait(self, E, deps):
        best = {}
        for d in deps:
            if d is None:
                continue
            sem, val, src = d
            if src is E and E.is_pe:
                continue
            k = id(sem)
            if E.seen.get(k, 0) >= val:
                continue
            if k not in best or best[k][1] < val:
                best[k] = (sem, val)
        for k, (sem, val) in best.items():
            E.e.wait_ge(sem, val)
            E.seen[k] = val
            self.n_ins += 1

    def _deps(self, r, w):
        deps = []
        for t in r:
            if t.w is not None:
                deps.append(t.w)
        for t in w:
            if t.w is not None:
                deps.append(t.w)
            deps.extend(t.rd.values())
        return deps

    def _mark(self, ev, r, w):
        for t in w:
            t.w = ev
            t.rd = {}
        for t in r:
            if t.const or t in w:
                continue
            t.rd[id(ev[0])] = ev

    def op(self, en, fn, r=(), w=(), extra=()):
        E = self.eng[en]
        self._wait(E, self._deps(r, w) + list(extra))
        ins = fn(E.e)
        ev = E.signal(ins)
        self._mark(ev, r, w)
        self.n_ins += 1
        return ev

    def mm(self, out_t, out_ap, pairs, r=(), extra=()):
        E = self.eng['pe']
        self._wait(E, self._deps(r, (out_t,)) + list(extra))
        n = len(pairs)
        ins = None
        for i, (l, rh) in enumerate(pairs):
            ins = E.e.matmul(out_ap, l, rh, start=(i == 0), stop=(i == n - 1))
            self.n_ins += 1
        ev = E.signal(ins)
        self._mark(ev, r, (out_t,))
        return ev

    def mm1(self, out_t, out_ap, l, rh, start, stop, r=(), sig=True):
        E = self.eng['pe']
        self._wait(E, self._deps(r, (out_t,)))
        ins = E.e.matmul(out_ap, l, rh, start=start, stop=stop)
        self.n_ins += 1
        if not sig:
            return None
        ev = E.signal(ins)
        self._mark(ev, r, (out_t,))
        return ev

    def dma(self, qn, out, in_, r=(), w=(), extra=(), **kw):
        E = self.eng[qn]
        if len(E.dsems) < self.ndma:
            E.dsems.append([self.nc.alloc_semaphore(f"d_{qn}_{len(E.dsems)}_0"), 0, 0])
        slot = E.dsems[E.di % self.ndma]
        E.di += 1
        if slot[1] * 16 >= SEM_LIMIT:
            slot[2] += 1
            slot[0] = self.nc.alloc_semaphore(f"d_{qn}_{E.di % self.ndma}_{slot[2]}")
            slot[1] = 0
        prev = (slot[0], slot[1] * 16, None) if slot[1] > 0 else None
        self._wait(E, self._deps(r, w) + list(extra) + [prev])
        ins = E.e.dma_start(out=out, in_=in_, **kw)
        slot[1] += 1
        ins.then_inc(slot[0], 16)
        ev = (slot[0], slot[1] * 16, None)
        self.dma_latest[id(slot[0])] = ev
        self._mark(ev, r, w)
        self.n_ins += 1
        return ev

    def barrier(self):
        evs = [E.last for E in self.eng.values() if E.last is not None]
        evs += list(self.dma_latest.values())
        for E in self.eng.values():
            self._wait(E, [e for e in evs if not (e[2] is E and E.is_pe)])
        self.dma_latest = {}


def colblock(v):
    v = np.asarray(v, np.float32).reshape(-1)
    n = v.size // 128
    return np.ascontiguousarray(v.reshape(n, 128).T)


class ColTable:
    def __init__(self):
        self.parts, self.idx, self.n = [], {}, 0

    def add(self, name, v):
        b = colblock(v)
        self.idx[name] = self.n
        self.parts.append(b)
        self.n += b.shape[1]

    def build(self):
        return np.ascontiguousarray(np.concatenate(self.parts, axis=1))


def col_layout(L):
    idx, n = {}, 0
    for l in range(L):
        for nm, w in (('nmp', 8), ('nmo', 8), ('nfp', 8), ('nfo', 8), ('gb', 32), ('mu', 8), ('cw', 62), ('cb', 2), ('cg', 2), ('cbb', 2), ('slg', 1), ('w0', 2), ('a0', 2), ('kk', 2), ('ka', 2), ('rk', 2), ('lng', 2), ('lnb', 2), ('lmi', 1), ('oml', 1)):
            idx[(nm, l)] = n
            n += w
    return idx, n


def build_cols(inp, L, l0=0):
    import math
    ct = ColTable()
    for l in range(L):
        ct.add(('nmp', l), inp['norm_mix_pre'][l])
        ct.add(('nmo', l), inp['norm_mix_post'][l])
        ct.add(('nfp', l), inp['norm_ffn_pre'][l])
        ct.add(('nfo', l), inp['norm_ffn_post'][l])
        ct.add(('gb', l), inp['gate_bias'][l])
        ct.add(('mu', l), inp['rwkv_mu'][l])
        ct.add(('cw', l), inp['conv_dw_w'][l].reshape(-1))
        ct.add(('cb', l), inp['conv_dw_b'][l])
        ct.add(('cg', l), inp['conv_ln_g'][l])
        ct.add(('cbb', l), inp['conv_ln_b'][l])
        ct.add(('slg', l), np.concatenate([inp['diff_subln_g'][l], np.zeros(64, np.float32)]))
        ct.add(('w0', l), inp['rwkv_w0'][l])
        ct.add(('a0', l), inp['rwkv_a0'][l])
        ct.add(('kk', l), inp['rwkv_k_k'][l])
        ct.add(('ka', l), inp['rwkv_k_a'][l])
        ct.add(('rk', l), inp['rwkv_r_k'][l].reshape(-1))
        ct.add(('lng', l), inp['rwkv_ln_g'][l])
        ct.add(('lnb', l), inp['rwkv_ln_b'][l])
        li = 0.8 - 0.6 * math.exp(-0.3 * (l0 + l))
        ct.add(('lmi', l), np.full(128, -li, np.float32))
        ct.add(('oml', l), np.full(128, 1.0 - li, np.float32))
    return ct.build()


class Prog:
    def __init__(self, L=DEPTH, NS=2, debug=()):
        self.L, self.NS, self.NT = L, NS, NS * S
        self.debug = set(debug)
        nc = bass.Bass("TRN2", target_bir_lowering=False)
        self.nc = nc
        self.kb = KB(nc)
        NT = self.NT
        ein = lambda n, s, d=F32: nc.dram_tensor(n, list(s), d, kind="ExternalInput").ap()
        self.x = ein("x", [NT, D])
        self.w_in = ein("w_in", [L, D, IN_COLS])
        self.w_branch = ein("w_branch", [L, 4, 256, D])
        self.w_out = ein("w_out", [L, D, D])
        self.w_g = ein("ffn_w_gate", [L, D, D_FF])
        self.w_u = ein("ffn_w_up", [L, D, D_FF])
        self.w_d = ein("ffn_w_down", [L, D_FF, D])
        self.cidx, ncols = col_layout(L)
        self.cols_d = ein("cols", [128, ncols])
        self.cm_d = ein("cmats", [128, NCM])
        self.dxq = ein("dxq", [16, S])
        self.dxk = ein("dxk", [16, S])
        self.dlq = ein("dlq", [96, S])
        self.dlk = ein("dlk", [96, S])
        self.lamv = ein("lamv", [L, 128, 128])
        self.wa_up = ein("wa_up", [L, 128, 256])
        self.g_up = ein("g_up", [L, 128, 256])
        self.out = nc.dram_tensor("out", [NT, D], F32, kind="ExternalOutput").ap()
        self.ncols = ncols

    def scratch(self, name, shape, dt):
        kind = "ExternalOutput" if name in self.debug else "Internal"
        return self.nc.dram_tensor(name, list(shape), dt, kind=kind).ap()

    def build(self):
        nc, kb, L, NT = self.nc, self.kb, self.L, self.NT
        self.cols = kb.sb("cols", [128, self.ncols], F32, const=True)
        self.cm = kb.sb("cmats", [128, NCM], F32, const=True)
        self.identb = kb.sb("identb", [128, 128], BF16, const=True)
        kb.dma('sp', self.cols[:, :], self.cols_d[:, :], w=(self.cols,))
        kb.dma('sp', self.cm[:, :], self.cm_d[:, :], w=(self.cm,))
        kb.op('dve', lambda e: e.tensor_copy(self.identb[:, :], self.cm[:, 0:128]), r=(self.cm,), w=(self.identb,))
        self.ident = self.cm
        self.epsc = kb.sb("epsc", [128, 4], F32, const=True)
        kb.op('dve', lambda e: e.memset(self.epsc[:, 0:1], 1e-6), w=(self.epsc,))
        kb.op('dve', lambda e: e.memset(self.epsc[:, 1:2], 1e-5), w=(self.epsc,))
        kb.op('dve', lambda e: e.memset(self.epsc[:, 2:3], 64e-5), w=(self.epsc,))
        kb.op('dve', lambda e: e.memset(self.epsc[:, 3:4], 0.0), w=(self.epsc,))
        self.psb = [kb.ps(f"psb{i}") for i in range(4)]
        self.psi = 0
        self.pacc = [kb.ps(f"pacc{i}") for i in range(3)]
        self.psT = kb.ps("psT", (128, 1024), BF16)
        self.mUL = kb.sb("mUL", [128, 256], BF16, const=True)
        kb.op('dve', lambda e: e.tensor_copy(self.mUL[:, :], self.cm[:, 256:512]), r=(self.cm,), w=(self.mUL,))
        self.xT = self.scratch("xT", [D, NT], F32)
        self.wb_in = self.scratch("wb_in", [L, D, IN_COLS], BF16)
        self.wb_br = self.scratch("wb_br", [L, 1024, D], BF16)
        self.wb_out = self.scratch("wb_out", [L, D, D], BF16)
        self.wb_g = self.scratch("wb_g", [L, D, D_FF], BF16)
        self.wb_u = self.scratch("wb_u", [L, D, D_FF], BF16)
        self.wb_d = self.scratch("wb_d", [L, D_FF, D], BF16)
        self.P_rw = self.scratch("P_rw", [1024, NT], F32)
        self.P_dq = self.scratch("P_dq", [768, NT], BF16)
        self.P_dk = self.scratch("P_dk", [768, NT], BF16)
        self.P_dv = self.scratch("P_dv", [768, NT], BF16)
        self.P_cq = self.scratch("P_cq", [256, NT], BF16)
        self.P_ck = self.scratch("P_ck", [256, NT], BF16)
        self.P_cv = self.scratch("P_cv", [256, NT], BF16)
        self.P_cn = self.scratch("P_cn", [512, NT], F32)
        self.P_g = self.scratch("P_g", [4096, NT], BF16)
        self.Y = self.scratch("Y", [1024, NT], BF16)
        self.ROWS = self.scratch("ROWS", [S, 5, 2, 256], F32)
        self.Vf = self.scratch("Vf", [256, NT], F32)
        self.Gf = self.scratch("Gf", [256, NT], F32)
        self.BNVf = self.scratch("BNVf", [256, NT], F32)
        self.Of = self.scratch("Of", [256, NT], F32)

        sb0 = nc.sbuf_base
        self.stage_cast()
        kb.barrier()
        nc.sbuf_base = sb0
        self.stage_xin()
        kb.barrier()
        for l in range(L):
            nc.sbuf_base = sb0
            self.stage_proj(l)
            kb.barrier()
            nc.sbuf_base = sb0
            self.stage_mixers(l)
            kb.barrier()
            nc.sbuf_base = sb0
            self.stage_merge(l)
            kb.barrier()
            nc.sbuf_base = sb0
            self.stage_ffn(l)
            kb.barrier()
        nc.sbuf_base = sb0
        self.stage_xout()
        kb.barrier()
        return nc

    def next_ps(self):
        p = self.psb[self.psi % 4]
        self.psi += 1
        return p

    def stage_cast(self):
        kb, L = self.kb, self.L
        CW = 2176
        NB = 3
        fin = [kb.sb(f"cast_in{i}", [128, CW], F32) for i in range(NB)]
        fout = [kb.sb(f"cast_out{i}", [128, CW], BF16) for i in range(NB)]
        engs = ['act', 'dve', 'pool']
        jobs = []
        for l in range(L):
            jobs.append((self.wb_in[l], self.w_in[l], D, IN_COLS))
            jobs.append((self.wb_br[l], self.w_branch[l].rearrange("n c d -> (n c) d"), 1024, D))
            jobs.append((self.wb_out[l], self.w_out[l], D, D))
            jobs.append((self.wb_g[l], self.w_g[l], D, D_FF))
            jobs.append((self.wb_u[l], self.w_u[l], D, D_FF))
            jobs.append((self.wb_d[l], self.w_d[l], D_FF, D))
        i = 0
        for dst, src, rows, cols in jobs:
            for r0 in range(0, rows, 128):
                for c0 in range(0, cols, CW):
                    cw = min(CW, cols - c0)
                    a, b = fin[i % NB], fout[i % NB]
                    kb.dma('sp', a[:, 0:cw], src[r0:r0 + 128, c0:c0 + cw], w=(a,))
                    en = engs[i % 3]
                    if en == 'act':
                        kb.op('act', lambda e, a=a, b=b, cw=cw: e.copy(b[:, 0:cw], a[:, 0:cw]), r=(a,), w=(b,))
                    else:
                        kb.op(en, lambda e, a=a, b=b, cw=cw: e.tensor_copy(b[:, 0:cw], a[:, 0:cw]), r=(a,), w=(b,))
                    kb.dma('pool', dst[r0:r0 + 128, c0:c0 + cw], b[:, 0:cw], r=(b,))
                    i += 1

    def stage_xin(self):
        kb, NT = self.kb, self.NT
        NB = 2
        xin = [kb.sb(f"xin{i}", [128, 4, D], F32) for i in range(NB)]
        xo = [kb.sb(f"xo{i}", [128, KC, 512], F32) for i in range(NB)]
        xv = self.x.rearrange("(n j p) d -> n p j d", p=128, j=4)
        xTv = self.xT.rearrange("(kc p) t -> p kc t", p=128)
        for n in range(NT // 512):
            a, b = xin[n % NB], xo[n % NB]
            kb.dma('sp', a[:, :, :], xv[n], w=(a,))
            for kc in range(KC):
                ps = self.next_ps()
                for j in range(4):
                    kb.op('pe', lambda e, ps=ps, a=a, j=j, kc=kc: e.transpose(
                        ps[:, j * 128:(j + 1) * 128], a[:, j, kc * 128:(kc + 1) * 128], self.ident[:, 0:128]),
                        r=(a,), w=(ps,))
                en = 'act' if kc % 2 == 0 else 'dve'
                if en == 'act':
                    kb.op('act', lambda e, ps=ps, b=b, kc=kc: e.copy(b[:, kc, :], ps[:, :]), r=(ps,), w=(b,))
                else:
                    kb.op('dve', lambda e, ps=ps, b=b, kc=kc: e.tensor_copy(b[:, kc, :], ps[:, :]), r=(ps,), w=(b,))
            kb.dma('pool', xTv[:, :, n * 512:(n + 1) * 512], b[:, :, :], r=(b,))

    def stage_xout(self):
        kb, NT = self.kb, self.NT
        NB = 2
        xi = [kb.sb(f"xoi{i}", [128, KC, 512], F32) for i in range(NB)]
        xo = [kb.sb(f"xoo{i}", [128, 4, D], F32) for i in range(NB)]
        ov = self.out.rearrange("(n j p) d -> n p j d", p=128, j=4)
        xTv = self.xT.rearrange("(kc p) t -> p kc t", p=128)
        for n in range(NT // 512):
            a, b = xi[n % NB], xo[n % NB]
            kb.dma('sp', a[:, :, :], xTv[:, :, n * 512:(n + 1) * 512], w=(a,))
            for j in range(4):
                for h in range(2):
                    ps = self.next_ps()
                    for q in range(4):
                        kc = h * 4 + q
                        kb.op('pe', lambda e, ps=ps, a=a, j=j, kc=kc, q=q: e.transpose(
                            ps[:, q * 128:(q + 1) * 128], a[:, kc, j * 128:(j + 1) * 128], self.ident[:, 0:128]),
                            r=(a,), w=(ps,))
                    if h == 0:
                        kb.op('act', lambda e, ps=ps, b=b, j=j: e.copy(b[:, j, 0:512], ps[:, :]), r=(ps,), w=(b,))
                    else:
                        kb.op('dve', lambda e, ps=ps, b=b, j=j: e.tensor_copy(b[:, j, 512:1024], ps[:, :]), r=(ps,), w=(b,))
            kb.dma('pool', ov[n], b[:, :, :], r=(b,))

    def stage_proj(self, l):
        kb, NT = self.kb, self.NT
        TT = 2048
        NSUB = TT // 512
        xt = [kb.sb(f"pj_x{i}", [128, KC, 512], F32) for i in range(2)]
        sq = kb.sb("pj_sq", [128, KC, 512], F32)
        rstd = kb.sb("pj_rstd", [128, 512], F32)
        hT = kb.sb("pj_h", [128, KC, TT], BF16)
        hsub = [T(hT.h) for _ in range(NSUB)]
        NW = 3
        wt = [kb.sb(f"pj_w{i}", [128, KC, 128], BF16) for i in range(NW)]
        NO = 4
        ot32 = [kb.sb(f"pj_o32_{i}", [128, 512], F32) for i in range(NO)]
        ot16 = [kb.sb(f"pj_o16_{i}", [128, 512], BF16) for i in range(NO)]
        xTv = self.xT.rearrange("(kc p) t -> p kc t", p=128)
        wv = self.wb_in[l].rearrange("(kc p) n -> p kc n", p=128)
        gb0 = self.cidx[('gb', l)]
        oi = 0
        wi = 0
        for st in range(NT // TT):
            t0 = st * TT
            for s in range(NSUB):
                a = xt[s % 2]
                kb.dma('sp', a[:, :, :], xTv[:, :, t0 + s * 512:t0 + (s + 1) * 512], w=(a,))
                hs = hsub[s]
                self._norm_into(a, hT, s * 512, hs, self.cidx[('nmp', l)], sq, rstd)
            for oc in range(IN_COLS // 128):
                w = wt[wi % NW]
                wi += 1
                kb.dma('sp', w[:, :, :], wv[:, :, oc * 128:(oc + 1) * 128], w=(w,))
                c0 = oc * 128
                for s in range(NSUB):
                    ps = self.next_ps()
                    kb.mm(ps, ps[:, :], [(w[:, kc, :], hT[:, kc, s * 512:(s + 1) * 512]) for kc in range(KC)],
                          r=(w, hsub[s]))
                    tsl = slice(t0 + s * 512, t0 + (s + 1) * 512)
                    en = 'act' if oi % 2 == 0 else 'dve'
                    if c0 < DIL0:
                        o = ot32[oi % NO]
                        self._evac(en, o, ps, None)
                        dst = self.P_rw[c0:c0 + 128, tsl]
                    elif c0 < DIF0:
                        o = ot16[oi % NO]
                        cc = c0 - DIL0
                        if cc < 768:
                            self._evac(en, o, ps, 0.125)
                            dst = self.P_dq[cc:cc + 128, tsl]
                        elif cc < 1536:
                            self._evac(en, o, ps, None)
                            dst = self.P_dk[cc - 768:cc - 640, tsl]
                        else:
                            self._evac(en, o, ps, None)
                            dst = self.P_dv[cc - 1536:cc - 1408, tsl]
                    elif c0 < CONV0:
                        o = ot16[oi % NO]
                        cc = c0 - DIF0
                        if cc < 256:
                            self._evac(en, o, ps, 32.0 ** -0.5)
                            dst = self.P_cq[cc:cc + 128, tsl]
                        elif cc < 512:
                            self._evac(en, o, ps, None)
                            dst = self.P_ck[cc - 256:cc - 128, tsl]
                        else:
                            self._evac(en, o, ps, None)
                            dst = self.P_cv[cc - 512:cc - 384, tsl]
                    elif c0 < GATE0:
                        o = ot32[oi % NO]
                        self._evac(en, o, ps, None)
                        dst = self.P_cn[c0 - CONV0:c0 - CONV0 + 128, tsl]
                    else:
                        o = ot16[oi % NO]
                        gc = (c0 - GATE0) // 128
                        kb.op('act', lambda e, o=o, ps=ps, gc=gc: e.activation(
                            out=o[:, :], in_=ps[:, :], func=AF.Sigmoid, bias=self.cols[:, gb0 + gc:gb0 + gc + 1]),
                            r=(ps,), w=(o,))
                        dst = self.P_g[c0 - GATE0:c0 - GATE0 + 128, tsl]
                    kb.dma('pool', dst, o[:, :], r=(o,))
                    oi += 1

    def _evac(self, en, o, ps, scale):
        kb = self.kb
        if en == 'act':
            if scale is None:
                kb.op('act', lambda e: e.copy(o[:, :], ps[:, :]), r=(ps,), w=(o,))
            else:
                kb.op('act', lambda e: e.mul(o[:, :], ps[:, :], scale), r=(ps,), w=(o,))
        else:
            if scale is None:
                kb.op('dve', lambda e: e.tensor_copy(o[:, :], ps[:, :]), r=(ps,), w=(o,))
            else:
                kb.op('dve', lambda e: e.tensor_scalar(o[:, :], ps[:, :], scale, None, ALU.mult), r=(ps,), w=(o,))

    def _norm_into(self, xt, hT, off, htrk, gcol0, sq, rstd, n=512):
        kb = self.kb
        kb.op('act', lambda e: e.activation(out=sq[:, :, 0:n], in_=xt[:, :, 0:n], func=AF.Square), r=(xt,), w=(sq,))
        ps = self.next_ps()
        kb.mm(ps, ps[:, 0:n], [(self.cm[:, 128:256], sq[:, kc, 0:n]) for kc in range(KC)], r=(sq, self.cm))
        kb.op('act', lambda e: e.activation(out=rstd[:, 0:n], in_=ps[:, 0:n], func=AF.Sqrt, bias=self.epsc[:, 0:1], scale=1.0 / D),
              r=(ps,), w=(rstd,))
        kb.op('dve', lambda e: e.reciprocal(rstd[:, 0:n], rstd[:, 0:n]), r=(rstd,), w=(rstd,))
        for kc in range(KC):
            kb.op('dve', lambda e, kc=kc: e.scalar_tensor_tensor(
                hT[:, kc, off:off + n], xt[:, kc, 0:n], self.cols[:, gcol0 + kc:gcol0 + kc + 1], rstd[:, 0:n],
                ALU.mult, ALU.mult), r=(xt, rstd), w=(htrk,))

    def stage_mixers(self, l):
        nc = self.nc
        sb0 = nc.sbuf_base
        for fn in (self.mixer_conv, self.mixer_diff, self.mixer_dil, self.mixer_rwkv):
            nc.sbuf_base = sb0
            fn(l)
            self.kb.barrier()

    def mixer_rwkv(self, l):
        nc, kb = self.nc, self.kb
        sb0 = nc.sbuf_base
        self.rwkv_prelude(l)
        kb.barrier()
        nc.sbuf_base = sb0
        self.rwkv_scan(l)
        kb.barrier()
        nc.sbuf_base = sb0
        self.rwkv_post(l)

    def rwkv_prelude(self, l):
        kb = self.kb
        ci = lambda k: self.cidx[(k, l)]
        col = lambda i: self.cols[:, i:i + 1]
        blk = self.cm[:, 640:768]
        wa = kb.sb("rw_wa", [128, 256], F32)
        gup = kb.sb("rw_gup", [128, 256], F32)
        kb.dma('sp', wa[:, :], self.wa_up[l], w=(wa,))
        kb.dma('sp', gup[:, :], self.g_up[l], w=(gup,))
        pin = [kb.sb(f"rw_pin{i}", [128, 8, 513], F32) for i in range(2)]
        pm = kb.sb("rw_pm", [128, 8, 512], F32)
        tz = kb.sb("rw_tz", [128, 512], F32)
        sg = kb.sb("rw_sg", [128, 512], F32)
        Q = [kb.sb(f"rw_q{i}", [128, 2, 512], F32) for i in range(5)]
        Wq, NKK, Bq, K2, Rq = Q
        aq = kb.sb("rw_a", [128, 2, 512], F32)
        gq = kb.sb("rw_g", [128, 2, 512], F32)
        kkq = kb.sb("rw_kk", [128, 2, 512], F32)
        sq = kb.sb("rw_sq", [128, 2, 512], F32)
        rn = kb.sb("rw_rn", [128, 2, 512], F32)
        tq = kb.sb("rw_t", [128, 2, 512], F32)
        bnv = kb.sb("rw_bnv", [128, 2, 512], F32)
        vq = kb.sb("rw_v", [128, 2, 512], F32)
        rowb = [kb.sb(f"rw_rowb{i}", [128, 5, 256], F32) for i in range(2)]
        Pv = self.P_rw.rearrange("(c p) t -> p c t", p=128)
        fview = lambda dr: dr.rearrange("(c p) t -> p c t", p=128)
        ri = 0
        for si in range(self.NS):
            tb = si * S
            for n in range(S // 512):
                t0 = tb + n * 512
                p_ = pin[n % 2]
                if n == 0:
                    kb.op('dve', lambda e, p_=p_: e.memset(p_[:, :, 0:1], 0.0), w=(p_,))
                    kb.dma('sp', p_[:, :, 1:513], Pv[:, :, t0:t0 + 512], w=(p_,))
                else:
                    kb.dma('sp', p_[:, :, 0:513], Pv[:, :, t0 - 1:t0 + 512], w=(p_,))
                kb.op('pool', lambda e, p_=p_: e.tensor_tensor(pm[:, :, :], p_[:, :, 0:512], p_[:, :, 1:513], ALU.subtract),
                      r=(p_,), w=(pm,))
                for c in range(8):
                    kb.op('dve', lambda e, p_=p_, c=c: e.scalar_tensor_tensor(
                        pm[:, c, :], pm[:, c, :], col(ci('mu') + c), p_[:, c, 1:513], ALU.mult, ALU.add), r=(pm, p_), w=(pm,))
                kb.op('act', lambda e: e.copy(Rq[:, :, :], pm[:, 0:2, :]), r=(pm,), w=(Rq,))
                kb.op('act', lambda e: e.copy(vq[:, :, :], pm[:, 4:6, :]), r=(pm,), w=(vq,))
                kb.op('act', lambda e: e.activation(out=tz[0:64, :], in_=pm[0:64, 6, :], func=AF.Tanh), r=(pm,), w=(tz,))
                for c in range(2):
                    ps = self.next_ps()
                    kb.mm(ps, ps[:, :], [(wa[0:64, c * 128:(c + 1) * 128], tz[0:64, :])], r=(wa, tz))
                    kb.op('act', lambda e, ps=ps, c=c: e.activation(out=Wq[:, c, :], in_=ps[:, :], func=AF.Sigmoid, bias=col(ci('w0') + c)),
                          r=(ps,), w=(Wq,))
                kb.op('act', lambda e: e.activation(out=Wq[:, :, :], in_=Wq[:, :, :], func=AF.Exp, scale=-0.606531), r=(Wq,), w=(Wq,))
                for c in range(2):
                    ps = self.next_ps()
                    kb.mm(ps, ps[:, :], [(wa[64:128, c * 128:(c + 1) * 128], pm[64:128, 6, :])], r=(wa, pm))
                    kb.op('act', lambda e, ps=ps, c=c: e.activation(out=aq[:, c, :], in_=ps[:, :], func=AF.Sigmoid, bias=col(ci('a0') + c)),
                          r=(ps,), w=(aq,))
                kb.op('act', lambda e: e.activation(out=sg[:, :], in_=pm[:, 7, :], func=AF.Sigmoid), r=(pm,), w=(sg,))
                for c in range(2):
                    ps = self.next_ps()
                    kb.mm(ps, ps[:, :], [(gup[:, c * 128:(c + 1) * 128], sg[:, :])], r=(gup, sg))
                    kb.op('act', lambda e, ps=ps, c=c: e.copy(gq[:, c, :], ps[:, :]), r=(ps,), w=(gq,))
                for c in range(2):
                    kb.op('dve', lambda e, c=c: e.tensor_scalar(kkq[:, c, :], pm[:, 2 + c, :], col(ci('kk') + c), None, ALU.mult),
                          r=(pm,), w=(kkq,))
                kb.op('act', lambda e: e.activation(out=sq[:, :, :], in_=kkq[:, :, :], func=AF.Square), r=(kkq,), w=(sq,))
                for c in range(2):
                    ps = self.next_ps()
                    kb.mm(ps, ps[:, :], [(blk, sq[:, c, :])], r=(sq,))
                    kb.op('act', lambda e, ps=ps, c=c: e.activation(out=rn[:, c, :], in_=ps[:, :], func=AF.Sqrt), r=(ps,), w=(rn,))
                kb.op('dve', lambda e: e.tensor_scalar(rn[:, :, :], rn[:, :, :], 1e-12, None, ALU.max), r=(rn,), w=(rn,))
                kb.op('dve', lambda e: e.reciprocal(rn[:, :, :], rn[:, :, :]), r=(rn,), w=(rn,))
                kb.op('dve', lambda e: e.tensor_tensor(kkq[:, :, :], kkq[:, :, :], rn[:, :, :], ALU.mult), r=(kkq, rn), w=(kkq,))
                kb.op('act', lambda e: e.mul(NKK[:, :, :], kkq[:, :, :], -1.0), r=(kkq,), w=(NKK,))
                kb.op('dve', lambda e: e.tensor_tensor(Bq[:, :, :], kkq[:, :, :], aq[:, :, :], ALU.mult), r=(kkq, aq), w=(Bq,))
                for c in range(2):
                    kb.op('dve', lambda e, c=c: e.tensor_scalar(tq[:, c, :], aq[:, c, :], -1.0, col(ci('ka') + c), ALU.add, ALU.mult),
                          r=(aq,), w=(tq,))
                kb.op('dve', lambda e: e.scalar_tensor_tensor(K2[:, :, :], tq[:, :, :], 1.0, pm[:, 2:4, :], ALU.add, ALU.mult),
                      r=(tq, pm), w=(K2,))
                for c in range(2):
                    kb.op('dve', lambda e, c=c: e.scalar_tensor_tensor(tq[:, c, :], Rq[:, c, :], col(ci('rk') + c), K2[:, c, :],
                                                                       ALU.mult, ALU.mult), r=(Rq, K2, tq), w=(tq,))
                for c in range(2):
                    ps = self.next_ps()
                    kb.mm(ps, ps[:, :], [(blk, tq[:, c, :])], r=(tq,))
                    kb.op('dve', lambda e, ps=ps, c=c: e.tensor_tensor(bnv[:, c, :], ps[:, :], vq[:, c, :], ALU.mult),
                          r=(ps, vq), w=(bnv,))
                kb.dma('pool', fview(self.Vf)[:, :, t0:t0 + 512], vq[:, :, :], r=(vq,))
                kb.dma('pool', fview(self.Gf)[:, :, t0:t0 + 512], gq[:, :, :], r=(gq,))
                kb.dma('pool', fview(self.BNVf)[:, :, t0:t0 + 512], bnv[:, :, :], r=(bnv,))
                for j in range(4):
                    rb = rowb[ri % 2]
                    ri += 1
                    for q0 in range(0, 5, 2):
                        ps = self.next_ps()
                        nq = min(2, 5 - q0)
                        for qq in range(nq):
                            for c in range(2):
                                kb.op('pe', lambda e, ps=ps, qq=qq, c=c, q0=q0, j=j: e.transpose(
                                    ps[:, (qq * 2 + c) * 128:(qq * 2 + c + 1) * 128], Q[q0 + qq][:, c, j * 128:(j + 1) * 128],
                                    self.ident[:, 0:128]), r=(Q[q0 + qq],), w=(ps,))
                        kb.op('act' if q0 != 2 else 'dve',
                              (lambda e, ps=ps, rb=rb, q0=q0, nq=nq: e.copy(
                                  rb[:, q0:q0 + nq, :], ps[:, 0:nq * 256].rearrange("p (q f) -> p q f", f=256))) if q0 != 2 else
                              (lambda e, ps=ps, rb=rb, q0=q0, nq=nq: e.tensor_copy(
                                  rb[:, q0:q0 + nq, :], ps[:, 0:nq * 256].rearrange("p (q f) -> p q f", f=256))),
                              r=(ps,), w=(rb,))
                    tl = n * 512 + j * 128
                    kb.dma('pool', self.ROWS[tl:tl + 128, :, si, :], rb[:, :, :], r=(rb,))

    def rwkv_scan(self, l):
        kb = self.kb
        NS = self.NS
        NH = 4 * NS
        W_ = NH * 64
        St = kb.sb("rs_S", [64, W_], F32)
        tmp = [kb.sb(f"rs_tmp{i}", [64, W_], F32) for i in range(2)]
        tmp2 = [kb.sb(f"rs_tp{i}", [64, W_], F32) for i in range(2)]
        sa = [kb.sb(f"rs_sa{i}", [64, NH], F32) for i in range(2)]
        NBB = 4
        bt = [kb.sb(f"rs_bt{i}", [64, NBB, 5, 2, 256], F32) for i in range(2)]
        VB = 64
        vv = [kb.sb(f"rs_vv{i}", [64, NH, VB], F32) for i in range(2)]
        oo = [kb.sb(f"rs_oo{i}", [64, NH, VB], F32) for i in range(2)]
        kb.op('dve', lambda e: e.memset(St[:, :], 0.0), w=(St,))
        v3 = lambda ap: ap.rearrange("p (h k) -> p h k", k=64)
        hview = lambda dr: dr.rearrange("(h p) t -> p h t", p=64)
        for blk in range(S // VB):
            tB = blk * VB
            V_, O_ = vv[blk % 2], oo[blk % 2]
            for si in range(NS):
                kb.dma('sp', V_[:, si * 4:(si + 1) * 4, :], hview(self.Vf)[:, :, si * S + tB:si * S + tB + VB], w=(V_,))
            for sb_ in range(VB // NBB):
                t0 = tB + sb_ * NBB
                B_ = bt[sb_ % 2]
                kb.dma('sp', B_[:, :, :, 0:NS, :], self.ROWS[t0:t0 + NBB, :, 0:NS, :].partition_broadcast(64), w=(B_,))
                for j in range(NBB):
                    tt = sb_ * NBB + j
                    X = lambda q: B_[:, j, q, 0:NS, :]
                    X2 = lambda q: B_[:, j, q, 0:NS, :].rearrange("p s (h k) -> p (s h) k", k=64)
                    tm, t2, s_ = tmp[tt % 2], tmp2[tt % 2], sa[tt % 2]
                    S3 = St[:, :].rearrange("p (s f) -> p s f", s=NS)
                    tm3 = tm[:, :].rearrange("p (s f) -> p s f", s=NS)
                    kb.op('pool', lambda e, t2=t2, X2=X2, V_=V_, tt=tt: e.tensor_tensor(
                        v3(t2[:, :]), X2(3), V_[:, :, tt:tt + 1].to_broadcast([64, NH, 64]), ALU.mult), r=(B_, V_), w=(t2,))
                    kb.op('dve', lambda e, tm3=tm3, S3=S3, X=X: e.tensor_tensor(tm3, S3, X(1), ALU.mult), r=(St, B_), w=(tm,))
                    kb.op('dve', lambda e, tm=tm, s_=s_: e.tensor_reduce(s_[:, :], v3(tm[:, :]), AX.X, ALU.add), r=(tm,), w=(s_,))
                    kb.op('dve', lambda e, S3=S3, X=X: e.tensor_tensor(S3, S3, X(0), ALU.mult), r=(St, B_), w=(St,))
                    kb.op('dve', lambda e, tm=tm, X2=X2, s_=s_: e.tensor_tensor(
                        v3(tm[:, :]), X2(2), s_[:, :].unsqueeze(2).to_broadcast([64, NH, 64]), ALU.mult), r=(B_, s_), w=(tm,))
                    kb.op('dve', lambda e, tm=tm: e.tensor_tensor(St[:, :], St[:, :], tm[:, :], ALU.add), r=(St, tm), w=(St,))
                    kb.op('dve', lambda e, t2=t2: e.tensor_tensor(St[:, :], St[:, :], t2[:, :], ALU.add), r=(St, t2), w=(St,))
                    kb.op('dve', lambda e, tm3=tm3, S3=S3, X=X: e.tensor_tensor(tm3, S3, X(4), ALU.mult), r=(St, B_), w=(tm,))
                    kb.op('dve', lambda e, tm=tm, O_=O_, tt=tt: e.tensor_reduce(O_[:, :, tt], v3(tm[:, :]), AX.X, ALU.add),
                          r=(tm,), w=(O_,))
            for si in range(NS):
                kb.dma('pool', hview(self.Of)[:, :, si * S + tB:si * S + tB + VB], O_[:, si * 4:(si + 1) * 4, :], r=(O_,))

    def rwkv_post(self, l):
        kb = self.kb
        ci = lambda k: self.cidx[(k, l)]
        col = lambda i: self.cols[:, i:i + 1]
        blk = self.cm[:, 640:768]
        ot = [kb.sb(f"rp_o{i}", [128, 512], F32) for i in range(2)]
        bt_ = [kb.sb(f"rp_b{i}", [128, 512], F32) for i in range(2)]
        gt = [kb.sb(f"rp_g{i}", [128, 512], F32) for i in range(2)]
        sq = kb.sb("rp_sq", [128, 512], F32)
        mean = kb.sb("rp_mean", [128, 512], F32)
        msq = kb.sb("rp_msq", [128, 512], F32)
        rstd = kb.sb("rp_rstd", [128, 512], F32)
        t1 = kb.sb("rp_t1", [128, 512], F32)
        ob = [kb.sb(f"rp_ob{i}", [128, 512], BF16) for i in range(2)]
        i = 0
        for c in range(2):
            for n in range(self.NT // 512):
                ts_ = slice(n * 512, (n + 1) * 512)
                o, b_, g_ = ot[i % 2], bt_[i % 2], gt[i % 2]
                rows = slice(c * 128, (c + 1) * 128)
                kb.dma('sp', o[:, :], self.Of[rows, ts_], w=(o,))
                kb.dma('sp', b_[:, :], self.BNVf[rows, ts_], w=(b_,))
                kb.dma('sp', g_[:, :], self.Gf[rows, ts_], w=(g_,))
                ps1 = self.next_ps()
                kb.mm(ps1, ps1[:, :], [(blk, o[:, :])], r=(o,))
                kb.op('act', lambda e, o=o: e.activation(out=sq[:, :], in_=o[:, :], func=AF.Square), r=(o,), w=(sq,))
                ps2 = self.next_ps()
                kb.mm(ps2, ps2[:, :], [(blk, sq[:, :])], r=(sq,))
                kb.op('dve', lambda e, ps1=ps1: e.tensor_scalar(mean[:, :], ps1[:, :], 1.0 / 64, None, ALU.mult), r=(ps1,), w=(mean,))
                kb.op('dve', lambda e: e.tensor_tensor(msq[:, :], mean[:, :], mean[:, :], ALU.mult), r=(mean,), w=(msq,))
                kb.op('dve', lambda e, ps2=ps2: e.scalar_tensor_tensor(rstd[:, :], ps2[:, :], 1.0 / 64, msq[:, :], ALU.mult, ALU.subtract),
                      r=(ps2, msq), w=(rstd,))
                kb.op('act', lambda e: e.activation(out=rstd[:, :], in_=rstd[:, :], func=AF.Sqrt, bias=self.epsc[:, 2:3], scale=1.0),
                      r=(rstd,), w=(rstd,))
                kb.op('dve', lambda e: e.reciprocal(rstd[:, :], rstd[:, :]), r=(rstd,), w=(rstd,))
                kb.op('dve', lambda e, o=o: e.tensor_tensor(t1[:, :], o[:, :], mean[:, :], ALU.subtract), r=(o, mean), w=(t1,))
                kb.op('dve', lambda e: e.tensor_tensor(t1[:, :], t1[:, :], rstd[:, :], ALU.mult), r=(t1, rstd), w=(t1,))
                kb.op('dve', lambda e, c=c: e.tensor_scalar(t1[:, :], t1[:, :], col(ci('lng') + c), col(ci('lnb') + c), ALU.mult, ALU.add),
                      r=(t1,), w=(t1,))
                kb.op('dve', lambda e, b_=b_: e.tensor_tensor(t1[:, :], t1[:, :], b_[:, :], ALU.add), r=(t1, b_), w=(t1,))
                ob_ = ob[i % 2]
                kb.op('dve', lambda e, g_=g_, ob_=ob_: e.tensor_tensor(ob_[:, :], t1[:, :], g_[:, :], ALU.mult), r=(t1, g_), w=(ob_,))
                kb.dma('pool', self.Y[rows, ts_], ob_[:, :], r=(ob_,))
                i += 1

    def mixer_conv(self, l):
        kb = self.kb
        cw, cb, cg, cbb = (self.cidx[(k, l)] for k in ('cw', 'cb', 'cg', 'cbb'))
        col = lambda i: self.cols[:, i:i + 1]
        a_t = kb.sb("cv_a", [128, S], F32)
        b_t = kb.sb("cv_b", [128, S], F32)
        u = kb.sb("cv_u", [128, S + 32], F32)
        yc = [kb.sb(f"cv_y{i}", [128, S], F32) for i in range(2)]
        sq = kb.sb("cv_sq", [128, 2, 512], F32)
        mean = kb.sb("cv_mean", [128, 512], F32)
        msq = kb.sb("cv_msq", [128, 512], F32)
        rstd = kb.sb("cv_rstd", [128, 512], F32)
        t1 = [kb.sb(f"cv_t{i}", [128, 512], F32) for i in range(2)]
        ob = [kb.sb(f"cv_o{i}", [128, 512], BF16) for i in range(2)]
        kb.op('dve', lambda e: e.memset(u[:, 0:32], 0.0), w=(u,))
        for sq_i in range(self.NS):
            tb = sq_i * S
            for c in range(2):
                kb.dma('sp', a_t[:, :], self.P_cn[c * 128:(c + 1) * 128, tb:tb + S], w=(a_t,))
                kb.dma('sp', b_t[:, :], self.P_cn[256 + c * 128:256 + (c + 1) * 128, tb:tb + S], w=(b_t,))
                kb.op('act', lambda e: e.activation(out=b_t[:, :], in_=b_t[:, :], func=AF.Sigmoid), r=(b_t,), w=(b_t,))
                kb.op('dve', lambda e: e.tensor_tensor(u[:, 30:30 + S], a_t[:, :], b_t[:, :], ALU.mult), r=(a_t, b_t), w=(u,))
                y = yc[c]
                kb.op('dve', lambda e, y=y, c=c: e.tensor_scalar(y[:, :], u[:, 0:S], col(cw + c), col(cb + c), ALU.mult, ALU.add),
                      r=(u,), w=(y,))
                for j in range(1, 31):
                    kb.op('dve', lambda e, y=y, c=c, j=j: e.scalar_tensor_tensor(
                        y[:, :], u[:, j:j + S], col(cw + j * 2 + c), y[:, :], ALU.mult, ALU.add), r=(u, y), w=(y,))
            for n in range(S // 512):
                ts_ = slice(n * 512, (n + 1) * 512)
                ps1 = self.next_ps()
                kb.mm(ps1, ps1[:, :], [(self.cm[:, 128:256], yc[c][:, ts_]) for c in range(2)], r=(yc[0], yc[1]))
                kb.op('act', lambda e: e.activation(out=sq[:, 0, :], in_=yc[0][:, ts_], func=AF.Square), r=(yc[0],), w=(sq,))
                kb.op('act', lambda e: e.activation(out=sq[:, 1, :], in_=yc[1][:, ts_], func=AF.Square), r=(yc[1],), w=(sq,))
                ps2 = self.next_ps()
                kb.mm(ps2, ps2[:, :], [(self.cm[:, 128:256], sq[:, c, :]) for c in range(2)], r=(sq,))
                kb.op('dve', lambda e: e.tensor_scalar(mean[:, :], ps1[:, :], 1.0 / 256, None, ALU.mult), r=(ps1,), w=(mean,))
                kb.op('dve', lambda e: e.tensor_tensor(msq[:, :], mean[:, :], mean[:, :], ALU.mult), r=(mean,), w=(msq,))
                kb.op('dve', lambda e: e.scalar_tensor_tensor(rstd[:, :], ps2[:, :], 1.0 / 256, msq[:, :], ALU.mult, ALU.subtract),
                      r=(ps2, msq), w=(rstd,))
                kb.op('act', lambda e: e.activation(out=rstd[:, :], in_=rstd[:, :], func=AF.Sqrt, bias=self.epsc[:, 1:2], scale=1.0),
                      r=(rstd,), w=(rstd,))
                kb.op('dve', lambda e: e.reciprocal(rstd[:, :], rstd[:, :]), r=(rstd,), w=(rstd,))
                for c in range(2):
                    t, o = t1[c], ob[c]
                    kb.op('dve', lambda e, t=t, c=c: e.tensor_tensor(t[:, :], yc[c][:, ts_], mean[:, :], ALU.subtract),
                          r=(yc[c], mean), w=(t,))
                    kb.op('dve', lambda e, t=t, c=c: e.scalar_tensor_tensor(t[:, :], t[:, :], col(cg + c), rstd[:, :], ALU.mult, ALU.mult),
                          r=(t, rstd), w=(t,))
                    kb.op('act', lambda e, t=t, o=o, c=c: e.activation(out=o[:, :], in_=t[:, :], func=AF.Silu, bias=col(cbb + c)),
                          r=(t,), w=(o,))
                    kb.dma('pool', self.Y[768 + c * 128:768 + (c + 1) * 128, tb + n * 512:tb + (n + 1) * 512], o[:, :], r=(o,))

    def _load_qk(self, dst, src_rows, xtab_rows, nd, nx, stage, tb):
        kb = self.kb
        kb.dma('sp', dst[0:nd, :], src_rows[:, tb:tb + S], w=(dst,))
        kb.dma('sp', stage[nd:nd + nx, :], xtab_rows, w=(stage,))
        kb.op('dve', lambda e: e.tensor_copy(dst[nd:nd + nx, :], stage[nd:nd + nx, :]), r=(stage,), w=(dst,))

    def _make_vp(self, Vp, vin, blocks):
        kb = self.kb
        nb = len(blocks)
        for b0 in range(0, nb, 16):
            for j in range(16):
                kb.op('pe', lambda e, j=j, sl=blocks[b0 + j]: e.transpose(
                    self.psT[:, j * 64:(j + 1) * 64], vin[0:64, sl], self.identb[0:64, 0:64]), r=(vin,), w=(self.psT,))
            kb.op('dve', lambda e, b0=b0: e.tensor_copy(
                Vp[:, b0:b0 + 16, 0:64], self.psT[:, :].rearrange("p (b d) -> p b d", d=64)), r=(self.psT,), w=(Vp,))

    def mixer_diff(self, l):
        import math
        kb = self.kb
        slg = self.cidx[('slg', l)]
        Qp = [kb.sb(f"df_q{m}", [36, S], BF16) for m in range(2)]
        Kp = [kb.sb(f"df_k{m}", [36, S], BF16) for m in range(2)]
        stg = kb.sb("df_stg", [36, S], F32)
        vin = kb.sb("df_vin", [64, S], BF16)
        Vp = kb.sb("df_vp", [128, 32, 65], BF16)
        pt = [kb.sb(f"df_pt{i}", [128, 512], BF16) for i in range(3)]
        osb = [kb.sb(f"df_o{m}", [65, 512], F32) for m in range(2)]
        rb = [kb.sb(f"df_rb{m}", [64, 512], F32) for m in range(2)]
        yy = [kb.sb(f"df_y{m}", [64, 512], F32) for m in range(2)]
        att = kb.sb("df_att", [64, 512], F32)
        sq = kb.sb("df_sq", [64, 512], F32)
        rstd = kb.sb("df_rstd", [64, 512], F32)
        ob = [kb.sb(f"df_ob{i}", [64, 512], BF16) for i in range(2)]
        lamt = kb.sb("df_lamt", [128, 128], F32)
        lsm = kb.sb("df_lsm", [128, 8], F32)
        kb.op('dve', lambda e: e.memset(Vp[:, :, 64:65], 1.0), w=(Vp,))
        kb.dma('sp', lamt[:, :], self.lamv[l], w=(lamt,))
        kb.op('dve', lambda e: e.tensor_tensor(lamt[:, 0:32], lamt[:, 0:32], lamt[:, 32:64], ALU.mult), r=(lamt,), w=(lamt,))
        kb.op('dve', lambda e: e.tensor_tensor(lamt[:, 64:96], lamt[:, 64:96], lamt[:, 96:128], ALU.mult), r=(lamt,), w=(lamt,))
        kb.op('dve', lambda e: e.tensor_reduce(lsm[:, 0:1], lamt[:, 0:32], AX.X, ALU.add), r=(lamt,), w=(lsm,))
        kb.op('dve', lambda e: e.tensor_reduce(lsm[:, 1:2], lamt[:, 64:96], AX.X, ALU.add), r=(lamt,), w=(lsm,))
        kb.op('act', lambda e: e.activation(out=lsm[:, 2:4], in_=lsm[:, 0:2], func=AF.Exp), r=(lsm,), w=(lsm,))
        kb.op('dve', lambda e: e.tensor_tensor(lsm[:, 4:5], lsm[:, 3:4], lsm[:, 2:3], ALU.subtract), r=(lsm,), w=(lsm,))
        lmi, oml = self.cidx[('lmi', l)], self.cidx[('oml', l)]
        kb.op('dve', lambda e: e.tensor_tensor(lsm[:, 5:6], lsm[:, 4:5], self.cols[:, lmi:lmi + 1], ALU.add), r=(lsm,), w=(lsm,))
        kb.op('dve', lambda e: e.tensor_tensor(lsm[:, 6:7], self.cols[:, slg:slg + 1], self.cols[:, oml:oml + 1], ALU.mult),
              r=(lsm,), w=(lsm,))
        pi = 0
        oi = 0
        for si in range(self.NS):
            tb = si * S
            for h in range(4):
                for m in range(2):
                    r0 = (h * 2 + m) * 32
                    self._load_qk(Qp[m], self.P_cq[r0:r0 + 32], self.dxq[h * 4:(h + 1) * 4, :], 32, 4, stg, tb)
                    self._load_qk(Kp[m], self.P_ck[r0:r0 + 32], self.dxk[h * 4:(h + 1) * 4, :], 32, 4, stg, tb)
                kb.dma('sp', vin[:, :], self.P_cv[h * 64:(h + 1) * 64, tb:tb + S], w=(vin,))
                self._make_vp(Vp, vin, [slice(b * 128, (b + 1) * 128) for b in range(32)])
                for qt in range(S // 512):
                    q0 = qt * 512
                    nkb = 4 * qt + 4
                    for m in range(2):
                        acc = self.pacc[m]
                        for kbi in range(nkb):
                            md = kbi - 4 * qt
                            c0 = 128 * md if md > 0 else 0
                            ps = self.next_ps()
                            kb.mm(ps, ps[:, c0:512], [(Kp[m][:, kbi * 128:(kbi + 1) * 128], Qp[m][:, q0 + c0:q0 + 512])],
                                  r=(Kp[m], Qp[m]))
                            P = pt[pi % 3]
                            pi += 1
                            kb.op('act', lambda e, P=P, ps=ps, c0=c0: e.activation(out=P[:, c0:512], in_=ps[:, c0:512], func=AF.Exp),
                                  r=(ps,), w=(P,))
                            if md >= 0:
                                kb.op('dve', lambda e, P=P, c0=c0: e.tensor_tensor(
                                    P[:, c0:c0 + 128], P[:, c0:c0 + 128], self.mUL[:, 0:128], ALU.mult), r=(P,), w=(P,))
                            kb.mm1(acc, acc[0:65, c0:512], Vp[:, kbi, :], P[:, c0:512], start=(kbi == 0), stop=(kbi == nkb - 1),
                                   r=(Vp, P))
                        kb.op('act', lambda e, m=m, acc=acc: e.copy(osb[m][:, :], acc[0:65, :]), r=(acc,), w=(osb[m],))
                    for m in range(2):
                        bc = self.next_ps()
                        kb.mm(bc, bc[0:64, :], [(self.cm[0:65, 512:576], osb[m][0:65, :])], r=(osb[m],))
                        kb.op('dve', lambda e, m=m, bc=bc: e.reciprocal(rb[m][:, :], bc[0:64, :]), r=(bc,), w=(rb[m],))
                        kb.op('dve', lambda e, m=m: e.tensor_tensor(yy[m][:, :], osb[m][0:64, :], rb[m][:, :], ALU.mult),
                              r=(osb[m], rb[m]), w=(yy[m],))
                    kb.op('dve', lambda e: e.scalar_tensor_tensor(att[:, :], yy[1][:, :], lsm[0:64, 5:6], yy[0][:, :], ALU.mult, ALU.add),
                          r=(yy[0], yy[1], lsm), w=(att,))
                    kb.op('act', lambda e: e.activation(out=sq[:, :], in_=att[:, :], func=AF.Square), r=(att,), w=(sq,))
                    ss = self.next_ps()
                    kb.mm(ss, ss[0:64, :], [(self.cm[0:64, 128:192], sq[:, :])], r=(sq,))
                    kb.op('act', lambda e, ss=ss: e.activation(out=rstd[:, :], in_=ss[0:64, :], func=AF.Sqrt, bias=self.epsc[0:64, 1:2],
                                                               scale=1.0 / 64), r=(ss,), w=(rstd,))
                    kb.op('dve', lambda e: e.reciprocal(rstd[:, :], rstd[:, :]), r=(rstd,), w=(rstd,))
                    o = ob[oi % 2]
                    oi += 1
                    kb.op('dve', lambda e, o=o: e.scalar_tensor_tensor(o[:, :], att[:, :], lsm[0:64, 6:7], rstd[:, :], ALU.mult, ALU.mult),
                          r=(att, rstd, lsm), w=(o,))
                    kb.dma('pool', self.Y[512 + h * 64:512 + (h + 1) * 64, tb + q0:tb + q0 + 512], o[:, :], r=(o,))

    def mixer_dil(self, l):
        kb = self.kb
        Qp = kb.sb("dl_q", [72, S], BF16)
        Kp = kb.sb("dl_k", [72, S], BF16)
        stg = kb.sb("dl_stg", [72, S], F32)
        vin = kb.sb("dl_vin", [64, S], BF16)
        Vp = kb.sb("dl_vp", [128, 32, 65], BF16)
        Acc = kb.sb("dl_acc", [65, S], F32)
        pt = [kb.sb(f"dl_pt{i}", [128, 256], BF16) for i in range(3)]
        rb = kb.sb("dl_rb", [64, 512], F32)
        ob = [kb.sb(f"dl_ob{i}", [64, 512], BF16) for i in range(2)]
        kb.op('dve', lambda e: e.memset(Vp[:, :, 64:65], 1.0), w=(Vp,))
        pi = 0
        oi = 0
        for si in range(self.NS):
            tb = si * S
            for h in range(4):
                for g, (win, d) in enumerate(DIL_PAT):
                    gh = g * 4 + h
                    nb = S // d // 128
                    self._load_qk(Qp, self.P_dq[gh * 64:(gh + 1) * 64], self.dlq[gh * 8:(gh + 1) * 8, :], 64, 8, stg, tb)
                    self._load_qk(Kp, self.P_dk[gh * 64:(gh + 1) * 64], self.dlk[gh * 8:(gh + 1) * 8, :], 64, 8, stg, tb)
                    kb.dma('sp', vin[:, :], self.P_dv[gh * 64:(gh + 1) * 64, tb:tb + S], w=(vin,))
                    blk = lambda r, c: slice(r + d * 128 * c, r + d * 128 * c + d * 127 + 1, d)
                    blocks = [blk(r, c) for r in range(d) for c in range(nb)]
                    self._make_vp(Vp, vin, blocks)
                    for r in range(d):
                        for c in range(nb):
                            bi = r * nb + c
                            sc = blocks[bi]
                            ps = self.next_ps()
                            kb.mm(ps, ps[:, 0:128], [(Kp[:, sc], Qp[:, sc])], r=(Kp, Qp))
                            w_ = 128
                            if c > 0:
                                kb.mm(ps, ps[:, 128:256], [(Kp[:, blocks[bi - 1]], Qp[:, sc])], r=(Kp, Qp))
                                w_ = 256
                            P = pt[pi % 3]
                            pi += 1
                            kb.op('act', lambda e, P=P, ps=ps, w_=w_: e.activation(out=P[:, 0:w_], in_=ps[:, 0:w_], func=AF.Exp),
                                  r=(ps,), w=(P,))
                            kb.op('dve', lambda e, P=P, w_=w_: e.tensor_tensor(P[:, 0:w_], P[:, 0:w_], self.mUL[:, 0:w_], ALU.mult),
                                  r=(P,), w=(P,))
                            po = self.next_ps()
                            pairs = [(Vp[:, bi, :], P[:, 0:128])]
                            if c > 0:
                                pairs.append((Vp[:, bi - 1, :], P[:, 128:256]))
                            kb.mm(po, po[0:65, 0:128], pairs, r=(Vp, P))
                            if g == 0:
                                kb.op('act', lambda e, po=po, sc=sc: e.copy(Acc[:, sc], po[0:65, 0:128]), r=(po,), w=(Acc,))
                            else:
                                kb.op('dve', lambda e, po=po, sc=sc: e.tensor_tensor(Acc[:, sc], Acc[:, sc], po[0:65, 0:128], ALU.add),
                                      r=(po, Acc), w=(Acc,))
                for n in range(S // 512):
                    ts_ = slice(n * 512, (n + 1) * 512)
                    bc = self.next_ps()
                    kb.mm(bc, bc[0:64, :], [(self.cm[0:65, 512:576], Acc[0:65, ts_])], r=(Acc,))
                    kb.op('dve', lambda e, bc=bc: e.reciprocal(rb[:, :], bc[0:64, :]), r=(bc,), w=(rb,))
                    o = ob[oi % 2]
                    oi += 1
                    kb.op('dve', lambda e, o=o, ts_=ts_: e.tensor_tensor(o[:, :], Acc[0:64, ts_], rb[:, :], ALU.mult), r=(Acc, rb), w=(o,))
                    kb.dma('pool', self.Y[256 + h * 64:256 + (h + 1) * 64, tb + n * 512:tb + (n + 1) * 512], o[:, :], r=(o,))

    def _post_norm_add(self, xt, y, gcol0, sq, rstd, tmp):
        kb = self.kb
        kb.op('act', lambda e: e.activation(out=sq[:, :, :], in_=y[:, :, :], func=AF.Square), r=(y,), w=(sq,))
        ps = self.next_ps()
        kb.mm(ps, ps[:, :], [(self.cm[:, 128:256], sq[:, kc, :]) for kc in range(KC)], r=(sq, self.cm))
        kb.op('act', lambda e: e.activation(out=rstd[:, :], in_=ps[:, :], func=AF.Sqrt, bias=self.epsc[:, 0:1], scale=1.0 / D),
              r=(ps,), w=(rstd,))
        kb.op('dve', lambda e: e.reciprocal(rstd[:, :], rstd[:, :]), r=(rstd,), w=(rstd,))
        for kc in range(KC):
            kb.op('dve', lambda e, kc=kc: e.scalar_tensor_tensor(
                tmp[:, kc, :], y[:, kc, :], self.cols[:, gcol0 + kc:gcol0 + kc + 1], rstd[:, :],
                ALU.mult, ALU.mult), r=(y, rstd), w=(tmp,))
        kb.op('pool', lambda e: e.tensor_tensor(xt[:, :, :], xt[:, :, :], tmp[:, :, :], ALU.add), r=(tmp, xt), w=(xt,))

    def stage_merge(self, l):
        kb, NT = self.kb, self.NT
        wbr = kb.sb("mg_wbr", [128, 8, D], BF16)
        wo = kb.sb("mg_wo", [128, KC, D], BF16)
        kb.dma('sp', wbr[:, :, :], self.wb_br[l].rearrange("(c p) d -> p c d", p=128), w=(wbr,))
        kb.dma('sp', wo[:, :, :], self.wb_out[l].rearrange("(c p) d -> p c d", p=128), w=(wo,))
        yt = [kb.sb(f"mg_y{i}", [128, 8, 512], BF16) for i in range(2)]
        gt = [kb.sb(f"mg_g{i}", [128, 32, 512], BF16) for i in range(2)]
        xt = [kb.sb(f"mg_x{i}", [128, KC, 512], F32) for i in range(2)]
        acc = [kb.sb(f"mg_acc{i}", [128, 512], F32) for i in range(2)]
        tm = [kb.sb(f"mg_tm{i}", [128, 512], F32) for i in range(2)]
        mT = kb.sb("mg_m", [128, KC, 512], BF16)
        yo = kb.sb("mg_yo", [128, KC, 512], F32)
        sq = kb.sb("mg_sq", [128, KC, 512], F32)
        rstd = kb.sb("mg_rstd", [128, 512], F32)
        Yv = self.Y.rearrange("(c p) t -> p c t", p=128)
        Gv = self.P_g.rearrange("(c p) t -> p c t", p=128)
        xTv = self.xT.rearrange("(kc p) t -> p kc t", p=128)
        g0 = self.cidx[('nmo', l)]
        for n in range(NT // 512):
            ts_ = slice(n * 512, (n + 1) * 512)
            y, g, x = yt[n % 2], gt[n % 2], xt[n % 2]
            kb.dma('sp', y[:, :, :], Yv[:, :, ts_], w=(y,))
            for gq_ in range(4):
                kb.dma('sp', g[:, gq_ * 8:(gq_ + 1) * 8, :], Gv[:, gq_ * 8:(gq_ + 1) * 8, ts_], w=(g,))
            kb.dma('sp', x[:, :, :], xTv[:, :, ts_], w=(x,))
            for dc in range(KC):
                a = acc[dc % 2]
                for br in range(4):
                    ps = self.next_ps()
                    kb.mm(ps, ps[:, :], [(wbr[:, br * 2 + c, dc * 128:(dc + 1) * 128], y[:, br * 2 + c, :]) for c in range(2)],
                          r=(wbr, y))
                    if br == 0:
                        kb.op('dve', lambda e, a=a, ps=ps, g=g, br=br, dc=dc: e.tensor_tensor(
                            a[:, :], ps[:, :], g[:, br * 8 + dc, :], ALU.mult), r=(ps, g), w=(a,))
                    else:
                        t = tm[br % 2]
                        kb.op('dve', lambda e, t=t, ps=ps, g=g, br=br, dc=dc: e.tensor_tensor(
                            t[:, :], ps[:, :], g[:, br * 8 + dc, :], ALU.mult), r=(ps, g), w=(t,))
                        if br < 3:
                            kb.op('pool', lambda e, a=a, t=t: e.tensor_tensor(a[:, :], a[:, :], t[:, :], ALU.add), r=(t, a), w=(a,))
                        else:
                            kb.op('pool', lambda e, a=a, t=t, dc=dc: e.tensor_tensor(mT[:, dc, :], a[:, :], t[:, :], ALU.add),
                                  r=(t, a), w=(mT,))
            for oc in range(KC):
                ps = self.next_ps()
                kb.mm(ps, ps[:, :], [(wo[:, kc, oc * 128:(oc + 1) * 128], mT[:, kc, :]) for kc in range(KC)], r=(wo, mT))
                kb.op('act', lambda e, ps=ps, oc=oc: e.copy(yo[:, oc, :], ps[:, :]), r=(ps,), w=(yo,))
            self._post_norm_add(x, yo, g0, sq, rstd, sq)
            kb.dma('pool', xTv[:, :, ts_], x[:, :, :], r=(x,))

    def stage_ffn(self, l):
        kb, NT = self.kb, self.NT
        xt = [kb.sb(f"ff_x{i}", [128, KC, 512], F32) for i in range(2)]
        hT = kb.sb("ff_h", [128, KC, 512], BF16)
        aT = kb.sb("ff_a", [128, FC, 512], BF16)
        sq = kb.sb("ff_sq", [128, KC, 512], F32)
        yo = kb.sb("ff_yo", [128, KC, 512], F32)
        rstd = kb.sb("ff_rstd", [128, 512], F32)
        sg = [kb.sb(f"ff_sg{i}", [128, 512], F32) for i in range(2)]
        NW = 3
        wg = [kb.sb(f"ff_wg{i}", [128, KC, 256], BF16) for i in range(NW)]
        wu = [kb.sb(f"ff_wu{i}", [128, KC, 256], BF16) for i in range(NW)]
        wd = [kb.sb(f"ff_wd{i}", [128, FC, 256], BF16) for i in range(2)]
        xTv = self.xT.rearrange("(kc p) t -> p kc t", p=128)
        wgv = self.wb_g[l].rearrange("(kc p) n -> p kc n", p=128)
        wuv = self.wb_u[l].rearrange("(kc p) n -> p kc n", p=128)
        wdv = self.wb_d[l].rearrange("(kc p) n -> p kc n", p=128)
        gpre, gpost = self.cidx[('nfp', l)], self.cidx[('nfo', l)]
        wi = 0
        di = 0
        for n in range(NT // 512):
            ts_ = slice(n * 512, (n + 1) * 512)
            x = xt[n % 2]
            kb.dma('sp', x[:, :, :], xTv[:, :, ts_], w=(x,))
            self._norm_into(x, hT, 0, hT, gpre, sq, rstd)
            for f2 in range(FC // 2):
                g_, u_ = wg[wi % NW], wu[wi % NW]
                wi += 1
                kb.dma('sp', g_[:, :, :], wgv[:, :, f2 * 256:(f2 + 1) * 256], w=(g_,))
                kb.dma('sp', u_[:, :, :], wuv[:, :, f2 * 256:(f2 + 1) * 256], w=(u_,))
                for j in range(2):
                    fc = f2 * 2 + j
                    pg = self.next_ps()
                    kb.mm(pg, pg[:, :], [(g_[:, kc, j * 128:(j + 1) * 128], hT[:, kc, :]) for kc in range(KC)], r=(g_, hT))
                    pu = self.next_ps()
                    kb.mm(pu, pu[:, :], [(u_[:, kc, j * 128:(j + 1) * 128], hT[:, kc, :]) for kc in range(KC)], r=(u_, hT))
                    sgt = sg[fc % 2]
                    kb.op('act', lambda e, sgt=sgt, pg=pg: e.activation(out=sgt[:, :], in_=pg[:, :], func=AF.Silu), r=(pg,), w=(sgt,))
                    kb.op('dve', lambda e, sgt=sgt, pu=pu, fc=fc: e.tensor_tensor(aT[:, fc, :], sgt[:, :], pu[:, :], ALU.mult),
                          r=(sgt, pu), w=(aT,))
            for o2 in range(KC // 2):
                d_ = wd[di % 2]
                di += 1
                for f0, f1 in ((0, 8), (8, 15), (15, 22)):
                    kb.dma('sp', d_[:, f0:f1, :], wdv[:, f0:f1, o2 * 256:(o2 + 1) * 256], w=(d_,))
                for j in range(2):
                    oc = o2 * 2 + j
                    ps = self.next_ps()
                    kb.mm(ps, ps[:, :], [(d_[:, fc, j * 128:(j + 1) * 128], aT[:, fc, :]) for fc in range(FC)], r=(d_, aT))
                    kb.op('act', lambda e, ps=ps, oc=oc: e.copy(yo[:, oc, :], ps[:, :]), r=(ps,), w=(yo,))
            self._post_norm_add(x, yo, gpost, sq, rstd, sq)
            kb.dma('pool', xTv[:, :, ts_], x[:, :, :], r=(x,))


DIL_PAT = ((128, 1), (512, 4), (2048, 16))


def _bf16_round(v):
    import ml_dtypes
    return np.asarray(v, np.float32).astype(ml_dtypes.bfloat16).astype(np.float32)


def pos_tables():
    pos = np.arange(S)
    hi_, lo_ = (pos // 128 * 128).astype(np.float32), (pos % 128).astype(np.float32)
    one = np.ones(S, np.float32)
    dsl = 2.0 ** (-8.0 * np.arange(1, 5) / 4)
    dxq = np.zeros((16, S), np.float32)
    dxk = np.zeros((16, S), np.float32)
    for h in range(4):
        sl = np.float32(dsl[h])
        dxk[h * 4:(h + 1) * 4] = np.stack([hi_, lo_, -sl * one, -sl * one])
        dxq[h * 4:(h + 1) * 4] = np.stack([sl * one, sl * one, hi_, lo_])
    asl = 2.0 ** (-8.0 * np.arange(1, 13) / 12)
    dlq = np.zeros((96, S), np.float32)
    dlk = np.zeros((96, S), np.float32)
    for g, (win, d) in enumerate(DIL_PAT):
        Tt = pos // d
        th, tl = (Tt // 128 * 128).astype(np.float32), (Tt % 128).astype(np.float32)
        for h in range(4):
            gh = g * 4 + h
            sd = np.float32(asl[gh]) * np.float32(d)
            shi = _bf16_round(sd)
            slo = _bf16_round(np.float32(sd) - shi)
            dlk[gh * 8:(gh + 1) * 8] = np.stack([th, th, tl, tl, -shi * one, -slo * one, -shi * one, -slo * one])
            dlq[gh * 8:(gh + 1) * 8] = np.stack([shi * one, slo * one, shi * one, slo * one, th, th, tl, tl])
    return dxq, dxk, dlq, dlk


def make_inmaps(inp, L, NS, ncores, l0=0, x=None):
    inp = dict(inp)
    for k_ in list(inp.keys()):
        if k_ != 'x':
            inp[k_] = np.asarray(inp[k_])[l0:l0 + L]
    cols = build_cols(inp, L, l0)
    ii = np.arange(128)
    U = (ii[:, None] <= ii[None, :]).astype(np.float32)
    Lm = (ii[:, None] >= ii[None, :]).astype(np.float32)
    sel = np.zeros((128, 128), np.float32)
    sel[64, :] = 1.0
    blk = np.zeros((128, 128), np.float32)
    blk[0:64, 0:64] = 1.0
    blk[64:128, 64:128] = 1.0
    cm = np.concatenate([np.eye(128, dtype=np.float32), np.ones((128, 128), np.float32), U, Lm, sel, blk], axis=1)
    if x is None:
        x = inp['x']
    x = np.ascontiguousarray(np.asarray(x, np.float32)).reshape(-1, D)
    dxq, dxk, dlq, dlk = pos_tables()
    lamv = np.stack([np.broadcast_to(np.concatenate([inp['diff_lam_q1'][l], inp['diff_lam_k1'][l],
                                                      inp['diff_lam_q2'][l], inp['diff_lam_k2'][l]])[None, :], (128, 128))
                     for l in range(L)]).astype(np.float32)
    lamv = np.ascontiguousarray(lamv)
    maps = []
    for c in range(ncores):
        m = {
            "x": np.ascontiguousarray(x[c * NS * S:(c + 1) * NS * S]),
            "w_in": np.ascontiguousarray(inp['w_in'][:L]),
            "w_branch": np.ascontiguousarray(inp['w_branch'][:L]),
            "w_out": np.ascontiguousarray(inp['w_out'][:L]),
            "ffn_w_gate": np.ascontiguousarray(inp['ffn_w_gate'][:L]),
            "ffn_w_up": np.ascontiguousarray(inp['ffn_w_up'][:L]),
            "ffn_w_down": np.ascontiguousarray(inp['ffn_w_down'][:L]),
            "cols": cols,
            "cmats": cm,
            "dxq": dxq, "dxk": dxk, "dlq": dlq, "dlk": dlk, "lamv": lamv,
            "wa_up": np.ascontiguousarray(np.concatenate([inp['rwkv_w_up'][:L], inp['rwkv_a_up'][:L]], axis=1).astype(np.float32)),
            "g_up": np.ascontiguousarray(inp['rwkv_g_up'][:L].astype(np.float32)),
        }
        maps.append(m)
    return maps


def kernel(**inputs):
    inp = {k: np.asarray(v) for k, v in inputs.items()}
    prog = Prog(L=1, NS=2)
    nc = prog.build()
    x = np.asarray(inp['x'], np.float32).reshape(-1, D)
    for l in range(DEPTH):
        maps = make_inmaps(inp, 1, 2, 8, l0=l, x=x)
        res = run_bass_kernel_spmd(nc, maps, core_ids=list(range(8)))
        x = np.concatenate([r["out"] for r in res.results], axis=0)
    return x.reshape(16, S, D).astype(np.float32)
```

```python
import numpy as np
import concourse.bass as bass
import concourse.mybir as mybir
from concourse.bass_utils import run_bass_kernel_spmd

F32 = mybir.dt.float32
BF16 = mybir.dt.bfloat16
AF = mybir.ActivationFunctionType
ALU = mybir.AluOpType
AX = mybir.AxisListType

D = 1024
S = 4096
DEPTH = 4
KC = 8
IN_COLS = 8704
D_FF = 2816
FC = 22
RW0, DIL0, DIF0, CONV0, GATE0 = 0, 1024, 3328, 4096, 4608
SEM_LIMIT = 30000
NCM = 768


class T:
    def __init__(self, h, const=False):
        self.h = h
        self.w = None
        self.rd = {}
        self.const = const

    def __getitem__(self, idx):
        return self.h[idx]


class Eng:
    def __init__(self, kb, name, e, is_pe=False):
        self.kb, self.name, self.e, self.is_pe = kb, name, e, is_pe
        self.gen = 0
        self.sem = kb.nc.alloc_semaphore(f"s_{name}_0")
        self.cnt = 0
        self.seen = {}
        self.last = None
        self.dsems = []
        self.di = 0

    def signal(self, ins):
        if self.cnt >= SEM_LIMIT:
            self.gen += 1
            self.sem = self.kb.nc.alloc_semaphore(f"s_{self.name}_{self.gen}")
            self.cnt = 0
        self.cnt += 1
        ins.then_inc(self.sem, 1)
        ev = (self.sem, self.cnt, self)
        self.last = ev
        return ev


class KB:
    def __init__(self, nc, ndma_sems=12):
        self.nc = nc
        self.eng = {
            'pe': Eng(self, 'pe', nc.tensor, True),
            'act': Eng(self, 'act', nc.scalar),
            'dve': Eng(self, 'dve', nc.vector),
            'pool': Eng(self, 'pool', nc.gpsimd),
            'sp': Eng(self, 'sp', nc.sync),
        }
        self.ndma = ndma_sems
        self.dma_latest = {}
        self.n_ins = 0
        self.uid = 0

    def sb(self, name, shape, dt, const=False):
        self.uid += 1
        return T(self.nc.alloc_sbuf_tensor(f"{name}_{self.uid}", list(shape), dt), const)

    def ps(self, name, shape=(128, 512), dt=F32):
        self.uid += 1
        return T(self.nc.alloc_psum_tensor(f"{name}_{self.uid}", list(shape), dt))

    def _wait(self, E, deps):
        best = {}
        for d in deps:
            if d is None:
                continue
            sem, val, src = d
            if src is E and E.is_pe:
                continue
            k = id(sem)
            if E.seen.get(k, 0) >= val:
                continue
            if k not in best or best[k][1] < val:
                best[k] = (sem, val)
        for k, (sem, val) in best.items():
            E.e.wait_ge(sem, val)
            E.seen[k] = val
            self.n_ins += 1

    def _deps(self, r, w):
        deps = []
        for t in r:
            if t.w is not None:
                deps.append(t.w)
        for t in w:
            if t.w is not None:
                deps.append(t.w)
            deps.extend(t.rd.values())
        return deps

    def _mark(self, ev, r, w):
        for t in w:
            t.w = ev
            t.rd = {}
        for t in r:
            if t.const or t in w:
                continue
            t.rd[id(ev[0])] = ev

    def op(self, en, fn, r=(), w=(), extra=()):
        E = self.eng[en]
        self._wait(E, self._deps(r, w) + list(extra))
        ins = fn(E.e)
        ev = E.signal(ins)
        self._mark(ev, r, w)
        self.n_ins += 1
        return ev

    def mm(self, out_t, out_ap, pairs, r=(), extra=()):
        E = self.eng['pe']
        self._wait(E, self._deps(r, (out_t,)) + list(extra))
        n = len(pairs)
        ins = None
        for i, (l, rh) in enumerate(pairs):
            ins = E.e.matmul(out_ap, l, rh, start=(i == 0), stop=(i == n - 1))
            self.n_ins += 1
        ev = E.signal(ins)
        self._mark(ev, r, (out_t,))
        return ev

    def mm1(self, out_t, out_ap, l, rh, start, stop, r=(), sig=True):
        E = self.eng['pe']
        self._wait(E, self._deps(r, (out_t,)))
        ins = E.e.matmul(out_ap, l, rh, start=start, stop=stop)
        self.n_ins += 1
        if not sig:
            return None
        ev = E.signal(ins)
        self._mark(ev, r, (out_t,))
        return ev

    def dma(self, qn, out, in_, r=(), w=(), extra=(), **kw):
        E = self.eng[qn]
        if len(E.dsems) < self.ndma:
            E.dsems.append([self.nc.alloc_semaphore(f"d_{qn}_{len(E.dsems)}_0"), 0, 0])
        slot = E.dsems[E.di % self.ndma]
        E.di += 1
        if slot[1] * 16 >= SEM_LIMIT:
            slot[2] += 1
            slot[0] = self.nc.alloc_semaphore(f"d_{qn}_{E.di % self.ndma}_{slot[2]}")
            slot[1] = 0
        prev = (slot[0], slot[1] * 16, None) if slot[1] > 0 else None
        self._wait(E, self._deps(r, w) + list(extra) + [prev])
        ins = E.e.dma_start(out=out, in_=in_, **kw)
        slot[1] += 1
        ins.then_inc(slot[0], 16)
        ev = (slot[0], slot[1] * 16, None)
        self.dma_latest[id(slot[0])] = ev
        self._mark(ev, r, w)
        self.n_ins += 1
        return ev

    def barrier(self):
        evs = [E.last for E in self.eng.values() if E.last is not None]
        evs += list(self.dma_latest.values())
        for E in self.eng.values():
            self._wait(E, [e for e in evs if not (e[2] is E and E.is_pe)])
        self.dma_latest = {}


def colblock(v):
    v = np.asarray(v, np.float32).reshape(-1)
    n = v.size // 128
    return np.ascontiguousarray(v.reshape(n, 128).T)


class ColTable:
    def __init__(self):
        self.parts, self.idx, self.n = [], {}, 0

    def add(self, name, v):
        b = colblock(v)
        self.idx[name] = self.n
        self.parts.append(b)
        self.n += b.shape[1]

    def build(self):
        return np.ascontiguousarray(np.concatenate(self.parts, axis=1))


def col_layout(L):
    idx, n = {}, 0
    for l in range(L):
        for nm, w in (('nmp', 8), ('nmo', 8), ('nfp', 8), ('nfo', 8), ('gb', 32), ('mu', 8), ('cw', 62), ('cb', 2), ('cg', 2), ('cbb', 2), ('slg', 1), ('w0', 2), ('a0', 2), ('kk', 2), ('ka', 2), ('rk', 2), ('lng', 2), ('lnb', 2), ('lmi', 1), ('oml', 1)):
            idx[(nm, l)] = n
            n += w
    return idx, n


def build_cols(inp, L, l0=0):
    import math
    ct = ColTable()
    for l in range(L):
        ct.add(('nmp', l), inp['norm_mix_pre'][l])
        ct.add(('nmo', l), inp['norm_mix_post'][l])
        ct.add(('nfp', l), inp['norm_ffn_pre'][l])
        ct.add(('nfo', l), inp['norm_ffn_post'][l])
        ct.add(('gb', l), inp['gate_bias'][l])
        ct.add(('mu', l), inp['rwkv_mu'][l])
        ct.add(('cw', l), inp['conv_dw_w'][l].reshape(-1))
        ct.add(('cb', l), inp['conv_dw_b'][l])
        ct.add(('cg', l), inp['conv_ln_g'][l])
        ct.add(('cbb', l), inp['conv_ln_b'][l])
        ct.add(('slg', l), np.concatenate([inp['diff_subln_g'][l], np.zeros(64, np.float32)]))
        ct.add(('w0', l), inp['rwkv_w0'][l])
        ct.add(('a0', l), inp['rwkv_a0'][l])
        ct.add(('kk', l), inp['rwkv_k_k'][l])
        ct.add(('ka', l), inp['rwkv_k_a'][l])
        ct.add(('rk', l), inp['rwkv_r_k'][l].reshape(-1))
        ct.add(('lng', l), inp['rwkv_ln_g'][l])
        ct.add(('lnb', l), inp['rwkv_ln_b'][l])
        li = 0.8 - 0.6 * math.exp(-0.3 * (l0 + l))
        ct.add(('lmi', l), np.full(128, -li, np.float32))
        ct.add(('oml', l), np.full(128, 1.0 - li, np.float32))
    return ct.build()


class Prog:
    def __init__(self, L=DEPTH, NS=2, debug=()):
        self.L, self.NS, self.NT = L, NS, NS * S
        self.debug = set(debug)
        nc = bass.Bass("TRN2", target_bir_lowering=False)
        self.nc = nc
        self.kb = KB(nc)
        NT = self.NT
        ein = lambda n, s, d=F32: nc.dram_tensor(n, list(s), d, kind="ExternalInput").ap()
        self.x = ein("x", [NT, D])
        self.w_in = ein("w_in", [L, D, IN_COLS])
        self.w_branch = ein("w_branch", [L, 4, 256, D])
        self.w_out = ein("w_out", [L, D, D])
        self.w_g = ein("ffn_w_gate", [L, D, D_FF])
        self.w_u = ein("ffn_w_up", [L, D, D_FF])
        self.w_d = ein("ffn_w_down", [L, D_FF, D])
        self.cidx, ncols = col_layout(L)
        self.cols_d = ein("cols", [128, ncols])
        self.cm_d = ein("cmats", [128, NCM])
        self.dxq = ein("dxq", [16, S])
        self.dxk = ein("dxk", [16, S])
        self.dlq = ein("dlq", [96, S])
        self.dlk = ein("dlk", [96, S])
        self.lamv = ein("lamv", [L, 128, 128])
        self.wa_up = ein("wa_up", [L, 128, 256])
        self.g_up = ein("g_up", [L, 128, 256])
        self.out = nc.dram_tensor("out", [NT, D], F32, kind="ExternalOutput").ap()
        self.ncols = ncols

    def scratch(self, name, shape, dt):
        kind = "ExternalOutput" if name in self.debug else "Internal"
        return self.nc.dram_tensor(name, list(shape), dt, kind=kind).ap()

    def build(self):
        nc, kb, L, NT = self.nc, self.kb, self.L, self.NT
        self.cols = kb.sb("cols", [128, self.ncols], F32, const=True)
        self.cm = kb.sb("cmats", [128, NCM], F32, const=True)
        self.identb = kb.sb("identb", [128, 128], BF16, const=True)
        kb.dma('sp', self.cols[:, :], self.cols_d[:, :], w=(self.cols,))
        kb.dma('sp', self.cm[:, :], self.cm_d[:, :], w=(self.cm,))
        kb.op('dve', lambda e: e.tensor_copy(self.identb[:, :], self.cm[:, 0:128]), r=(self.cm,), w=(self.identb,))
        self.ident = self.cm
        self.epsc = kb.sb("epsc", [128, 4], F32, const=True)
        kb.op('dve', lambda e: e.memset(self.epsc[:, 0:1], 1e-6), w=(self.epsc,))
        kb.op('dve', lambda e: e.memset(self.epsc[:, 1:2], 1e-5), w=(self.epsc,))
        kb.op('dve', lambda e: e.memset(self.epsc[:, 2:3], 64e-5), w=(self.epsc,))
        kb.op('dve', lambda e: e.memset(self.epsc[:, 3:4], 0.0), w=(self.epsc,))
        self.psb = [kb.ps(f"psb{i}") for i in range(4)]
        self.psi = 0
        self.pacc = [kb.ps(f"pacc{i}") for i in range(3)]
        self.psT = kb.ps("psT", (128, 1024), BF16)
        self.mUL = kb.sb("mUL", [128, 256], BF16, const=True)
        kb.op('dve', lambda e: e.tensor_copy(self.mUL[:, :], self.cm[:, 256:512]), r=(self.cm,), w=(self.mUL,))
        self.xT = self.scratch("xT", [D, NT], F32)
        self.wb_in = self.scratch("wb_in", [L, D, IN_COLS], BF16)
        self.wb_br = self.scratch("wb_br", [L, 1024, D], BF16)
        self.wb_out = self.scratch("wb_out", [L, D, D], BF16)
        self.wb_g = self.scratch("wb_g", [L, D, D_FF], BF16)
        self.wb_u = self.scratch("wb_u", [L, D, D_FF], BF16)
        self.wb_d = self.scratch("wb_d", [L, D_FF, D], BF16)
        self.P_rw = self.scratch("P_rw", [1024, NT], F32)
        self.P_dq = self.scratch("P_dq", [768, NT], BF16)
        self.P_dk = self.scratch("P_dk", [768, NT], BF16)
        self.P_dv = self.scratch("P_dv", [768, NT], BF16)
        self.P_cq = self.scratch("P_cq", [256, NT], BF16)
        self.P_ck = self.scratch("P_ck", [256, NT], BF16)
        self.P_cv = self.scratch("P_cv", [256, NT], BF16)
        self.P_cn = self.scratch("P_cn", [512, NT], F32)
        self.P_g = self.scratch("P_g", [4096, NT], BF16)
        self.Y = self.scratch("Y", [1024, NT], BF16)
        self.ROWS = self.scratch("ROWS", [S, 5, 2, 256], F32)
        self.Vf = self.scratch("Vf", [256, NT], F32)
        self.Gf = self.scratch("Gf", [256, NT], F32)
        self.BNVf = self.scratch("BNVf", [256, NT], F32)
        self.Of = self.scratch("Of", [256, NT], F32)

        sb0 = nc.sbuf_base
        self.stage_cast()
        kb.barrier()
        nc.sbuf_base = sb0
        self.stage_xin()
        kb.barrier()
        for l in range(L):
            nc.sbuf_base = sb0
            self.stage_proj(l)
            kb.barrier()
            nc.sbuf_base = sb0
            self.stage_mixers(l)
            kb.barrier()
            nc.sbuf_base = sb0
            self.stage_merge(l)
            kb.barrier()
            nc.sbuf_base = sb0
            self.stage_ffn(l)
            kb.barrier()
        nc.sbuf_base = sb0
        self.stage_xout()
        kb.barrier()
        return nc

    def next_ps(self):
        p = self.psb[self.psi % 4]
        self.psi += 1
        return p

    def stage_cast(self):
        kb, L = self.kb, self.L
        CW = 2176
        NB = 3
        fin = [kb.sb(f"cast_in{i}", [128, CW], F32) for i in range(NB)]
        fout = [kb.sb(f"cast_out{i}", [128, CW], BF16) for i in range(NB)]
        engs = ['act', 'dve', 'pool']
        jobs = []
        for l in range(L):
            jobs.append((self.wb_in[l], self.w_in[l], D, IN_COLS))
            jobs.append((self.wb_br[l], self.w_branch[l].rearrange("n c d -> (n c) d"), 1024, D))
            jobs.append((self.wb_out[l], self.w_out[l], D, D))
            jobs.append((self.wb_g[l], self.w_g[l], D, D_FF))
            jobs.append((self.wb_u[l], self.w_u[l], D, D_FF))
            jobs.append((self.wb_d[l], self.w_d[l], D_FF, D))
        i = 0
        for dst, src, rows, cols in jobs:
            for r0 in range(0, rows, 128):
                for c0 in range(0, cols, CW):
                    cw = min(CW, cols - c0)
                    a, b = fin[i % NB], fout[i % NB]
                    kb.dma('sp', a[:, 0:cw], src[r0:r0 + 128, c0:c0 + cw], w=(a,))
                    en = engs[i % 3]
                    if en == 'act':
                        kb.op('act', lambda e, a=a, b=b, cw=cw: e.copy(b[:, 0:cw], a[:, 0:cw]), r=(a,), w=(b,))
                    else:
                        kb.op(en, lambda e, a=a, b=b, cw=cw: e.tensor_copy(b[:, 0:cw], a[:, 0:cw]), r=(a,), w=(b,))
                    kb.dma('pool', dst[r0:r0 + 128, c0:c0 + cw], b[:, 0:cw], r=(b,))
                    i += 1

    def stage_xin(self):
        kb, NT = self.kb, self.NT
        NB = 2
        xin = [kb.sb(f"xin{i}", [128, 4, D], F32) for i in range(NB)]
        xo = [kb.sb(f"xo{i}", [128, KC, 512], F32) for i in range(NB)]
        xv = self.x.rearrange("(n j p) d -> n p j d", p=128, j=4)
        xTv = self.xT.rearrange("(kc p) t -> p kc t", p=128)
        for n in range(NT // 512):
            a, b = xin[n % NB], xo[n % NB]
            kb.dma('sp', a[:, :, :], xv[n], w=(a,))
            for kc in range(KC):
                ps = self.next_ps()
                for j in range(4):
                    kb.op('pe', lambda e, ps=ps, a=a, j=j, kc=kc: e.transpose(
                        ps[:, j * 128:(j + 1) * 128], a[:, j, kc * 128:(kc + 1) * 128], self.ident[:, 0:128]),
                        r=(a,), w=(ps,))
                en = 'act' if kc % 2 == 0 else 'dve'
                if en == 'act':
                    kb.op('act', lambda e, ps=ps, b=b, kc=kc: e.copy(b[:, kc, :], ps[:, :]), r=(ps,), w=(b,))
                else:
                    kb.op('dve', lambda e, ps=ps, b=b, kc=kc: e.tensor_copy(b[:, kc, :], ps[:, :]), r=(ps,), w=(b,))
            kb.dma('pool', xTv[:, :, n * 512:(n + 1) * 512], b[:, :, :], r=(b,))

    def stage_xout(self):
        kb, NT = self.kb, self.NT
        NB = 2
        xi = [kb.sb(f"xoi{i}", [128, KC, 512], F32) for i in range(NB)]
        xo = [kb.sb(f"xoo{i}", [128, 4, D], F32) for i in range(NB)]
        ov = self.out.rearrange("(n j p) d -> n p j d", p=128, j=4)
        xTv = self.xT.rearrange("(kc p) t -> p kc t", p=128)
        for n in range(NT // 512):
            a, b = xi[n % NB], xo[n % NB]
            kb.dma('sp', a[:, :, :], xTv[:, :, n * 512:(n + 1) * 512], w=(a,))
            for j in range(4):
                for h in range(2):
                    ps = self.next_ps()
                    for q in range(4):
                        kc = h * 4 + q
                        kb.op('pe', lambda e, ps=ps, a=a, j=j, kc=kc, q=q: e.transpose(
                            ps[:, q * 128:(q + 1) * 128], a[:, kc, j * 128:(j + 1) * 128], self.ident[:, 0:128]),
                            r=(a,), w=(ps,))
                    if h == 0:
                        kb.op('act', lambda e, ps=ps, b=b, j=j: e.copy(b[:, j, 0:512], ps[:, :]), r=(ps,), w=(b,))
                    else:
                        kb.op('dve', lambda e, ps=ps, b=b, j=j: e.tensor_copy(b[:, j, 512:1024], ps[:, :]), r=(ps,), w=(b,))
            kb.dma('pool', ov[n], b[:, :, :], r=(b,))

    def stage_proj(self, l):
        kb, NT = self.kb, self.NT
        TT = 2048
        NSUB = TT // 512
        xt = [kb.sb(f"pj_x{i}", [128, KC, 512], F32) for i in range(2)]
        sq = kb.sb("pj_sq", [128, KC, 512], F32)
        rstd = kb.sb("pj_rstd", [128, 512], F32)
        hT = kb.sb("pj_h", [128, KC, TT], BF16)
        hsub = [T(hT.h) for _ in range(NSUB)]
        NW = 3
        wt = [kb.sb(f"pj_w{i}", [128, KC, 128], BF16) for i in range(NW)]
        NO = 4
        ot32 = [kb.sb(f"pj_o32_{i}", [128, 512], F32) for i in range(NO)]
        ot16 = [kb.sb(f"pj_o16_{i}", [128, 512], BF16) for i in range(NO)]
        xTv = self.xT.rearrange("(kc p) t -> p kc t", p=128)
        wv = self.wb_in[l].rearrange("(kc p) n -> p kc n", p=128)
        gb0 = self.cidx[('gb', l)]
        oi = 0
        wi = 0
        for st in range(NT // TT):
            t0 = st * TT
            for s in range(NSUB):
                a = xt[s % 2]
                kb.dma('sp', a[:, :, :], xTv[:, :, t0 + s * 512:t0 + (s + 1) * 512], w=(a,))
                hs = hsub[s]
                self._norm_into(a, hT, s * 512, hs, self.cidx[('nmp', l)], sq, rstd)
            for oc in range(IN_COLS // 128):
                w = wt[wi % NW]
                wi += 1
                kb.dma('sp', w[:, :, :], wv[:, :, oc * 128:(oc + 1) * 128], w=(w,))
                c0 = oc * 128
                for s in range(NSUB):
                    ps = self.next_ps()
                    kb.mm(ps, ps[:, :], [(w[:, kc, :], hT[:, kc, s * 512:(s + 1) * 512]) for kc in range(KC)],
                          r=(w, hsub[s]))
                    tsl = slice(t0 + s * 512, t0 + (s + 1) * 512)
                    en = 'act' if oi % 2 == 0 else 'dve'
                    if c0 < DIL0:
                        o = ot32[oi % NO]
                        self._evac(en, o, ps, None)
                        dst = self.P_rw[c0:c0 + 128, tsl]
                    elif c0 < DIF0:
                        o = ot16[oi % NO]
                        cc = c0 - DIL0
                        if cc < 768:
                            self._evac(en, o, ps, 0.125)
                            dst = self.P_dq[cc:cc + 128, tsl]
                        elif cc < 1536:
                            self._evac(en, o, ps, None)
                            dst = self.P_dk[cc - 768:cc - 640, tsl]
                        else:
                            self._evac(en, o, ps, None)
                            dst = self.P_dv[cc - 1536:cc - 1408, tsl]
                    elif c0 < CONV0:
                        o = ot16[oi % NO]
                        cc = c0 - DIF0
                        if cc < 256:
                            self._evac(en, o, ps, 32.0 ** -0.5)
                            dst = self.P_cq[cc:cc + 128, tsl]
                        elif cc < 512:
                            self._evac(en, o, ps, None)
                            dst = self.P_ck[cc - 256:cc - 128, tsl]
                        else:
                            self._evac(en, o, ps, None)
                            dst = self.P_cv[cc - 512:cc - 384, tsl]
                    elif c0 < GATE0:
                        o = ot32[oi % NO]
                        self._evac(en, o, ps, None)
                        dst = self.P_cn[c0 - CONV0:c0 - CONV0 + 128, tsl]
                    else:
                        o = ot16[oi % NO]
                        gc = (c0 - GATE0) // 128
                        kb.op('act', lambda e, o=o, ps=ps, gc=gc: e.activation(
                            out=o[:, :], in_=ps[:, :], func=AF.Sigmoid, bias=self.cols[:, gb0 + gc:gb0 + gc + 1]),
                            r=(ps,), w=(o,))
                        dst = self.P_g[c0 - GATE0:c0 - GATE0 + 128, tsl]
                    kb.dma('pool', dst, o[:, :], r=(o,))
                    oi += 1

    def _evac(self, en, o, ps, scale):
        kb = self.kb
        if en == 'act':
            if scale is None:
                kb.op('act', lambda e: e.copy(o[:, :], ps[:, :]), r=(ps,), w=(o,))
            else:
                kb.op('act', lambda e: e.mul(o[:, :], ps[:, :], scale), r=(ps,), w=(o,))
        else:
            if scale is None:
                kb.op('dve', lambda e: e.tensor_copy(o[:, :], ps[:, :]), r=(ps,), w=(o,))
            else:
                kb.op('dve', lambda e: e.tensor_scalar(o[:, :], ps[:, :], scale, None, ALU.mult), r=(ps,), w=(o,))

    def _norm_into(self, xt, hT, off, htrk, gcol0, sq, rstd, n=512):
        kb = self.kb
        kb.op('act', lambda e: e.activation(out=sq[:, :, 0:n], in_=xt[:, :, 0:n], func=AF.Square), r=(xt,), w=(sq,))
        ps = self.next_ps()
        kb.mm(ps, ps[:, 0:n], [(self.cm[:, 128:256], sq[:, kc, 0:n]) for kc in range(KC)], r=(sq, self.cm))
        kb.op('act', lambda e: e.activation(out=rstd[:, 0:n], in_=ps[:, 0:n], func=AF.Sqrt, bias=self.epsc[:, 0:1], scale=1.0 / D),
              r=(ps,), w=(rstd,))
        kb.op('dve', lambda e: e.reciprocal(rstd[:, 0:n], rstd[:, 0:n]), r=(rstd,), w=(rstd,))
        for kc in range(KC):
            kb.op('dve', lambda e, kc=kc: e.scalar_tensor_tensor(
                hT[:, kc, off:off + n], xt[:, kc, 0:n], self.cols[:, gcol0 + kc:gcol0 + kc + 1], rstd[:, 0:n],
                ALU.mult, ALU.mult), r=(xt, rstd), w=(htrk,))

    def stage_mixers(self, l):
        nc = self.nc
        sb0 = nc.sbuf_base
        for fn in (self.mixer_conv, self.mixer_diff, self.mixer_dil, self.mixer_rwkv):
            nc.sbuf_base = sb0
            fn(l)
            self.kb.barrier()

    def mixer_rwkv(self, l):
        nc, kb = self.nc, self.kb
        sb0 = nc.sbuf_base
        self.rwkv_prelude(l)
        kb.barrier()
        nc.sbuf_base = sb0
        self.rwkv_scan(l)
        kb.barrier()
        nc.sbuf_base = sb0
        self.rwkv_post(l)

    def rwkv_prelude(self, l):
        kb = self.kb
        ci = lambda k: self.cidx[(k, l)]
        col = lambda i: self.cols[:, i:i + 1]
        blk = self.cm[:, 640:768]
        wa = kb.sb("rw_wa", [128, 256], F32)
        gup = kb.sb("rw_gup", [128, 256], F32)
        kb.dma('sp', wa[:, :], self.wa_up[l], w=(wa,))
        kb.dma('sp', gup[:, :], self.g_up[l], w=(gup,))
        pin = [kb.sb(f"rw_pin{i}", [128, 8, 513], F32) for i in range(2)]
        pm = kb.sb("rw_pm", [128, 8, 512], F32)
        tz = kb.sb("rw_tz", [128, 512], F32)
        sg = kb.sb("rw_sg", [128, 512], F32)
        Q = [kb.sb(f"rw_q{i}", [128, 2, 512], F32) for i in range(5)]
        Wq, NKK, Bq, K2, Rq = Q
        aq = kb.sb("rw_a", [128, 2, 512], F32)
        gq = kb.sb("rw_g", [128, 2, 512], F32)
        kkq = kb.sb("rw_kk", [128, 2, 512], F32)
        sq = kb.sb("rw_sq", [128, 2, 512], F32)
        rn = kb.sb("rw_rn", [128, 2, 512], F32)
        tq = kb.sb("rw_t", [128, 2, 512], F32)
        bnv = kb.sb("rw_bnv", [128, 2, 512], F32)
        vq = kb.sb("rw_v", [128, 2, 512], F32)
        rowb = [kb.sb(f"rw_rowb{i}", [128, 5, 256], F32) for i in range(2)]
        Pv = self.P_rw.rearrange("(c p) t -> p c t", p=128)
        fview = lambda dr: dr.rearrange("(c p) t -> p c t", p=128)
        ri = 0
        for si in range(self.NS):
            tb = si * S
            for n in range(S // 512):
                t0 = tb + n * 512
                p_ = pin[n % 2]
                if n == 0:
                    kb.op('dve', lambda e, p_=p_: e.memset(p_[:, :, 0:1], 0.0), w=(p_,))
                    kb.dma('sp', p_[:, :, 1:513], Pv[:, :, t0:t0 + 512], w=(p_,))
                else:
                    kb.dma('sp', p_[:, :, 0:513], Pv[:, :, t0 - 1:t0 + 512], w=(p_,))
                kb.op('pool', lambda e, p_=p_: e.tensor_tensor(pm[:, :, :], p_[:, :, 0:512], p_[:, :, 1:513], ALU.subtract),
                      r=(p_,), w=(pm,))
                for c in range(8):
                    kb.op('dve', lambda e, p_=p_, c=c: e.scalar_tensor_tensor(
                        pm[:, c, :], pm[:, c, :], col(ci('mu') + c), p_[:, c, 1:513], ALU.mult, ALU.add), r=(pm, p_), w=(pm,))
                kb.op('act', lambda e: e.copy(Rq[:, :, :], pm[:, 0:2, :]), r=(pm,), w=(Rq,))
                kb.op('act', lambda e: e.copy(vq[:, :, :], pm[:, 4:6, :]), r=(pm,), w=(vq,))
                kb.op('act', lambda e: e.activation(out=tz[0:64, :], in_=pm[0:64, 6, :], func=AF.Tanh), r=(pm,), w=(tz,))
                for c in range(2):
                    ps = self.next_ps()
                    kb.mm(ps, ps[:, :], [(wa[0:64, c * 128:(c + 1) * 128], tz[0:64, :])], r=(wa, tz))
                    kb.op('act', lambda e, ps=ps, c=c: e.activation(out=Wq[:, c, :], in_=ps[:, :], func=AF.Sigmoid, bias=col(ci('w0') + c)),
                          r=(ps,), w=(Wq,))
                kb.op('act', lambda e: e.activation(out=Wq[:, :, :], in_=Wq[:, :, :], func=AF.Exp, scale=-0.606531), r=(Wq,), w=(Wq,))
                for c in range(2):
                    ps = self.next_ps()
                    kb.mm(ps, ps[:, :], [(wa[64:128, c * 128:(c + 1) * 128], pm[64:128, 6, :])], r=(wa, pm))
                    kb.op('act', lambda e, ps=ps, c=c: e.activation(out=aq[:, c, :], in_=ps[:, :], func=AF.Sigmoid, bias=col(ci('a0') + c)),
                          r=(ps,), w=(aq,))
                kb.op('act', lambda e: e.activation(out=sg[:, :], in_=pm[:, 7, :], func=AF.Sigmoid), r=(pm,), w=(sg,))
                for c in range(2):
                    ps = self.next_ps()
                    kb.mm(ps, ps[:, :], [(gup[:, c * 128:(c + 1) * 128], sg[:, :])], r=(gup, sg))
                    kb.op('act', lambda e, ps=ps, c=c: e.copy(gq[:, c, :], ps[:, :]), r=(ps,), w=(gq,))
                for c in range(2):
                    kb.op('dve', lambda e, c=c: e.tensor_scalar(kkq[:, c, :], pm[:, 2 + c, :], col(ci('kk') + c), None, ALU.mult),
                          r=(pm,), w=(kkq,))
                kb.op('act', lambda e: e.activation(out=sq[:, :, :], in_=kkq[:, :, :], func=AF.Square), r=(kkq,), w=(sq,))
                for c in range(2):
                    ps = self.next_ps()
                    kb.mm(ps, ps[:, :], [(blk, sq[:, c, :])], r=(sq,))
                    kb.op('act', lambda e, ps=ps, c=c: e.activation(out=rn[:, c, :], in_=ps[:, :], func=AF.Sqrt), r=(ps,), w=(rn,))
                kb.op('dve', lambda e: e.tensor_scalar(rn[:, :, :], rn[:, :, :], 1e-12, None, ALU.max), r=(rn,), w=(rn,))
                kb.op('dve', lambda e: e.reciprocal(rn[:, :, :], rn[:, :, :]), r=(rn,), w=(rn,))
                kb.op('dve', lambda e: e.tensor_tensor(kkq[:, :, :], kkq[:, :, :], rn[:, :, :], ALU.mult), r=(kkq, rn), w=(kkq,))
                kb.op('act', lambda e: e.mul(NKK[:, :, :], kkq[:, :, :], -1.0), r=(kkq,), w=(NKK,))
                kb.op('dve', lambda e: e.tensor_tensor(Bq[:, :, :], kkq[:, :, :], aq[:, :, :], ALU.mult), r=(kkq, aq), w=(Bq,))
                for c in range(2):
                    kb.op('dve', lambda e, c=c: e.tensor_scalar(tq[:, c, :], aq[:, c, :], -1.0, col(ci('ka') + c), ALU.add, ALU.mult),
                          r=(aq,), w=(tq,))
                kb.op('dve', lambda e: e.scalar_tensor_tensor(K2[:, :, :], tq[:, :, :], 1.0, pm[:, 2:4, :], ALU.add, ALU.mult),
                      r=(tq, pm), w=(K2,))
                for c in range(2):
                    kb.op('dve', lambda e, c=c: e.scalar_tensor_tensor(tq[:, c, :], Rq[:, c, :], col(ci('rk') + c), K2[:, c, :],
                                                                       ALU.mult, ALU.mult), r=(Rq, K2, tq), w=(tq,))
                for c in range(2):
                    ps = self.next_ps()
                    kb.mm(ps, ps[:, :], [(blk, tq[:, c, :])], r=(tq,))
                    kb.op('dve', lambda e, ps=ps, c=c: e.tensor_tensor(bnv[:, c, :], ps[:, :], vq[:, c, :], ALU.mult),
                          r=(ps, vq), w=(bnv,))
                kb.dma('pool', fview(self.Vf)[:, :, t0:t0 + 512], vq[:, :, :], r=(vq,))
                kb.dma('pool', fview(self.Gf)[:, :, t0:t0 + 512], gq[:, :, :], r=(gq,))
                kb.dma('pool', fview(self.BNVf)[:, :, t0:t0 + 512], bnv[:, :, :], r=(bnv,))
                for j in range(4):
                    rb = rowb[ri % 2]
                    ri += 1
                    for q0 in range(0, 5, 2):
                        ps = self.next_ps()
                        nq = min(2, 5 - q0)
                        for qq in range(nq):
                            for c in range(2):
                                kb.op('pe', lambda e, ps=ps, qq=qq, c=c, q0=q0, j=j: e.transpose(
                                    ps[:, (qq * 2 + c) * 128:(qq * 2 + c + 1) * 128], Q[q0 + qq][:, c, j * 128:(j + 1) * 128],
                                    self.ident[:, 0:128]), r=(Q[q0 + qq],), w=(ps,))
                        kb.op('act' if q0 != 2 else 'dve',
                              (lambda e, ps=ps, rb=rb, q0=q0, nq=nq: e.copy(
                                  rb[:, q0:q0 + nq, :], ps[:, 0:nq * 256].rearrange("p (q f) -> p q f", f=256))) if q0 != 2 else
                              (lambda e, ps=ps, rb=rb, q0=q0, nq=nq: e.tensor_copy(
                                  rb[:, q0:q0 + nq, :], ps[:, 0:nq * 256].rearrange("p (q f) -> p q f", f=256))),
                              r=(ps,), w=(rb,))
                    tl = n * 512 + j * 128
                    kb.dma('pool', self.ROWS[tl:tl + 128, :, si, :], rb[:, :, :], r=(rb,))

    def rwkv_scan(self, l):
        kb = self.kb
        NS = self.NS
        NH = 4 * NS
        W_ = NH * 64
        St = kb.sb("rs_S", [64, W_], F32)
        tmp = [kb.sb(f"rs_tmp{i}", [64, W_], F32) for i in range(2)]
        tmp2 = [kb.sb(f"rs_tp{i}", [64, W_], F32) for i in range(2)]
        sa = [kb.sb(f"rs_sa{i}", [64, NH], F32) for i in range(2)]
        NBB = 4
        bt = [kb.sb(f"rs_bt{i}", [64, NBB, 5, 2, 256], F32) for i in range(2)]
        VB = 64
        vv = [kb.sb(f"rs_vv{i}", [64, NH, VB], F32) for i in range(2)]
        oo = [kb.sb(f"rs_oo{i}", [64, NH, VB], F32) for i in range(2)]
        kb.op('dve', lambda e: e.memset(St[:, :], 0.0), w=(St,))
        v3 = lambda ap: ap.rearrange("p (h k) -> p h k", k=64)
        hview = lambda dr: dr.rearrange("(h p) t -> p h t", p=64)
        for blk in range(S // VB):
            tB = blk * VB
            V_, O_ = vv[blk % 2], oo[blk % 2]
            for si in range(NS):
                kb.dma('sp', V_[:, si * 4:(si + 1) * 4, :], hview(self.Vf)[:, :, si * S + tB:si * S + tB + VB], w=(V_,))
            for sb_ in range(VB // NBB):
                t0 = tB + sb_ * NBB
                B_ = bt[sb_ % 2]
                kb.dma('sp', B_[:, :, :, 0:NS, :], self.ROWS[t0:t0 + NBB, :, 0:NS, :].partition_broadcast(64), w=(B_,))
                for j in range(NBB):
                    tt = sb_ * NBB + j
                    X = lambda q: B_[:, j, q, 0:NS, :]
                    X2 = lambda q: B_[:, j, q, 0:NS, :].rearrange("p s (h k) -> p (s h) k", k=64)
                    tm, t2, s_ = tmp[tt % 2], tmp2[tt % 2], sa[tt % 2]
                    S3 = St[:, :].rearrange("p (s f) -> p s f", s=NS)
                    tm3 = tm[:, :].rearrange("p (s f) -> p s f", s=NS)
                    kb.op('pool', lambda e, t2=t2, X2=X2, V_=V_, tt=tt: e.tensor_tensor(
                        v3(t2[:, :]), X2(3), V_[:, :, tt:tt + 1].to_broadcast([64, NH, 64]), ALU.mult), r=(B_, V_), w=(t2,))
                    kb.op('dve', lambda e, tm3=tm3, S3=S3, X=X: e.tensor_tensor(tm3, S3, X(1), ALU.mult), r=(St, B_), w=(tm,))
                    kb.op('dve', lambda e, tm=tm, s_=s_: e.tensor_reduce(s_[:, :], v3(tm[:, :]), AX.X, ALU.add), r=(tm,), w=(s_,))
                    kb.op('dve', lambda e, S3=S3, X=X: e.tensor_tensor(S3, S3, X(0), ALU.mult), r=(St, B_), w=(St,))
                    kb.op('dve', lambda e, tm=tm, X2=X2, s_=s_: e.tensor_tensor(
                        v3(tm[:, :]), X2(2), s_[:, :].unsqueeze(2).to_broadcast([64, NH, 64]), ALU.mult), r=(B_, s_), w=(tm,))
                    kb.op('dve', lambda e, tm=tm: e.tensor_tensor(St[:, :], St[:, :], tm[:, :], ALU.add), r=(St, tm), w=(St,))
                    kb.op('dve', lambda e, t2=t2: e.tensor_tensor(St[:, :], St[:, :], t2[:, :], ALU.add), r=(St, t2), w=(St,))
                    kb.op('dve', lambda e, tm3=tm3, S3=S3, X=X: e.tensor_tensor(tm3, S3, X(4), ALU.mult), r=(St, B_), w=(tm,))
                    kb.op('dve', lambda e, tm=tm, O_=O_, tt=tt: e.tensor_reduce(O_[:, :, tt], v3(tm[:, :]), AX.X, ALU.add),
                          r=(tm,), w=(O_,))
            for si in range(NS):
                kb.dma('pool', hview(self.Of)[:, :, si * S + tB:si * S + tB + VB], O_[:, si * 4:(si + 1) * 4, :], r=(O_,))

    def rwkv_post(self, l):
        kb = self.kb
        ci = lambda k: self.cidx[(k, l)]
        col = lambda i: self.cols[:, i:i + 1]
        blk = self.cm[:, 640:768]
        ot = [kb.sb(f"rp_o{i}", [128, 512], F32) for i in range(2)]
        bt_ = [kb.sb(f"rp_b{i}", [128, 512], F32) for i in range(2)]
        gt = [kb.sb(f"rp_g{i}", [128, 512], F32) for i in range(2)]
        sq = kb.sb("rp_sq", [128, 512], F32)
        mean = kb.sb("rp_mean", [128, 512], F32)
        msq = kb.sb("rp_msq", [128, 512], F32)
        rstd = kb.sb("rp_rstd", [128, 512], F32)
        t1 = kb.sb("rp_t1", [128, 512], F32)
        ob = [kb.sb(f"rp_ob{i}", [128, 512], BF16) for i in range(2)]
        i = 0
        for c in range(2):
            for n in range(self.NT // 512):
                ts_ = slice(n * 512, (n + 1) * 512)
                o, b_, g_ = ot[i % 2], bt_[i % 2], gt[i % 2]
                rows = slice(c * 128, (c + 1) * 128)
                kb.dma('sp', o[:, :], self.Of[rows, ts_], w=(o,))
                kb.dma('sp', b_[:, :], self.BNVf[rows, ts_], w=(b_,))
                kb.dma('sp', g_[:, :], self.Gf[rows, ts_], w=(g_,))
                ps1 = self.next_ps()
                kb.mm(ps1, ps1[:, :], [(blk, o[:, :])], r=(o,))
                kb.op('act', lambda e, o=o: e.activation(out=sq[:, :], in_=o[:, :], func=AF.Square), r=(o,), w=(sq,))
                ps2 = self.next_ps()
                kb.mm(ps2, ps2[:, :], [(blk, sq[:, :])], r=(sq,))
                kb.op('dve', lambda e, ps1=ps1: e.tensor_scalar(mean[:, :], ps1[:, :], 1.0 / 64, None, ALU.mult), r=(ps1,), w=(mean,))
                kb.op('dve', lambda e: e.tensor_tensor(msq[:, :], mean[:, :], mean[:, :], ALU.mult), r=(mean,), w=(msq,))
                kb.op('dve', lambda e, ps2=ps2: e.scalar_tensor_tensor(rstd[:, :], ps2[:, :], 1.0 / 64, msq[:, :], ALU.mult, ALU.subtract),
                      r=(ps2, msq), w=(rstd,))
                kb.op('act', lambda e: e.activation(out=rstd[:, :], in_=rstd[:, :], func=AF.Sqrt, bias=self.epsc[:, 2:3], scale=1.0),
                      r=(rstd,), w=(rstd,))
                kb.op('dve', lambda e: e.reciprocal(rstd[:, :], rstd[:, :]), r=(rstd,), w=(rstd,))
                kb.op('dve', lambda e, o=o: e.tensor_tensor(t1[:, :], o[:, :], mean[:, :], ALU.subtract), r=(o, mean), w=(t1,))
                kb.op('dve', lambda e: e.tensor_tensor(t1[:, :], t1[:, :], rstd[:, :], ALU.mult), r=(t1, rstd), w=(t1,))
                kb.op('dve', lambda e, c=c: e.tensor_scalar(t1[:, :], t1[:, :], col(ci('lng') + c), col(ci('lnb') + c), ALU.mult, ALU.add),
                      r=(t1,), w=(t1,))
                kb.op('dve', lambda e, b_=b_: e.tensor_tensor(t1[:, :], t1[:, :], b_[:, :], ALU.add), r=(t1, b_), w=(t1,))
                ob_ = ob[i % 2]
                kb.op('dve', lambda e, g_=g_, ob_=ob_: e.tensor_tensor(ob_[:, :], t1[:, :], g_[:, :], ALU.mult), r=(t1, g_), w=(ob_,))
                kb.dma('pool', self.Y[rows, ts_], ob_[:, :], r=(ob_,))
                i += 1

    def mixer_conv(self, l):
        kb = self.kb
        cw, cb, cg, cbb = (self.cidx[(k, l)] for k in ('cw', 'cb', 'cg', 'cbb'))
        col = lambda i: self.cols[:, i:i + 1]
        a_t = kb.sb("cv_a", [128, S], F32)
        b_t = kb.sb("cv_b", [128, S], F32)
        u = kb.sb("cv_u", [128, S + 32], F32)
        yc = [kb.sb(f"cv_y{i}", [128, S], F32) for i in range(2)]
        sq = kb.sb("cv_sq", [128, 2, 512], F32)
        mean = kb.sb("cv_mean", [128, 512], F32)
        msq = kb.sb("cv_msq", [128, 512], F32)
        rstd = kb.sb("cv_rstd", [128, 512], F32)
        t1 = [kb.sb(f"cv_t{i}", [128, 512], F32) for i in range(2)]
        ob = [kb.sb(f"cv_o{i}", [128, 512], BF16) for i in range(2)]
        kb.op('dve', lambda e: e.memset(u[:, 0:32], 0.0), w=(u,))
        for sq_i in range(self.NS):
            tb = sq_i * S
            for c in range(2):
                kb.dma('sp', a_t[:, :], self.P_cn[c * 128:(c + 1) * 128, tb:tb + S], w=(a_t,))
                kb.dma('sp', b_t[:, :], self.P_cn[256 + c * 128:256 + (c + 1) * 128, tb:tb + S], w=(b_t,))
                kb.op('act', lambda e: e.activation(out=b_t[:, :], in_=b_t[:, :], func=AF.Sigmoid), r=(b_t,), w=(b_t,))
                kb.op('dve', lambda e: e.tensor_tensor(u[:, 30:30 + S], a_t[:, :], b_t[:, :], ALU.mult), r=(a_t, b_t), w=(u,))
                y = yc[c]
                kb.op('dve', lambda e, y=y, c=c: e.tensor_scalar(y[:, :], u[:, 0:S], col(cw + c), col(cb + c), ALU.mult, ALU.add),
                      r=(u,), w=(y,))
                for j in range(1, 31):
                    kb.op('dve', lambda e, y=y, c=c, j=j: e.scalar_tensor_tensor(
                        y[:, :], u[:, j:j + S], col(cw + j * 2 + c), y[:, :], ALU.mult, ALU.add), r=(u, y), w=(y,))
            for n in range(S // 512):
                ts_ = slice(n * 512, (n + 1) * 512)
                ps1 = self.next_ps()
                kb.mm(ps1, ps1[:, :], [(self.cm[:, 128:256], yc[c][:, ts_]) for c in range(2)], r=(yc[0], yc[1]))
                kb.op('act', lambda e: e.activation(out=sq[:, 0, :], in_=yc[0][:, ts_], func=AF.Square), r=(yc[0],), w=(sq,))
                kb.op('act', lambda e: e.activation(out=sq[:, 1, :], in_=yc[1][:, ts_], func=AF.Square), r=(yc[1],), w=(sq,))
                ps2 = self.next_ps()
                kb.mm(ps2, ps2[:, :], [(self.cm[:, 128:256], sq[:, c, :]) for c in range(2)], r=(sq,))
                kb.op('dve', lambda e: e.tensor_scalar(mean[:, :], ps1[:, :], 1.0 / 256, None, ALU.mult), r=(ps1,), w=(mean,))
                kb.op('dve', lambda e: e.tensor_tensor(msq[:, :], mean[:, :], mean[:, :], ALU.mult), r=(mean,), w=(msq,))
                kb.op('dve', lambda e: e.scalar_tensor_tensor(rstd[:, :], ps2[:, :], 1.0 / 256, msq[:, :], ALU.mult, ALU.subtract),
                      r=(ps2, msq), w=(rstd,))
                kb.op('act', lambda e: e.activation(out=rstd[:, :], in_=rstd[:, :], func=AF.Sqrt, bias=self.epsc[:, 1:2], scale=1.0),
                      r=(rstd,), w=(rstd,))
                kb.op('dve', lambda e: e.reciprocal(rstd[:, :], rstd[:, :]), r=(rstd,), w=(rstd,))
                for c in range(2):
                    t, o = t1[c], ob[c]
                    kb.op('dve', lambda e, t=t, c=c: e.tensor_tensor(t[:, :], yc[c][:, ts_], mean[:, :], ALU.subtract),
                          r=(yc[c], mean), w=(t,))
                    kb.op('dve', lambda e, t=t, c=c: e.scalar_tensor_tensor(t[:, :], t[:, :], col(cg + c), rstd[:, :], ALU.mult, ALU.mult),
                          r=(t, rstd), w=(t,))
                    kb.op('act', lambda e, t=t, o=o, c=c: e.activation(out=o[:, :], in_=t[:, :], func=AF.Silu, bias=col(cbb + c)),
                          r=(t,), w=(o,))
                    kb.dma('pool', self.Y[768 + c * 128:768 + (c + 1) * 128, tb + n * 512:tb + (n + 1) * 512], o[:, :], r=(o,))

    def _load_qk(self, dst, src_rows, xtab_rows, nd, nx, stage, tb):
        kb = self.kb
        kb.dma('sp', dst[0:nd, :], src_rows[:, tb:tb + S], w=(dst,))
        kb.dma('sp', stage[nd:nd + nx, :], xtab_rows, w=(stage,))
        kb.op('dve', lambda e: e.tensor_copy(dst[nd:nd + nx, :], stage[nd:nd + nx, :]), r=(stage,), w=(dst,))

    def _make_vp(self, Vp, vin, blocks):
        kb = self.kb
        nb = len(blocks)
        for b0 in range(0, nb, 16):
            for j in range(16):
                kb.op('pe', lambda e, j=j, sl=blocks[b0 + j]: e.transpose(
                    self.psT[:, j * 64:(j + 1) * 64], vin[0:64, sl], self.identb[0:64, 0:64]), r=(vin,), w=(self.psT,))
            kb.op('dve', lambda e, b0=b0: e.tensor_copy(
                Vp[:, b0:b0 + 16, 0:64], self.psT[:, :].rearrange("p (b d) -> p b d", d=64)), r=(self.psT,), w=(Vp,))

    def mixer_diff(self, l):
        import math
        kb = self.kb
        slg = self.cidx[('slg', l)]
        Qp = [kb.sb(f"df_q{m}", [36, S], BF16) for m in range(2)]
        Kp = [kb.sb(f"df_k{m}", [36, S], BF16) for m in range(2)]
        stg = kb.sb("df_stg", [36, S], F32)
        vin = kb.sb("df_vin", [64, S], BF16)
        Vp = kb.sb("df_vp", [128, 32, 65], BF16)
        pt = [kb.sb(f"df_pt{i}", [128, 512], BF16) for i in range(3)]
        osb = [kb.sb(f"df_o{m}", [65, 512], F32) for m in range(2)]
        rb = [kb.sb(f"df_rb{m}", [64, 512], F32) for m in range(2)]
        yy = [kb.sb(f"df_y{m}", [64, 512], F32) for m in range(2)]
        att = kb.sb("df_att", [64, 512], F32)
        sq = kb.sb("df_sq", [64, 512], F32)
        rstd = kb.sb("df_rstd", [64, 512], F32)
        ob = [kb.sb(f"df_ob{i}", [64, 512], BF16) for i in range(2)]
        lamt = kb.sb("df_lamt", [128, 128], F32)
        lsm = kb.sb("df_lsm", [128, 8], F32)
        kb.op('dve', lambda e: e.memset(Vp[:, :, 64:65], 1.0), w=(Vp,))
        kb.dma('sp', lamt[:, :], self.lamv[l], w=(lamt,))
        kb.op('dve', lambda e: e.tensor_tensor(lamt[:, 0:32], lamt[:, 0:32], lamt[:, 32:64], ALU.mult), r=(lamt,), w=(lamt,))
        kb.op('dve', lambda e: e.tensor_tensor(lamt[:, 64:96], lamt[:, 64:96], lamt[:, 96:128], ALU.mult), r=(lamt,), w=(lamt,))
        kb.op('dve', lambda e: e.tensor_reduce(lsm[:, 0:1], lamt[:, 0:32], AX.X, ALU.add), r=(lamt,), w=(lsm,))
        kb.op('dve', lambda e: e.tensor_reduce(lsm[:, 1:2], lamt[:, 64:96], AX.X, ALU.add), r=(lamt,), w=(lsm,))
        kb.op('act', lambda e: e.activation(out=lsm[:, 2:4], in_=lsm[:, 0:2], func=AF.Exp), r=(lsm,), w=(lsm,))
        kb.op('dve', lambda e: e.tensor_tensor(lsm[:, 4:5], lsm[:, 3:4], lsm[:, 2:3], ALU.subtract), r=(lsm,), w=(lsm,))
        lmi, oml = self.cidx[('lmi', l)], self.cidx[('oml', l)]
        kb.op('dve', lambda e: e.tensor_tensor(lsm[:, 5:6], lsm[:, 4:5], self.cols[:, lmi:lmi + 1], ALU.add), r=(lsm,), w=(lsm,))
        kb.op('dve', lambda e: e.tensor_tensor(lsm[:, 6:7], self.cols[:, slg:slg + 1], self.cols[:, oml:oml + 1], ALU.mult),
              r=(lsm,), w=(lsm,))
        pi = 0
        oi = 0
        for si in range(self.NS):
            tb = si * S
            for h in range(4):
                for m in range(2):
                    r0 = (h * 2 + m) * 32
                    self._load_qk(Qp[m], self.P_cq[r0:r0 + 32], self.dxq[h * 4:(h + 1) * 4, :], 32, 4, stg, tb)
                    self._load_qk(Kp[m], self.P_ck[r0:r0 + 32], self.dxk[h * 4:(h + 1) * 4, :], 32, 4, stg, tb)
                kb.dma('sp', vin[:, :], self.P_cv[h * 64:(h + 1) * 64, tb:tb + S], w=(vin,))
                self._make_vp(Vp, vin, [slice(b * 128, (b + 1) * 128) for b in range(32)])
                for qt in range(S // 512):
                    q0 = qt * 512
                    nkb = 4 * qt + 4
                    for m in range(2):
                        acc = self.pacc[m]
                        for kbi in range(nkb):
                            md = kbi - 4 * qt
                            c0 = 128 * md if md > 0 else 0
                            ps = self.next_ps()
                            kb.mm(ps, ps[:, c0:512], [(Kp[m][:, kbi * 128:(kbi + 1) * 128], Qp[m][:, q0 + c0:q0 + 512])],
                                  r=(Kp[m], Qp[m]))
                            P = pt[pi % 3]
                            pi += 1
                            kb.op('act', lambda e, P=P, ps=ps, c0=c0: e.activation(out=P[:, c0:512], in_=ps[:, c0:512], func=AF.Exp),
                                  r=(ps,), w=(P,))
                            if md >= 0:
                                kb.op('dve', lambda e, P=P, c0=c0: e.tensor_tensor(
                                    P[:, c0:c0 + 128], P[:, c0:c0 + 128], self.mUL[:, 0:128], ALU.mult), r=(P,), w=(P,))
                            kb.mm1(acc, acc[0:65, c0:512], Vp[:, kbi, :], P[:, c0:512], start=(kbi == 0), stop=(kbi == nkb - 1),
                                   r=(Vp, P))
                        kb.op('act', lambda e, m=m, acc=acc: e.copy(osb[m][:, :], acc[0:65, :]), r=(acc,), w=(osb[m],))
                    for m in range(2):
                        bc = self.next_ps()
                        kb.mm(bc, bc[0:64, :], [(self.cm[0:65, 512:576], osb[m][0:65, :])], r=(osb[m],))
                        kb.op('dve', lambda e, m=m, bc=bc: e.reciprocal(rb[m][:, :], bc[0:64, :]), r=(bc,), w=(rb[m],))
                        kb.op('dve', lambda e, m=m: e.tensor_tensor(yy[m][:, :], osb[m][0:64, :], rb[m][:, :], ALU.mult),
                              r=(osb[m], rb[m]), w=(yy[m],))
                    kb.op('dve', lambda e: e.scalar_tensor_tensor(att[:, :], yy[1][:, :], lsm[0:64, 5:6], yy[0][:, :], ALU.mult, ALU.add),
                          r=(yy[0], yy[1], lsm), w=(att,))
                    kb.op('act', lambda e: e.activation(out=sq[:, :], in_=att[:, :], func=AF.Square), r=(att,), w=(sq,))
                    ss = self.next_ps()
                    kb.mm(ss, ss[0:64, :], [(self.cm[0:64, 128:192], sq[:, :])], r=(sq,))
                    kb.op('act', lambda e, ss=ss: e.activation(out=rstd[:, :], in_=ss[0:64, :], func=AF.Sqrt, bias=self.epsc[0:64, 1:2],
                                                               scale=1.0 / 64), r=(ss,), w=(rstd,))
                    kb.op('dve', lambda e: e.reciprocal(rstd[:, :], rstd[:, :]), r=(rstd,), w=(rstd,))
                    o = ob[oi % 2]
                    oi += 1
                    kb.op('dve', lambda e, o=o: e.scalar_tensor_tensor(o[:, :], att[:, :], lsm[0:64, 6:7], rstd[:, :], ALU.mult, ALU.mult),
                          r=(att, rstd, lsm), w=(o,))
                    kb.dma('pool', self.Y[512 + h * 64:512 + (h + 1) * 64, tb + q0:tb + q0 + 512], o[:, :], r=(o,))

    def mixer_dil(self, l):
        kb = self.kb
        Qp = kb.sb("dl_q", [72, S], BF16)
        Kp = kb.sb("dl_k", [72, S], BF16)
        stg = kb.sb("dl_stg", [72, S], F32)
        vin = kb.sb("dl_vin", [64, S], BF16)
        Vp = kb.sb("dl_vp", [128, 32, 65], BF16)
        Acc = kb.sb("dl_acc", [65, S], F32)
        pt = [kb.sb(f"dl_pt{i}", [128, 256], BF16) for i in range(3)]
        rb = kb.sb("dl_rb", [64, 512], F32)
        ob = [kb.sb(f"dl_ob{i}", [64, 512], BF16) for i in range(2)]
        kb.op('dve', lambda e: e.memset(Vp[:, :, 64:65], 1.0), w=(Vp,))
        pi = 0
        oi = 0
        for si in range(self.NS):
            tb = si * S
            for h in range(4):
                for g, (win, d) in enumerate(DIL_PAT):
                    gh = g * 4 + h
                    nb = S // d // 128
                    self._load_qk(Qp, self.P_dq[gh * 64:(gh + 1) * 64], self.dlq[gh * 8:(gh + 1) * 8, :], 64, 8, stg, tb)
                    self._load_qk(Kp, self.P_dk[gh * 64:(gh + 1) * 64], self.dlk[gh * 8:(gh + 1) * 8, :], 64, 8, stg, tb)
                    kb.dma('sp', vin[:, :], self.P_dv[gh * 64:(gh + 1) * 64, tb:tb + S], w=(vin,))
                    blk = lambda r, c: slice(r + d * 128 * c, r + d * 128 * c + d * 127 + 1, d)
                    blocks = [blk(r, c) for r in range(d) for c in range(nb)]
                    self._make_vp(Vp, vin, blocks)
                    for r in range(d):
                        for c in range(nb):
                            bi = r * nb + c
                            sc = blocks[bi]
                            ps = self.next_ps()
                            kb.mm(ps, ps[:, 0:128], [(Kp[:, sc], Qp[:, sc])], r=(Kp, Qp))
                            w_ = 128
                            if c > 0:
                                kb.mm(ps, ps[:, 128:256], [(Kp[:, blocks[bi - 1]], Qp[:, sc])], r=(Kp, Qp))
                                w_ = 256
                            P = pt[pi % 3]
                            pi += 1
                            kb.op('act', lambda e, P=P, ps=ps, w_=w_: e.activation(out=P[:, 0:w_], in_=ps[:, 0:w_], func=AF.Exp),
                                  r=(ps,), w=(P,))
                            kb.op('dve', lambda e, P=P, w_=w_: e.tensor_tensor(P[:, 0:w_], P[:, 0:w_], self.mUL[:, 0:w_], ALU.mult),
                                  r=(P,), w=(P,))
                            po = self.next_ps()
                            pairs = [(Vp[:, bi, :], P[:, 0:128])]
                            if c > 0:
                                pairs.append((Vp[:, bi - 1, :], P[:, 128:256]))
                            kb.mm(po, po[0:65, 0:128], pairs, r=(Vp, P))
                            if g == 0:
                                kb.op('act', lambda e, po=po, sc=sc: e.copy(Acc[:, sc], po[0:65, 0:128]), r=(po,), w=(Acc,))
                            else:
                                kb.op('dve', lambda e, po=po, sc=sc: e.tensor_tensor(Acc[:, sc], Acc[:, sc], po[0:65, 0:128], ALU.add),
                                      r=(po, Acc), w=(Acc,))
                for n in range(S // 512):
                    ts_ = slice(n * 512, (n + 1) * 512)
                    bc = self.next_ps()
                    kb.mm(bc, bc[0:64, :], [(self.cm[0:65, 512:576], Acc[0:65, ts_])], r=(Acc,))
                    kb.op('dve', lambda e, bc=bc: e.reciprocal(rb[:, :], bc[0:64, :]), r=(bc,), w=(rb,))
                    o = ob[oi % 2]
                    oi += 1
                    kb.op('dve', lambda e, o=o, ts_=ts_: e.tensor_tensor(o[:, :], Acc[0:64, ts_], rb[:, :], ALU.mult), r=(Acc, rb), w=(o,))
                    kb.dma('pool', self.Y[256 + h * 64:256 + (h + 1) * 64, tb + n * 512:tb + (n + 1) * 512], o[:, :], r=(o,))

    def _post_norm_add(self, xt, y, gcol0, sq, rstd, tmp):
        kb = self.kb
        kb.op('act', lambda e: e.activation(out=sq[:, :, :], in_=y[:, :, :], func=AF.Square), r=(y,), w=(sq,))
        ps = self.next_ps()
        kb.mm(ps, ps[:, :], [(self.cm[:, 128:256], sq[:, kc, :]) for kc in range(KC)], r=(sq, self.cm))
        kb.op('act', lambda e: e.activation(out=rstd[:, :], in_=ps[:, :], func=AF.Sqrt, bias=self.epsc[:, 0:1], scale=1.0 / D),
              r=(ps,), w=(rstd,))
        kb.op('dve', lambda e: e.reciprocal(rstd[:, :], rstd[:, :]), r=(rstd,), w=(rstd,))
        for kc in range(KC):
            kb.op('dve', lambda e, kc=kc: e.scalar_tensor_tensor(
                tmp[:, kc, :], y[:, kc, :], self.cols[:, gcol0 + kc:gcol0 + kc + 1], rstd[:, :],
                ALU.mult, ALU.mult), r=(y, rstd), w=(tmp,))
        kb.op('pool', lambda e: e.tensor_tensor(xt[:, :, :], xt[:, :, :], tmp[:, :, :], ALU.add), r=(tmp, xt), w=(xt,))

    def stage_merge(self, l):
        kb, NT = self.kb, self.NT
        wbr = kb.sb("mg_wbr", [128, 8, D], BF16)
        wo = kb.sb("mg_wo", [128, KC, D], BF16)
        kb.dma('sp', wbr[:, :, :], self.wb_br[l].rearrange("(c p) d -> p c d", p=128), w=(wbr,))
        kb.dma('sp', wo[:, :, :], self.wb_out[l].rearrange("(c p) d -> p c d", p=128), w=(wo,))
        yt = [kb.sb(f"mg_y{i}", [128, 8, 512], BF16) for i in range(2)]
        gt = [kb.sb(f"mg_g{i}", [128, 32, 512], BF16) for i in range(2)]
        xt = [kb.sb(f"mg_x{i}", [128, KC, 512], F32) for i in range(2)]
        acc = [kb.sb(f"mg_acc{i}", [128, 512], F32) for i in range(2)]
        tm = [kb.sb(f"mg_tm{i}", [128, 512], F32) for i in range(2)]
        mT = kb.sb("mg_m", [128, KC, 512], BF16)
        yo = kb.sb("mg_yo", [128, KC, 512], F32)
        sq = kb.sb("mg_sq", [128, KC, 512], F32)
        rstd = kb.sb("mg_rstd", [128, 512], F32)
        Yv = self.Y.rearrange("(c p) t -> p c t", p=128)
        Gv = self.P_g.rearrange("(c p) t -> p c t", p=128)
        xTv = self.xT.rearrange("(kc p) t -> p kc t", p=128)
        g0 = self.cidx[('nmo', l)]
        for n in range(NT // 512):
            ts_ = slice(n * 512, (n + 1) * 512)
            y, g, x = yt[n % 2], gt[n % 2], xt[n % 2]
            kb.dma('sp', y[:, :, :], Yv[:, :, ts_], w=(y,))
            for gq_ in range(4):
                kb.dma('sp', g[:, gq_ * 8:(gq_ + 1) * 8, :], Gv[:, gq_ * 8:(gq_ + 1) * 8, ts_], w=(g,))
            kb.dma('sp', x[:, :, :], xTv[:, :, ts_], w=(x,))
            for dc in range(KC):
                a = acc[dc % 2]
                for br in range(4):
                    ps = self.next_ps()
                    kb.mm(ps, ps[:, :], [(wbr[:, br * 2 + c, dc * 128:(dc + 1) * 128], y[:, br * 2 + c, :]) for c in range(2)],
                          r=(wbr, y))
                    if br == 0:
                        kb.op('dve', lambda e, a=a, ps=ps, g=g, br=br, dc=dc: e.tensor_tensor(
                            a[:, :], ps[:, :], g[:, br * 8 + dc, :], ALU.mult), r=(ps, g), w=(a,))
                    else:
                        t = tm[br % 2]
                        kb.op('dve', lambda e, t=t, ps=ps, g=g, br=br, dc=dc: e.tensor_tensor(
                            t[:, :], ps[:, :], g[:, br * 8 + dc, :], ALU.mult), r=(ps, g), w=(t,))
                        if br < 3:
                            kb.op('pool', lambda e, a=a, t=t: e.tensor_tensor(a[:, :], a[:, :], t[:, :], ALU.add), r=(t, a), w=(a,))
                        else:
                            kb.op('pool', lambda e, a=a, t=t, dc=dc: e.tensor_tensor(mT[:, dc, :], a[:, :], t[:, :], ALU.add),
                                  r=(t, a), w=(mT,))
            for oc in range(KC):
                ps = self.next_ps()
                kb.mm(ps, ps[:, :], [(wo[:, kc, oc * 128:(oc + 1) * 128], mT[:, kc, :]) for kc in range(KC)], r=(wo, mT))
                kb.op('act', lambda e, ps=ps, oc=oc: e.copy(yo[:, oc, :], ps[:, :]), r=(ps,), w=(yo,))
            self._post_norm_add(x, yo, g0, sq, rstd, sq)
            kb.dma('pool', xTv[:, :, ts_], x[:, :, :], r=(x,))

    def stage_ffn(self, l):
        kb, NT = self.kb, self.NT
        xt = [kb.sb(f"ff_x{i}", [128, KC, 512], F32) for i in range(2)]
        hT = kb.sb("ff_h", [128, KC, 512], BF16)
        aT = kb.sb("ff_a", [128, FC, 512], BF16)
        sq = kb.sb("ff_sq", [128, KC, 512], F32)
        yo = kb.sb("ff_yo", [128, KC, 512], F32)
        rstd = kb.sb("ff_rstd", [128, 512], F32)
        sg = [kb.sb(f"ff_sg{i}", [128, 512], F32) for i in range(2)]
        NW = 3
        wg = [kb.sb(f"ff_wg{i}", [128, KC, 256], BF16) for i in range(NW)]
        wu = [kb.sb(f"ff_wu{i}", [128, KC, 256], BF16) for i in range(NW)]
        wd = [kb.sb(f"ff_wd{i}", [128, FC, 256], BF16) for i in range(2)]
        xTv = self.xT.rearrange("(kc p) t -> p kc t", p=128)
        wgv = self.wb_g[l].rearrange("(kc p) n -> p kc n", p=128)
        wuv = self.wb_u[l].rearrange("(kc p) n -> p kc n", p=128)
        wdv = self.wb_d[l].rearrange("(kc p) n -> p kc n", p=128)
        gpre, gpost = self.cidx[('nfp', l)], self.cidx[('nfo', l)]
        wi = 0
        di = 0
        for n in range(NT // 512):
            ts_ = slice(n * 512, (n + 1) * 512)
            x = xt[n % 2]
            kb.dma('sp', x[:, :, :], xTv[:, :, ts_], w=(x,))
            self._norm_into(x, hT, 0, hT, gpre, sq, rstd)
            for f2 in range(FC // 2):
                g_, u_ = wg[wi % NW], wu[wi % NW]
                wi += 1
                kb.dma('sp', g_[:, :, :], wgv[:, :, f2 * 256:(f2 + 1) * 256], w=(g_,))
                kb.dma('sp', u_[:, :, :], wuv[:, :, f2 * 256:(f2 + 1) * 256], w=(u_,))
                for j in range(2):
                    fc = f2 * 2 + j
                    pg = self.next_ps()
                    kb.mm(pg, pg[:, :], [(g_[:, kc, j * 128:(j + 1) * 128], hT[:, kc, :]) for kc in range(KC)], r=(g_, hT))
                    pu = self.next_ps()
                    kb.mm(pu, pu[:, :], [(u_[:, kc, j * 128:(j + 1) * 128], hT[:, kc, :]) for kc in range(KC)], r=(u_, hT))
                    sgt = sg[fc % 2]
                    kb.op('act', lambda e, sgt=sgt, pg=pg: e.activation(out=sgt[:, :], in_=pg[:, :], func=AF.Silu), r=(pg,), w=(sgt,))
                    kb.op('dve', lambda e, sgt=sgt, pu=pu, fc=fc: e.tensor_tensor(aT[:, fc, :], sgt[:, :], pu[:, :], ALU.mult),
                          r=(sgt, pu), w=(aT,))
            for o2 in range(KC // 2):
                d_ = wd[di % 2]
                di += 1
                for f0, f1 in ((0, 8), (8, 15), (15, 22)):
                    kb.dma('sp', d_[:, f0:f1, :], wdv[:, f0:f1, o2 * 256:(o2 + 1) * 256], w=(d_,))
                for j in range(2):
                    oc = o2 * 2 + j
                    ps = self.next_ps()
                    kb.mm(ps, ps[:, :], [(d_[:, fc, j * 128:(j + 1) * 128], aT[:, fc, :]) for fc in range(FC)], r=(d_, aT))
                    kb.op('act', lambda e, ps=ps, oc=oc: e.copy(yo[:, oc, :], ps[:, :]), r=(ps,), w=(yo,))
            self._post_norm_add(x, yo, gpost, sq, rstd, sq)
            kb.dma('pool', xTv[:, :, ts_], x[:, :, :], r=(x,))


DIL_PAT = ((128, 1), (512, 4), (2048, 16))


def _bf16_round(v):
    import ml_dtypes
    return np.asarray(v, np.float32).astype(ml_dtypes.bfloat16).astype(np.float32)


def pos_tables():
    pos = np.arange(S)
    hi_, lo_ = (pos // 128 * 128).astype(np.float32), (pos % 128).astype(np.float32)
    one = np.ones(S, np.float32)
    dsl = 2.0 ** (-8.0 * np.arange(1, 5) / 4)
    dxq = np.zeros((16, S), np.float32)
    dxk = np.zeros((16, S), np.float32)
    for h in range(4):
        sl = np.float32(dsl[h])
        dxk[h * 4:(h + 1) * 4] = np.stack([hi_, lo_, -sl * one, -sl * one])
        dxq[h * 4:(h + 1) * 4] = np.stack([sl * one, sl * one, hi_, lo_])
    asl = 2.0 ** (-8.0 * np.arange(1, 13) / 12)
    dlq = np.zeros((96, S), np.float32)
    dlk = np.zeros((96, S), np.float32)
    for g, (win, d) in enumerate(DIL_PAT):
        Tt = pos // d
        th, tl = (Tt // 128 * 128).astype(np.float32), (Tt % 128).astype(np.float32)
        for h in range(4):
            gh = g * 4 + h
            sd = np.float32(asl[gh]) * np.float32(d)
            shi = _bf16_round(sd)
            slo = _bf16_round(np.float32(sd) - shi)
            dlk[gh * 8:(gh + 1) * 8] = np.stack([th, th, tl, tl, -shi * one, -slo * one, -shi * one, -slo * one])
            dlq[gh * 8:(gh + 1) * 8] = np.stack([shi * one, slo * one, shi * one, slo * one, th, th, tl, tl])
    return dxq, dxk, dlq, dlk


def make_inmaps(inp, L, NS, ncores, l0=0, x=None):
    inp = dict(inp)
    for k_ in list(inp.keys()):
        if k_ != 'x':
            inp[k_] = np.asarray(inp[k_])[l0:l0 + L]
    cols = build_cols(inp, L, l0)
    ii = np.arange(128)
    U = (ii[:, None] <= ii[None, :]).astype(np.float32)
    Lm = (ii[:, None] >= ii[None, :]).astype(np.float32)
    sel = np.zeros((128, 128), np.float32)
    sel[64, :] = 1.0
    blk = np.zeros((128, 128), np.float32)
    blk[0:64, 0:64] = 1.0
    blk[64:128, 64:128] = 1.0
    cm = np.concatenate([np.eye(128, dtype=np.float32), np.ones((128, 128), np.float32), U, Lm, sel, blk], axis=1)
    if x is None:
        x = inp['x']
    x = np.ascontiguousarray(np.asarray(x, np.float32)).reshape(-1, D)
    dxq, dxk, dlq, dlk = pos_tables()
    lamv = np.stack([np.broadcast_to(np.concatenate([inp['diff_lam_q1'][l], inp['diff_lam_k1'][l],
                                                      inp['diff_lam_q2'][l], inp['diff_lam_k2'][l]])[None, :], (128, 128))
                     for l in range(L)]).astype(np.float32)
    lamv = np.ascontiguousarray(lamv)
    maps = []
    for c in range(ncores):
        m = {
            "x": np.ascontiguousarray(x[c * NS * S:(c + 1) * NS * S]),
            "w_in": np.ascontiguousarray(inp['w_in'][:L]),
            "w_branch": np.ascontiguousarray(inp['w_branch'][:L]),
            "w_out": np.ascontiguousarray(inp['w_out'][:L]),
            "ffn_w_gate": np.ascontiguousarray(inp['ffn_w_gate'][:L]),
            "ffn_w_up": np.ascontiguousarray(inp['ffn_w_up'][:L]),
            "ffn_w_down": np.ascontiguousarray(inp['ffn_w_down'][:L]),
            "cols": cols,
            "cmats": cm,
            "dxq": dxq, "dxk": dxk, "dlq": dlq, "dlk": dlk, "lamv": lamv,
            "wa_up": np.ascontiguousarray(np.concatenate([inp['rwkv_w_up'][:L], inp['rwkv_a_up'][:L]], axis=1).astype(np.float32)),
            "g_up": np.ascontiguousarray(inp['rwkv_g_up'][:L].astype(np.float32)),
        }
        maps.append(m)
    return maps


def kernel(**inputs):
    inp = {k: np.asarray(v) for k, v in inputs.items()}
    prog = Prog(L=DEPTH, NS=2)
    nc = prog.build()
    maps = make_inmaps(inp, DEPTH, 2, 8)
    res = run_bass_kernel_spmd(nc, maps, core_ids=list(range(8)))
    x = np.concatenate([r["out"] for r in res.results], axis=0)
    return x.reshape(16, S, D).astype(np.float32)
```

```python
import numpy as np
import concourse.bass as bass
import concourse.mybir as mybir
from concourse.bass_utils import run_bass_kernel_spmd

F32 = mybir.dt.float32
BF16 = mybir.dt.bfloat16
AF = mybir.ActivationFunctionType
ALU = mybir.AluOpType
AX = mybir.AxisListType

D = 1024
S = 4096
DEPTH = 4
KC = 8
IN_COLS = 8704
D_FF = 2816
FC = 22
RW0, DIL0, DIF0, CONV0, GATE0 = 0, 1024, 3328, 4096, 4608
SEM_LIMIT = 30000
NCM = 768


class T:
    def __init__(self, h, const=False):
        self.h = h
        self.w = None
        self.rd = {}
        self.const = const

    def __getitem__(self, idx):
        return self.h[idx]


class Eng:
    def __init__(self, kb, name, e, is_pe=False):
        self.kb, self.name, self.e, self.is_pe = kb, name, e, is_pe
        self.gen = 0
        self.sem = kb.nc.alloc_semaphore(f"s_{name}_0")
        self.cnt = 0
        self.seen = {}
        self.last = None
        self.dsems = []
        self.di = 0

    def signal(self, ins):
        if self.cnt >= SEM_LIMIT:
            self.gen += 1
            self.sem = self.kb.nc.alloc_semaphore(f"s_{self.name}_{self.gen}")
            self.cnt = 0
        self.cnt += 1
        ins.then_inc(self.sem, 1)
        ev = (self.sem, self.cnt, self)
        self.last = ev
        return ev


class KB:
    def __init__(self, nc, ndma_sems=12):
        self.nc = nc
        self.eng = {
            'pe': Eng(self, 'pe', nc.tensor, True),
            'act': Eng(self, 'act', nc.scalar),
            'dve': Eng(self, 'dve', nc.vector),
            'pool': Eng(self, 'pool', nc.gpsimd),
            'sp': Eng(self, 'sp', nc.sync),
        }
        self.ndma = ndma_sems
        self.dma_latest = {}
        self.n_ins = 0
        self.uid = 0

    def sb(self, name, shape, dt, const=False):
        self.uid += 1
        return T(self.nc.alloc_sbuf_tensor(f"{name}_{self.uid}", list(shape), dt), const)

    def ps(self, name, shape=(128, 512), dt=F32):
        self.uid += 1
        return T(self.nc.alloc_psum_tensor(f"{name}_{self.uid}", list(shape), dt))

    def _wait(self, E, deps):
        best = {}
        for d in deps:
            if d is None:
                continue
            sem, val, src = d
            if src is E and E.is_pe:
                continue
            k = id(sem)
            if E.seen.get(k, 0) >= val:
                continue
            if k not in best or best[k][1] < val:
                best[k] = (sem, val)
        for k, (sem, val) in best.items():
            E.e.wait_ge(sem, val)
            E.seen[k] = val
            self.n_ins += 1

    def _deps(self, r, w):
        deps = []
        for t in r:
            if t.w is not None:
                deps.append(t.w)
        for t in w:
            if t.w is not None:
                deps.append(t.w)
            deps.extend(t.rd.values())
        return deps

    def _mark(self, ev, r, w):
        for t in w:
            t.w = ev
            t.rd = {}
        for t in r:
            if t.const or t in w:
                continue
            t.rd[id(ev[0])] = ev

    def op(self, en, fn, r=(), w=(), extra=()):
        E = self.eng[en]
        self._wait(E, self._deps(r, w) + list(extra))
        ins = fn(E.e)
        ev = E.signal(ins)
        self._mark(ev, r, w)
        self.n_ins += 1
        return ev

    def mm(self, out_t, out_ap, pairs, r=(), extra=()):
        E = self.eng['pe']
        self._wait(E, self._deps(r, (out_t,)) + list(extra))
        n = len(pairs)
        ins = None
        for i, (l, rh) in enumerate(pairs):
            ins = E.e.matmul(out_ap, l, rh, start=(i == 0), stop=(i == n - 1))
            self.n_ins += 1
        ev = E.signal(ins)
        self._mark(ev, r, (out_t,))
        return ev

    def mm1(self, out_t, out_ap, l, rh, start, stop, r=(), sig=True):
        E = self.eng['pe']
        self._wait(E, self._deps(r, (out_t,)))
        ins = E.e.matmul(out_ap, l, rh, start=start, stop=stop)
        self.n_ins += 1
        if not sig:
            return None
        ev = E.signal(ins)
        self._mark(ev, r, (out_t,))
        return ev

    def dma(self, qn, out, in_, r=(), w=(), extra=(), **kw):
        E = self.eng[qn]
        if len(E.dsems) < self.ndma:
            E.dsems.append([self.nc.alloc_semaphore(f"d_{qn}_{len(E.dsems)}_0"), 0, 0])
        slot = E.dsems[E.di % self.ndma]
        E.di += 1
        if slot[1] * 16 >= SEM_LIMIT:
            slot[2] += 1
            slot[0] = self.nc.alloc_semaphore(f"d_{qn}_{E.di % self.ndma}_{slot[2]}")
            slot[1] = 0
        prev = (slot[0], slot[1] * 16, None) if slot[1] > 0 else None
        self._wait(E, self._deps(r, w) + list(extra) + [prev])
        ins = E.e.dma_start(out=out, in_=in_, **kw)
        slot[1] += 1
        ins.then_inc(slot[0], 16)
        ev = (slot[0], slot[1] * 16, None)
        self.dma_latest[id(slot[0])] = ev
        self._mark(ev, r, w)
        self.n_ins += 1
        return ev

    def barrier(self):
        evs = [E.last for E in self.eng.values() if E.last is not None]
        evs += list(self.dma_latest.values())
        for E in self.eng.values():
            self._wait(E, [e for e in evs if not (e[2] is E and E.is_pe)])
        self.dma_latest = {}


def colblock(v):
    v = np.asarray(v, np.float32).reshape(-1)
    n = v.size // 128
    return np.ascontiguousarray(v.reshape(n, 128).T)


class ColTable:
    def __init__(self):
        self.parts, self.idx, self.n = [], {}, 0

    def add(self, name, v):
        b = colblock(v)
        self.idx[name] = self.n
        self.parts.append(b)
        self.n += b.shape[1]

    def build(self):
        return np.ascontiguousarray(np.concatenate(self.parts, axis=1))


def col_layout(L):
    idx, n = {}, 0
    for l in range(L):
        for nm, w in (('nmp', 8), ('nmo', 8), ('nfp', 8), ('nfo', 8), ('gb', 32), ('mu', 8), ('cw', 62), ('cb', 2), ('cg', 2), ('cbb', 2), ('slg', 1), ('w0', 2), ('a0', 2), ('kk', 2), ('ka', 2), ('rk', 2), ('lng', 2), ('lnb', 2), ('lmi', 1), ('oml', 1)):
            idx[(nm, l)] = n
            n += w
    return idx, n


def build_cols(inp, L, l0=0):
    import math
    ct = ColTable()
    for l in range(L):
        ct.add(('nmp', l), inp['norm_mix_pre'][l])
        ct.add(('nmo', l), inp['norm_mix_post'][l])
        ct.add(('nfp', l), inp['norm_ffn_pre'][l])
        ct.add(('nfo', l), inp['norm_ffn_post'][l])
        ct.add(('gb', l), inp['gate_bias'][l])
        ct.add(('mu', l), inp['rwkv_mu'][l])
        ct.add(('cw', l), inp['conv_dw_w'][l].reshape(-1))
        ct.add(('cb', l), inp['conv_dw_b'][l])
        ct.add(('cg', l), inp['conv_ln_g'][l])
        ct.add(('cbb', l), inp['conv_ln_b'][l])
        ct.add(('slg', l), np.concatenate([inp['diff_subln_g'][l], np.zeros(64, np.float32)]))
        ct.add(('w0', l), inp['rwkv_w0'][l])
        ct.add(('a0', l), inp['rwkv_a0'][l])
        ct.add(('kk', l), inp['rwkv_k_k'][l])
        ct.add(('ka', l), inp['rwkv_k_a'][l])
        ct.add(('rk', l), inp['rwkv_r_k'][l].reshape(-1))
        ct.add(('lng', l), inp['rwkv_ln_g'][l])
        ct.add(('lnb', l), inp['rwkv_ln_b'][l])
        li = 0.8 - 0.6 * math.exp(-0.3 * (l0 + l))
        ct.add(('lmi', l), np.full(128, -li, np.float32))
        ct.add(('oml', l), np.full(128, 1.0 - li, np.float32))
    return ct.build()


class Prog:
    def __init__(self, L=DEPTH, NS=2, debug=()):
        self.L, self.NS, self.NT = L, NS, NS * S
        self.debug = set(debug)
        nc = bass.Bass("TRN2", target_bir_lowering=False)
        self.nc = nc
        self.kb = KB(nc)
        NT = self.NT
        ein = lambda n, s, d=F32: nc.dram_tensor(n, list(s), d, kind="ExternalInput").ap()
        self.x = ein("x", [NT, D])
        self.w_in = ein("w_in", [L, D, IN_COLS])
        self.w_branch = ein("w_branch", [L, 4, 256, D])
        self.w_out = ein("w_out", [L, D, D])
        self.w_g = ein("ffn_w_gate", [L, D, D_FF])
        self.w_u = ein("ffn_w_up", [L, D, D_FF])
        self.w_d = ein("ffn_w_down", [L, D_FF, D])
        self.cidx, ncols = col_layout(L)
        self.cols_d = ein("cols", [128, ncols])
        self.cm_d = ein("cmats", [128, NCM])
        self.dxq = ein("dxq", [16, S])
        self.dxk = ein("dxk", [16, S])
        self.dlq = ein("dlq", [96, S])
        self.dlk = ein("dlk", [96, S])
        self.lamv = ein("lamv", [L, 128, 128])
        self.wa_up = ein("wa_up", [L, 128, 256])
        self.g_up = ein("g_up", [L, 128, 256])
        self.out = nc.dram_tensor("out", [NT, D], F32, kind="ExternalOutput").ap()
        self.ncols = ncols

    def scratch(self, name, shape, dt):
        kind = "ExternalOutput" if name in self.debug else "Internal"
        return self.nc.dram_tensor(name, list(shape), dt, kind=kind).ap()

    def build(self):
        nc, kb, L, NT = self.nc, self.kb, self.L, self.NT
        self.cols = kb.sb("cols", [128, self.ncols], F32, const=True)
        self.cm = kb.sb("cmats", [128, NCM], F32, const=True)
        self.identb = kb.sb("identb", [128, 128], BF16, const=True)
        kb.dma('sp', self.cols[:, :], self.cols_d[:, :], w=(self.cols,))
        kb.dma('sp', self.cm[:, :], self.cm_d[:, :], w=(self.cm,))
        kb.op('dve', lambda e: e.tensor_copy(self.identb[:, :], self.cm[:, 0:128]), r=(self.cm,), w=(self.identb,))
        self.ident = self.cm
        self.epsc = kb.sb("epsc", [128, 4], F32, const=True)
        kb.op('dve', lambda e: e.memset(self.epsc[:, 0:1], 1e-6), w=(self.epsc,))
        kb.op('dve', lambda e: e.memset(self.epsc[:, 1:2], 1e-5), w=(self.epsc,))
        kb.op('dve', lambda e: e.memset(self.epsc[:, 2:3], 64e-5), w=(self.epsc,))
        kb.op('dve', lambda e: e.memset(self.epsc[:, 3:4], 0.0), w=(self.epsc,))
        self.psb = [kb.ps(f"psb{i}") for i in range(4)]
        self.psi = 0
        self.pacc = [kb.ps(f"pacc{i}") for i in range(3)]
        self.psT = kb.ps("psT", (128, 1024), BF16)
        self.mUL = kb.sb("mUL", [128, 256], BF16, const=True)
        kb.op('dve', lambda e: e.tensor_copy(self.mUL[:, :], self.cm[:, 256:512]), r=(self.cm,), w=(self.mUL,))
        self.xT = self.scratch("xT", [D, NT], F32)
        self.wb_in = self.scratch("wb_in", [L, D, IN_COLS], BF16)
        self.wb_br = self.scratch("wb_br", [L, 1024, D], BF16)
        self.wb_out = self.scratch("wb_out", [L, D, D], BF16)
        self.wb_g = self.scratch("wb_g", [L, D, D_FF], BF16)
        self.wb_u = self.scratch("wb_u", [L, D, D_FF], BF16)
        self.wb_d = self.scratch("wb_d", [L, D_FF, D], BF16)
        self.P_rw = self.scratch("P_rw", [1024, NT], F32)
        self.P_dq = self.scratch("P_dq", [768, NT], BF16)
        self.P_dk = self.scratch("P_dk", [768, NT], BF16)
        self.P_dv = self.scratch("P_dv", [768, NT], BF16)
        self.P_cq = self.scratch("P_cq", [256, NT], BF16)
        self.P_ck = self.scratch("P_ck", [256, NT], BF16)
        self.P_cv = self.scratch("P_cv", [256, NT], BF16)
        self.P_cn = self.scratch("P_cn", [512, NT], F32)
        self.P_g = self.scratch("P_g", [4096, NT], BF16)
        self.Y = self.scratch("Y", [1024, NT], BF16)
        self.ROWS = self.scratch("ROWS", [S, 5, 2, 256], F32)
        self.Vf = self.scratch("Vf", [256, NT], F32)
        self.Gf = self.scratch("Gf", [256, NT], F32)
        self.BNVf = self.scratch("BNVf", [256, NT], F32)
        self.Of = self.scratch("Of", [256, NT], F32)

        sb0 = nc.sbuf_base
        self.stage_cast()
        kb.barrier()
        nc.sbuf_base = sb0
        self.stage_xin()
        kb.barrier()
        for l in range(L):
            nc.sbuf_base = sb0
            self.stage_proj(l)
            kb.barrier()
            nc.sbuf_base = sb0
            self.stage_mixers(l)
            kb.barrier()
            nc.sbuf_base = sb0
            self.stage_merge(l)
            kb.barrier()
            nc.sbuf_base = sb0
            self.stage_ffn(l)
            kb.barrier()
        nc.sbuf_base = sb0
        self.stage_xout()
        kb.barrier()
        return nc

    def next_ps(self):
        p = self.psb[self.psi % 4]
        self.psi += 1
        return p

    def stage_cast(self):
        kb, L = self.kb, self.L
        CW = 2176
        NB = 3
        fin = [kb.sb(f"cast_in{i}", [128, CW], F32) for i in range(NB)]
        fout = [kb.sb(f"cast_out{i}", [128, CW], BF16) for i in range(NB)]
        engs = ['act', 'dve', 'pool']
        jobs = []
        for l in range(L):
            jobs.append((self.wb_in[l], self.w_in[l], D, IN_COLS))
            jobs.append((self.wb_br[l], self.w_branch[l].rearrange("n c d -> (n c) d"), 1024, D))
            jobs.append((self.wb_out[l], self.w_out[l], D, D))
            jobs.append((self.wb_g[l], self.w_g[l], D, D_FF))
            jobs.append((self.wb_u[l], self.w_u[l], D, D_FF))
            jobs.append((self.wb_d[l], self.w_d[l], D_FF, D))
        i = 0
        for dst, src, rows, cols in jobs:
            for r0 in range(0, rows, 128):
                for c0 in range(0, cols, CW):
                    cw = min(CW, cols - c0)
                    a, b = fin[i % NB], fout[i % NB]
                    kb.dma('sp', a[:, 0:cw], src[r0:r0 + 128, c0:c0 + cw], w=(a,))
                    en = engs[i % 3]
                    if en == 'act':
                        kb.op('act', lambda e, a=a, b=b, cw=cw: e.copy(b[:, 0:cw], a[:, 0:cw]), r=(a,), w=(b,))
                    else:
                        kb.op(en, lambda e, a=a, b=b, cw=cw: e.tensor_copy(b[:, 0:cw], a[:, 0:cw]), r=(a,), w=(b,))
                    kb.dma('pool', dst[r0:r0 + 128, c0:c0 + cw], b[:, 0:cw], r=(b,))
                    i += 1

    def stage_xin(self):
        kb, NT = self.kb, self.NT
        NB = 2
        xin = [kb.sb(f"xin{i}", [128, 4, D], F32) for i in range(NB)]
        xo = [kb.sb(f"xo{i}", [128, KC, 512], F32) for i in range(NB)]
        xv = self.x.rearrange("(n j p) d -> n p j d", p=128, j=4)
        xTv = self.xT.rearrange("(kc p) t -> p kc t", p=128)
        for n in range(NT // 512):
            a, b = xin[n % NB], xo[n % NB]
            kb.dma('sp', a[:, :, :], xv[n], w=(a,))
            for kc in range(KC):
                ps = self.next_ps()
                for j in range(4):
                    kb.op('pe', lambda e, ps=ps, a=a, j=j, kc=kc: e.transpose(
                        ps[:, j * 128:(j + 1) * 128], a[:, j, kc * 128:(kc + 1) * 128], self.ident[:, 0:128]),
                        r=(a,), w=(ps,))
                en = 'act' if kc % 2 == 0 else 'dve'
                if en == 'act':
                    kb.op('act', lambda e, ps=ps, b=b, kc=kc: e.copy(b[:, kc, :], ps[:, :]), r=(ps,), w=(b,))
                else:
                    kb.op('dve', lambda e, ps=ps, b=b, kc=kc: e.tensor_copy(b[:, kc, :], ps[:, :]), r=(ps,), w=(b,))
            kb.dma('pool', xTv[:, :, n * 512:(n + 1) * 512], b[:, :, :], r=(b,))

    def stage_xout(self):
        kb, NT = self.kb, self.NT
        NB = 2
        xi = [kb.sb(f"xoi{i}", [128, KC, 512], F32) for i in range(NB)]
        xo = [kb.sb(f"xoo{i}", [128, 4, D], F32) for i in range(NB)]
        ov = self.out.rearrange("(n j p) d -> n p j d", p=128, j=4)
        xTv = self.xT.rearrange("(kc p) t -> p kc t", p=128)
        for n in range(NT // 512):
            a, b = xi[n % NB], xo[n % NB]
            kb.dma('sp', a[:, :, :], xTv[:, :, n * 512:(n + 1) * 512], w=(a,))
            for j in range(4):
                for h in range(2):
                    ps = self.next_ps()
                    for q in range(4):
                        kc = h * 4 + q
                        kb.op('pe', lambda e, ps=ps, a=a, j=j, kc=kc, q=q: e.transpose(
                            ps[:, q * 128:(q + 1) * 128], a[:, kc, j * 128:(j + 1) * 128], self.ident[:, 0:128]),
                            r=(a,), w=(ps,))
                    if h == 0:
                        kb.op('act', lambda e, ps=ps, b=b, j=j: e.copy(b[:, j, 0:512], ps[:, :]), r=(ps,), w=(b,))
                    else:
                        kb.op('dve', lambda e, ps=ps, b=b, j=j: e.tensor_copy(b[:, j, 512:1024], ps[:, :]), r=(ps,), w=(b,))
            kb.dma('pool', ov[n], b[:, :, :], r=(b,))

    def stage_proj(self, l):
        kb, NT = self.kb, self.NT
        TT = 2048
        NSUB = TT // 512
        xt = [kb.sb(f"pj_x{i}", [128, KC, 512], F32) for i in range(2)]
        sq = kb.sb("pj_sq", [128, KC, 512], F32)
        rstd = kb.sb("pj_rstd", [128, 512], F32)
        hT = kb.sb("pj_h", [128, KC, TT], BF16)
        hsub = [T(hT.h) for _ in range(NSUB)]
        NW = 3
        wt = [kb.sb(f"pj_w{i}", [128, KC, 128], BF16) for i in range(NW)]
        NO = 4
        ot32 = [kb.sb(f"pj_o32_{i}", [128, 512], F32) for i in range(NO)]
        ot16 = [kb.sb(f"pj_o16_{i}", [128, 512], BF16) for i in range(NO)]
        xTv = self.xT.rearrange("(kc p) t -> p kc t", p=128)
        wv = self.wb_in[l].rearrange("(kc p) n -> p kc n", p=128)
        gb0 = self.cidx[('gb', l)]
        oi = 0
        wi = 0
        for st in range(NT // TT):
            t0 = st * TT
            for s in range(NSUB):
                a = xt[s % 2]
                kb.dma('sp', a[:, :, :], xTv[:, :, t0 + s * 512:t0 + (s + 1) * 512], w=(a,))
                hs = hsub[s]
                self._norm_into(a, hT, s * 512, hs, self.cidx[('nmp', l)], sq, rstd)
            for oc in range(IN_COLS // 128):
                w = wt[wi % NW]
                wi += 1
                kb.dma('sp', w[:, :, :], wv[:, :, oc * 128:(oc + 1) * 128], w=(w,))
                c0 = oc * 128
                for s in range(NSUB):
                    ps = self.next_ps()
                    kb.mm(ps, ps[:, :], [(w[:, kc, :], hT[:, kc, s * 512:(s + 1) * 512]) for kc in range(KC)],
                          r=(w, hsub[s]))
                    tsl = slice(t0 + s * 512, t0 + (s + 1) * 512)
                    en = 'act' if oi % 2 == 0 else 'dve'
                    if c0 < DIL0:
                        o = ot32[oi % NO]
                        self._evac(en, o, ps, None)
                        dst = self.P_rw[c0:c0 + 128, tsl]
                    elif c0 < DIF0:
                        o = ot16[oi % NO]
                        cc = c0 - DIL0
                        if cc < 768:
                            self._evac(en, o, ps, 0.125)
                            dst = self.P_dq[cc:cc + 128, tsl]
                        elif cc < 1536:
                            self._evac(en, o, ps, None)
                            dst = self.P_dk[cc - 768:cc - 640, tsl]
                        else:
                            self._evac(en, o, ps, None)
                            dst = self.P_dv[cc - 1536:cc - 1408, tsl]
                    elif c0 < CONV0:
                        o = ot16[oi % NO]
                        cc = c0 - DIF0
                        if cc < 256:
                            self._evac(en, o, ps, 32.0 ** -0.5)
                            dst = self.P_cq[cc:cc + 128, tsl]
                        elif cc < 512:
                            self._evac(en, o, ps, None)
                            dst = self.P_ck[cc - 256:cc - 128, tsl]
                        else:
                            self._evac(en, o, ps, None)
                            dst = self.P_cv[cc - 512:cc - 384, tsl]
                    elif c0 < GATE0:
                        o = ot32[oi % NO]
                        self._evac(en, o, ps, None)
                        dst = self.P_cn[c0 - CONV0:c0 - CONV0 + 128, tsl]
                    else:
                        o = ot16[oi % NO]
                        gc = (c0 - GATE0) // 128
                        kb.op('act', lambda e, o=o, ps=ps, gc=gc: e.activation(
                            out=o[:, :], in_=ps[:, :], func=AF.Sigmoid, bias=self.cols[:, gb0 + gc:gb0 + gc + 1]),
                            r=(ps,), w=(o,))
                        dst = self.P_g[c0 - GATE0:c0 - GATE0 + 128, tsl]
                    kb.dma('pool', dst, o[:, :], r=(o,))
                    oi += 1

    def _evac(self, en, o, ps, scale):
        kb = self.kb
        if en == 'act':
            if scale is None:
                kb.op('act', lambda e: e.copy(o[:, :], ps[:, :]), r=(ps,), w=(o,))
            else:
                kb.op('act', lambda e: e.mul(o[:, :], ps[:, :], scale), r=(ps,), w=(o,))
        else:
            if scale is None:
                kb.op('dve', lambda e: e.tensor_copy(o[:, :], ps[:, :]), r=(ps,), w=(o,))
            else:
                kb.op('dve', lambda e: e.tensor_scalar(o[:, :], ps[:, :], scale, None, ALU.mult), r=(ps,), w=(o,))

    def _norm_into(self, xt, hT, off, htrk, gcol0, sq, rstd, n=512):
        kb = self.kb
        kb.op('act', lambda e: e.activation(out=sq[:, :, 0:n], in_=xt[:, :, 0:n], func=AF.Square), r=(xt,), w=(sq,))
        ps = self.next_ps()
        kb.mm(ps, ps[:, 0:n], [(self.cm[:, 128:256], sq[:, kc, 0:n]) for kc in range(KC)], r=(sq, self.cm))
        kb.op('act', lambda e: e.activation(out=rstd[:, 0:n], in_=ps[:, 0:n], func=AF.Sqrt, bias=self.epsc[:, 0:1], scale=1.0 / D),
              r=(ps,), w=(rstd,))
        kb.op('dve', lambda e: e.reciprocal(rstd[:, 0:n], rstd[:, 0:n]), r=(rstd,), w=(rstd,))
        for kc in range(KC):
            kb.op('dve', lambda e, kc=kc: e.scalar_tensor_tensor(
                hT[:, kc, off:off + n], xt[:, kc, 0:n], self.cols[:, gcol0 + kc:gcol0 + kc + 1], rstd[:, 0:n],
                ALU.mult, ALU.mult), r=(xt, rstd), w=(htrk,))

    def stage_mixers(self, l):
        nc = self.nc
        sb0 = nc.sbuf_base
        for fn in (self.mixer_conv, self.mixer_diff, self.mixer_dil, self.mixer_rwkv):
            nc.sbuf_base = sb0
            fn(l)
            self.kb.barrier()

    def mixer_rwkv(self, l):
        nc, kb = self.nc, self.kb
        sb0 = nc.sbuf_base
        self.rwkv_prelude(l)
        kb.barrier()
        nc.sbuf_base = sb0
        self.rwkv_scan(l)
        kb.barrier()
        nc.sbuf_base = sb0
        self.rwkv_post(l)

    def rwkv_prelude(self, l):
        kb = self.kb
        ci = lambda k: self.cidx[(k, l)]
        col = lambda i: self.cols[:, i:i + 1]
        blk = self.cm[:, 640:768]
        wa = kb.sb("rw_wa", [128, 256], F32)
        gup = kb.sb("rw_gup", [128, 256], F32)
        kb.dma('sp', wa[:, :], self.wa_up[l], w=(wa,))
        kb.dma('sp', gup[:, :], self.g_up[l], w=(gup,))
        pin = [kb.sb(f"rw_pin{i}", [128, 8, 513], F32) for i in range(2)]
        pm = kb.sb("rw_pm", [128, 8, 512], F32)
        tz = kb.sb("rw_tz", [128, 512], F32)
        sg = kb.sb("rw_sg", [128, 512], F32)
        Q = [kb.sb(f"rw_q{i}", [128, 2, 512], F32) for i in range(5)]
        Wq, NKK, Bq, K2, Rq = Q
        aq = kb.sb("rw_a", [128, 2, 512], F32)
        gq = kb.sb("rw_g", [128, 2, 512], F32)
        kkq = kb.sb("rw_kk", [128, 2, 512], F32)
        sq = kb.sb("rw_sq", [128, 2, 512], F32)
        rn = kb.sb("rw_rn", [128, 2, 512], F32)
        tq = kb.sb("rw_t", [128, 2, 512], F32)
        bnv = kb.sb("rw_bnv", [128, 2, 512], F32)
        vq = kb.sb("rw_v", [128, 2, 512], F32)
        rowb = [kb.sb(f"rw_rowb{i}", [128, 5, 256], F32) for i in range(2)]
        Pv = self.P_rw.rearrange("(c p) t -> p c t", p=128)
        fview = lambda dr: dr.rearrange("(c p) t -> p c t", p=128)
        ri = 0
        for si in range(self.NS):
            tb = si * S
            for n in range(S // 512):
                t0 = tb + n * 512
                p_ = pin[n % 2]
                if n == 0:
                    kb.op('dve', lambda e, p_=p_: e.memset(p_[:, :, 0:1], 0.0), w=(p_,))
                    kb.dma('sp', p_[:, :, 1:513], Pv[:, :, t0:t0 + 512], w=(p_,))
                else:
                    kb.dma('sp', p_[:, :, 0:513], Pv[:, :, t0 - 1:t0 + 512], w=(p_,))
                kb.op('pool', lambda e, p_=p_: e.tensor_tensor(pm[:, :, :], p_[:, :, 0:512], p_[:, :, 1:513], ALU.subtract),
                      r=(p_,), w=(pm,))
                for c in range(8):
                    kb.op('dve', lambda e, p_=p_, c=c: e.scalar_tensor_tensor(
                        pm[:, c, :], pm[:, c, :], col(ci('mu') + c), p_[:, c, 1:513], ALU.mult, ALU.add), r=(pm, p_), w=(pm,))
                kb.op('act', lambda e: e.copy(Rq[:, :, :], pm[:, 0:2, :]), r=(pm,), w=(Rq,))
                kb.op('act', lambda e: e.copy(vq[:, :, :], pm[:, 4:6, :]), r=(pm,), w=(vq,))
                kb.op('act', lambda e: e.activation(out=tz[0:64, :], in_=pm[0:64, 6, :], func=AF.Tanh), r=(pm,), w=(tz,))
                for c in range(2):
                    ps = self.next_ps()
                    kb.mm(ps, ps[:, :], [(wa[0:64, c * 128:(c + 1) * 128], tz[0:64, :])], r=(wa, tz))
                    kb.op('act', lambda e, ps=ps, c=c: e.activation(out=Wq[:, c, :], in_=ps[:, :], func=AF.Sigmoid, bias=col(ci('w0') + c)),
                          r=(ps,), w=(Wq,))
                kb.op('act', lambda e: e.activation(out=Wq[:, :, :], in_=Wq[:, :, :], func=AF.Exp, scale=-0.606531), r=(Wq,), w=(Wq,))
                for c in range(2):
                    ps = self.next_ps()
                    kb.mm(ps, ps[:, :], [(wa[64:128, c * 128:(c + 1) * 128], pm[64:128, 6, :])], r=(wa, pm))
                    kb.op('act', lambda e, ps=ps, c=c: e.activation(out=aq[:, c, :], in_=ps[:, :], func=AF.Sigmoid, bias=col(ci('a0') + c)),
                          r=(ps,), w=(aq,))
                kb.op('act', lambda e: e.activation(out=sg[:, :], in_=pm[:, 7, :], func=AF.Sigmoid), r=(pm,), w=(sg,))
                for c in range(2):
                    ps = self.next_ps()
                    kb.mm(ps, ps[:, :], [(gup[:, c * 128:(c + 1) * 128], sg[:, :])], r=(gup, sg))
                    kb.op('act', lambda e, ps=ps, c=c: e.copy(gq[:, c, :], ps[:, :]), r=(ps,), w=(gq,))
                for c in range(2):
                    kb.op('dve', lambda e, c=c: e.tensor_scalar(kkq[:, c, :], pm[:, 2 + c, :], col(ci('kk') + c), None, ALU.mult),
                          r=(pm,), w=(kkq,))
                kb.op('act', lambda e: e.activation(out=sq[:, :, :], in_=kkq[:, :, :], func=AF.Square), r=(kkq,), w=(sq,))
                for c in range(2):
                    ps = self.next_ps()
                    kb.mm(ps, ps[:, :], [(blk, sq[:, c, :])], r=(sq,))
                    kb.op('act', lambda e, ps=ps, c=c: e.activation(out=rn[:, c, :], in_=ps[:, :], func=AF.Sqrt), r=(ps,), w=(rn,))
                kb.op('dve', lambda e: e.tensor_scalar(rn[:, :, :], rn[:, :, :], 1e-12, None, ALU.max), r=(rn,), w=(rn,))
                kb.op('dve', lambda e: e.reciprocal(rn[:, :, :], rn[:, :, :]), r=(rn,), w=(rn,))
                kb.op('dve', lambda e: e.tensor_tensor(kkq[:, :, :], kkq[:, :, :], rn[:, :, :], ALU.mult), r=(kkq, rn), w=(kkq,))
                kb.op('act', lambda e: e.mul(NKK[:, :, :], kkq[:, :, :], -1.0), r=(kkq,), w=(NKK,))
                kb.op('dve', lambda e: e.tensor_tensor(Bq[:, :, :], kkq[:, :, :], aq[:, :, :], ALU.mult), r=(kkq, aq), w=(Bq,))
                for c in range(2):
                    kb.op('dve', lambda e, c=c: e.tensor_scalar(tq[:, c, :], aq[:, c, :], -1.0, col(ci('ka') + c), ALU.add, ALU.mult),
                          r=(aq,), w=(tq,))
                kb.op('dve', lambda e: e.scalar_tensor_tensor(K2[:, :, :], tq[:, :, :], 1.0, pm[:, 2:4, :], ALU.add, ALU.mult),
                      r=(tq, pm), w=(K2,))
                for c in range(2):
                    kb.op('dve', lambda e, c=c: e.scalar_tensor_tensor(tq[:, c, :], Rq[:, c, :], col(ci('rk') + c), K2[:, c, :],
                                                                       ALU.mult, ALU.mult), r=(Rq, K2, tq), w=(tq,))
                for c in range(2):
                    ps = self.next_ps()
                    kb.mm(ps, ps[:, :], [(blk, tq[:, c, :])], r=(tq,))
                    kb.op('dve', lambda e, ps=ps, c=c: e.tensor_tensor(bnv[:, c, :], ps[:, :], vq[:, c, :], ALU.mult),
                          r=(ps, vq), w=(bnv,))
                kb.dma('pool', fview(self.Vf)[:, :, t0:t0 + 512], vq[:, :, :], r=(vq,))
                kb.dma('pool', fview(self.Gf)[:, :, t0:t0 + 512], gq[:, :, :], r=(gq,))
                kb.dma('pool', fview(self.BNVf)[:, :, t0:t0 + 512], bnv[:, :, :], r=(bnv,))
                for j in range(4):
                    rb = rowb[ri % 2]
                    ri += 1
                    for q0 in range(0, 5, 2):
                        ps = self.next_ps()
                        nq = min(2, 5 - q0)
                        for qq in range(nq):
                            for c in range(2):
                                kb.op('pe', lambda e, ps=ps, qq=qq, c=c, q0=q0, j=j: e.transpose(
                                    ps[:, (qq * 2 + c) * 128:(qq * 2 + c + 1) * 128], Q[q0 + qq][:, c, j * 128:(j + 1) * 128],
                                    self.ident[:, 0:128]), r=(Q[q0 + qq],), w=(ps,))
                        kb.op('act' if q0 != 2 else 'dve',
                              (lambda e, ps=ps, rb=rb, q0=q0, nq=nq: e.copy(
                                  rb[:, q0:q0 + nq, :], ps[:, 0:nq * 256].rearrange("p (q f) -> p q f", f=256))) if q0 != 2 else
                              (lambda e, ps=ps, rb=rb, q0=q0, nq=nq: e.tensor_copy(
                                  rb[:, q0:q0 + nq, :], ps[:, 0:nq * 256].rearrange("p (q f) -> p q f", f=256))),
                              r=(ps,), w=(rb,))
                    tl = n * 512 + j * 128
                    kb.dma('pool', self.ROWS[tl:tl + 128, :, si, :], rb[:, :, :], r=(rb,))

    def rwkv_scan(self, l):
        kb = self.kb
        NS = self.NS
        P = 64 * NS
        St = kb.sb("rs_S", [P, 256], F32)
        tmp = [kb.sb(f"rs_tmp{i}", [P, 256], F32) for i in range(2)]
        tmp2 = [kb.sb(f"rs_tp{i}", [P, 256], F32) for i in range(2)]
        sa = [kb.sb(f"rs_sa{i}", [P, 4], F32) for i in range(2)]
        NBB = 8
        bt = [kb.sb(f"rs_bt{i}", [P, NBB, 5, 256], F32) for i in range(2)]
        VB = 64
        vv = [kb.sb(f"rs_vv{i}", [P, 4, VB], F32) for i in range(2)]
        oo = [kb.sb(f"rs_oo{i}", [P, 4, VB], F32) for i in range(2)]
        kb.op('dve', lambda e: e.memset(St[:, :], 0.0), w=(St,))
        v3 = lambda ap: ap.rearrange("p (h k) -> p h k", k=64)
        hview = lambda dr: dr.rearrange("(h p) t -> p h t", p=64)
        bi = 0
        for blk in range(S // VB):
            tB = blk * VB
            V_, O_ = vv[blk % 2], oo[blk % 2]
            for si in range(NS):
                kb.dma('sp', V_[si * 64:(si + 1) * 64, :, :], hview(self.Vf)[:, :, si * S + tB:si * S + tB + VB], w=(V_,))
            for sb_ in range(VB // NBB):
                t0 = tB + sb_ * NBB
                B_ = bt[bi % 2]
                bi += 1
                for si in range(NS):
                    kb.dma('sp', B_[si * 64:(si + 1) * 64, :, :, :], self.ROWS[t0:t0 + NBB, :, si, :].partition_broadcast(64), w=(B_,))
                for j in range(NBB):
                    tt = sb_ * NBB + j
                    X = lambda q: B_[:, j, q, :]
                    X3 = lambda q: B_[:, j, q, :].rearrange("p (h k) -> p h k", k=64)
                    tm, t2, s_ = tmp[tt % 2], tmp2[tt % 2], sa[tt % 2]
                    kb.op('pool', lambda e, t2=t2, X3=X3, V_=V_, tt=tt: e.tensor_tensor(
                        v3(t2[:, :]), X3(3), V_[:, :, tt:tt + 1].to_broadcast([P, 4, 64]), ALU.mult), r=(B_, V_), w=(t2,))
                    kb.op('dve', lambda e, tm=tm, X=X: e.tensor_tensor(tm[:, :], St[:, :], X(1), ALU.mult), r=(St, B_), w=(tm,))
                    kb.op('dve', lambda e, tm=tm, s_=s_: e.tensor_reduce(s_[:, :], v3(tm[:, :]), AX.X, ALU.add), r=(tm,), w=(s_,))
                    kb.op('dve', lambda e, X=X: e.tensor_tensor(St[:, :], St[:, :], X(0), ALU.mult), r=(St, B_), w=(St,))
                    kb.op('dve', lambda e, tm=tm, X3=X3, s_=s_: e.tensor_tensor(
                        v3(tm[:, :]), X3(2), s_[:, :].unsqueeze(2).to_broadcast([P, 4, 64]), ALU.mult), r=(B_, s_), w=(tm,))
                    kb.op('dve', lambda e, tm=tm: e.tensor_tensor(St[:, :], St[:, :], tm[:, :], ALU.add), r=(St, tm), w=(St,))
                    kb.op('dve', lambda e, t2=t2: e.tensor_tensor(St[:, :], St[:, :], t2[:, :], ALU.add), r=(St, t2), w=(St,))
                    kb.op('dve', lambda e, tm=tm, X=X: e.tensor_tensor(tm[:, :], St[:, :], X(4), ALU.mult), r=(St, B_), w=(tm,))
                    kb.op('dve', lambda e, tm=tm, O_=O_, tt=tt: e.tensor_reduce(O_[:, :, tt], v3(tm[:, :]), AX.X, ALU.add),
                          r=(tm,), w=(O_,))
            for si in range(NS):
                kb.dma('pool', hview(self.Of)[:, :, si * S + tB:si * S + tB + VB], O_[si * 64:(si + 1) * 64, :, :], r=(O_,))

    def rwkv_post(self, l):
        kb = self.kb
        ci = lambda k: self.cidx[(k, l)]
        col = lambda i: self.cols[:, i:i + 1]
        blk = self.cm[:, 640:768]
        ot = [kb.sb(f"rp_o{i}", [128, 512], F32) for i in range(2)]
        bt_ = [kb.sb(f"rp_b{i}", [128, 512], F32) for i in range(2)]
        gt = [kb.sb(f"rp_g{i}", [128, 512], F32) for i in range(2)]
        sq = kb.sb("rp_sq", [128, 512], F32)
        mean = kb.sb("rp_mean", [128, 512], F32)
        msq = kb.sb("rp_msq", [128, 512], F32)
        rstd = kb.sb("rp_rstd", [128, 512], F32)
        t1 = kb.sb("rp_t1", [128, 512], F32)
        ob = [kb.sb(f"rp_ob{i}", [128, 512], BF16) for i in range(2)]
        i = 0
        for c in range(2):
            for n in range(self.NT // 512):
                ts_ = slice(n * 512, (n + 1) * 512)
                o, b_, g_ = ot[i % 2], bt_[i % 2], gt[i % 2]
                rows = slice(c * 128, (c + 1) * 128)
                kb.dma('sp', o[:, :], self.Of[rows, ts_], w=(o,))
                kb.dma('sp', b_[:, :], self.BNVf[rows, ts_], w=(b_,))
                kb.dma('sp', g_[:, :], self.Gf[rows, ts_], w=(g_,))
                ps1 = self.next_ps()
                kb.mm(ps1, ps1[:, :], [(blk, o[:, :])], r=(o,))
                kb.op('act', lambda e, o=o: e.activation(out=sq[:, :], in_=o[:, :], func=AF.Square), r=(o,), w=(sq,))
                ps2 = self.next_ps()
                kb.mm(ps2, ps2[:, :], [(blk, sq[:, :])], r=(sq,))
                kb.op('dve', lambda e, ps1=ps1: e.tensor_scalar(mean[:, :], ps1[:, :], 1.0 / 64, None, ALU.mult), r=(ps1,), w=(mean,))
                kb.op('dve', lambda e: e.tensor_tensor(msq[:, :], mean[:, :], mean[:, :], ALU.mult), r=(mean,), w=(msq,))
                kb.op('dve', lambda e, ps2=ps2: e.scalar_tensor_tensor(rstd[:, :], ps2[:, :], 1.0 / 64, msq[:, :], ALU.mult, ALU.subtract),
                      r=(ps2, msq), w=(rstd,))
                kb.op('act', lambda e: e.activation(out=rstd[:, :], in_=rstd[:, :], func=AF.Sqrt, bias=self.epsc[:, 2:3], scale=1.0),
                      r=(rstd,), w=(rstd,))
                kb.op('dve', lambda e: e.reciprocal(rstd[:, :], rstd[:, :]), r=(rstd,), w=(rstd,))
                kb.op('dve', lambda e, o=o: e.tensor_tensor(t1[:, :], o[:, :], mean[:, :], ALU.subtract), r=(o, mean), w=(t1,))
                kb.op('dve', lambda e: e.tensor_tensor(t1[:, :], t1[:, :], rstd[:, :], ALU.mult), r=(t1, rstd), w=(t1,))
                kb.op('dve', lambda e, c=c: e.tensor_scalar(t1[:, :], t1[:, :], col(ci('lng') + c), col(ci('lnb') + c), ALU.mult, ALU.add),
                      r=(t1,), w=(t1,))
                kb.op('dve', lambda e, b_=b_: e.tensor_tensor(t1[:, :], t1[:, :], b_[:, :], ALU.add), r=(t1, b_), w=(t1,))
                ob_ = ob[i % 2]
                kb.op('dve', lambda e, g_=g_, ob_=ob_: e.tensor_tensor(ob_[:, :], t1[:, :], g_[:, :], ALU.mult), r=(t1, g_), w=(ob_,))
                kb.dma('pool', self.Y[rows, ts_], ob_[:, :], r=(ob_,))
                i += 1

    def mixer_conv(self, l):
        kb = self.kb
        cw, cb, cg, cbb = (self.cidx[(k, l)] for k in ('cw', 'cb', 'cg', 'cbb'))
        col = lambda i: self.cols[:, i:i + 1]
        a_t = kb.sb("cv_a", [128, S], F32)
        b_t = kb.sb("cv_b", [128, S], F32)
        u = kb.sb("cv_u", [128, S + 32], F32)
        yc = [kb.sb(f"cv_y{i}", [128, S], F32) for i in range(2)]
        sq = kb.sb("cv_sq", [128, 2, 512], F32)
        mean = kb.sb("cv_mean", [128, 512], F32)
        msq = kb.sb("cv_msq", [128, 512], F32)
        rstd = kb.sb("cv_rstd", [128, 512], F32)
        t1 = [kb.sb(f"cv_t{i}", [128, 512], F32) for i in range(2)]
        ob = [kb.sb(f"cv_o{i}", [128, 512], BF16) for i in range(2)]
        kb.op('dve', lambda e: e.memset(u[:, 0:32], 0.0), w=(u,))
        for sq_i in range(self.NS):
            tb = sq_i * S
            for c in range(2):
                kb.dma('sp', a_t[:, :], self.P_cn[c * 128:(c + 1) * 128, tb:tb + S], w=(a_t,))
                kb.dma('sp', b_t[:, :], self.P_cn[256 + c * 128:256 + (c + 1) * 128, tb:tb + S], w=(b_t,))
                kb.op('act', lambda e: e.activation(out=b_t[:, :], in_=b_t[:, :], func=AF.Sigmoid), r=(b_t,), w=(b_t,))
                kb.op('dve', lambda e: e.tensor_tensor(u[:, 30:30 + S], a_t[:, :], b_t[:, :], ALU.mult), r=(a_t, b_t), w=(u,))
                y = yc[c]
                kb.op('dve', lambda e, y=y, c=c: e.tensor_scalar(y[:, :], u[:, 0:S], col(cw + c), col(cb + c), ALU.mult, ALU.add),
                      r=(u,), w=(y,))
                for j in range(1, 31):
                    kb.op('dve', lambda e, y=y, c=c, j=j: e.scalar_tensor_tensor(
                        y[:, :], u[:, j:j + S], col(cw + j * 2 + c), y[:, :], ALU.mult, ALU.add), r=(u, y), w=(y,))
            for n in range(S // 512):
                ts_ = slice(n * 512, (n + 1) * 512)
                ps1 = self.next_ps()
                kb.mm(ps1, ps1[:, :], [(self.cm[:, 128:256], yc[c][:, ts_]) for c in range(2)], r=(yc[0], yc[1]))
                kb.op('act', lambda e: e.activation(out=sq[:, 0, :], in_=yc[0][:, ts_], func=AF.Square), r=(yc[0],), w=(sq,))
                kb.op('act', lambda e: e.activation(out=sq[:, 1, :], in_=yc[1][:, ts_], func=AF.Square), r=(yc[1],), w=(sq,))
                ps2 = self.next_ps()
                kb.mm(ps2, ps2[:, :], [(self.cm[:, 128:256], sq[:, c, :]) for c in range(2)], r=(sq,))
                kb.op('dve', lambda e: e.tensor_scalar(mean[:, :], ps1[:, :], 1.0 / 256, None, ALU.mult), r=(ps1,), w=(mean,))
                kb.op('dve', lambda e: e.tensor_tensor(msq[:, :], mean[:, :], mean[:, :], ALU.mult), r=(mean,), w=(msq,))
                kb.op('dve', lambda e: e.scalar_tensor_tensor(rstd[:, :], ps2[:, :], 1.0 / 256, msq[:, :], ALU.mult, ALU.subtract),
                      r=(ps2, msq), w=(rstd,))
                kb.op('act', lambda e: e.activation(out=rstd[:, :], in_=rstd[:, :], func=AF.Sqrt, bias=self.epsc[:, 1:2], scale=1.0),
                      r=(rstd,), w=(rstd,))
                kb.op('dve', lambda e: e.reciprocal(rstd[:, :], rstd[:, :]), r=(rstd,), w=(rstd,))
                for c in range(2):
                    t, o = t1[c], ob[c]
                    kb.op('dve', lambda e, t=t, c=c: e.tensor_tensor(t[:, :], yc[c][:, ts_], mean[:, :], ALU.subtract),
                          r=(yc[c], mean), w=(t,))
                    kb.op('dve', lambda e, t=t, c=c: e.scalar_tensor_tensor(t[:, :], t[:, :], col(cg + c), rstd[:, :], ALU.mult, ALU.mult),
                          r=(t, rstd), w=(t,))
                    kb.op('act', lambda e, t=t, o=o, c=c: e.activation(out=o[:, :], in_=t[:, :], func=AF.Silu, bias=col(cbb + c)),
                          r=(t,), w=(o,))
                    kb.dma('pool', self.Y[768 + c * 128:768 + (c + 1) * 128, tb + n * 512:tb + (n + 1) * 512], o[:, :], r=(o,))

    def _load_qk(self, dst, src_rows, xtab_rows, nd, nx, stage, tb):
        kb = self.kb
        kb.dma('sp', dst[0:nd, :], src_rows[:, tb:tb + S], w=(dst,))
        kb.dma('sp', stage[nd:nd + nx, :], xtab_rows, w=(stage,))
        kb.op('dve', lambda e: e.tensor_copy(dst[nd:nd + nx, :], stage[nd:nd + nx, :]), r=(stage,), w=(dst,))

    def _make_vp(self, Vp, vin, blocks):
        kb = self.kb
        nb = len(blocks)
        for b0 in range(0, nb, 16):
            for j in range(16):
                kb.op('pe', lambda e, j=j, sl=blocks[b0 + j]: e.transpose(
                    self.psT[:, j * 64:(j + 1) * 64], vin[0:64, sl], self.identb[0:64, 0:64]), r=(vin,), w=(self.psT,))
            kb.op('dve', lambda e, b0=b0: e.tensor_copy(
                Vp[:, b0:b0 + 16, 0:64], self.psT[:, :].rearrange("p (b d) -> p b d", d=64)), r=(self.psT,), w=(Vp,))

    def mixer_diff(self, l):
        import math
        kb = self.kb
        slg = self.cidx[('slg', l)]
        Qp = [kb.sb(f"df_q{m}", [36, S], BF16) for m in range(2)]
        Kp = [kb.sb(f"df_k{m}", [36, S], BF16) for m in range(2)]
        stg = kb.sb("df_stg", [36, S], F32)
        vin = kb.sb("df_vin", [64, S], BF16)
        Vp = kb.sb("df_vp", [128, 32, 65], BF16)
        pt = [kb.sb(f"df_pt{i}", [128, 512], BF16) for i in range(3)]
        osb = [kb.sb(f"df_o{m}", [65, 512], F32) for m in range(2)]
        rb = [kb.sb(f"df_rb{m}", [64, 512], F32) for m in range(2)]
        yy = [kb.sb(f"df_y{m}", [64, 512], F32) for m in range(2)]
        att = kb.sb("df_att", [64, 512], F32)
        sq = kb.sb("df_sq", [64, 512], F32)
        rstd = kb.sb("df_rstd", [64, 512], F32)
        ob = [kb.sb(f"df_ob{i}", [64, 512], BF16) for i in range(2)]
        lamt = kb.sb("df_lamt", [128, 128], F32)
        lsm = kb.sb("df_lsm", [128, 8], F32)
        kb.op('dve', lambda e: e.memset(Vp[:, :, 64:65], 1.0), w=(Vp,))
        kb.dma('sp', lamt[:, :], self.lamv[l], w=(lamt,))
        kb.op('dve', lambda e: e.tensor_tensor(lamt[:, 0:32], lamt[:, 0:32], lamt[:, 32:64], ALU.mult), r=(lamt,), w=(lamt,))
        kb.op('dve', lambda e: e.tensor_tensor(lamt[:, 64:96], lamt[:, 64:96], lamt[:, 96:128], ALU.mult), r=(lamt,), w=(lamt,))
        kb.op('dve', lambda e: e.tensor_reduce(lsm[:, 0:1], lamt[:, 0:32], AX.X, ALU.add), r=(lamt,), w=(lsm,))
        kb.op('dve', lambda e: e.tensor_reduce(lsm[:, 1:2], lamt[:, 64:96], AX.X, ALU.add), r=(lamt,), w=(lsm,))
        kb.op('act', lambda e: e.activation(out=lsm[:, 2:4], in_=lsm[:, 0:2], func=AF.Exp), r=(lsm,), w=(lsm,))
        kb.op('dve', lambda e: e.tensor_tensor(lsm[:, 4:5], lsm[:, 3:4], lsm[:, 2:3], ALU.subtract), r=(lsm,), w=(lsm,))
        lmi, oml = self.cidx[('lmi', l)], self.cidx[('oml', l)]
        kb.op('dve', lambda e: e.tensor_tensor(lsm[:, 5:6], lsm[:, 4:5], self.cols[:, lmi:lmi + 1], ALU.add), r=(lsm,), w=(lsm,))
        kb.op('dve', lambda e: e.tensor_tensor(lsm[:, 6:7], self.cols[:, slg:slg + 1], self.cols[:, oml:oml + 1], ALU.mult),
              r=(lsm,), w=(lsm,))
        pi = 0
        oi = 0
        for si in range(self.NS):
            tb = si * S
            for h in range(4):
                for m in range(2):
                    r0 = (h * 2 + m) * 32
                    self._load_qk(Qp[m], self.P_cq[r0:r0 + 32], self.dxq[h * 4:(h + 1) * 4, :], 32, 4, stg, tb)
                    self._load_qk(Kp[m], self.P_ck[r0:r0 + 32], self.dxk[h * 4:(h + 1) * 4, :], 32, 4, stg, tb)
                kb.dma('sp', vin[:, :], self.P_cv[h * 64:(h + 1) * 64, tb:tb + S], w=(vin,))
                self._make_vp(Vp, vin, [slice(b * 128, (b + 1) * 128) for b in range(32)])
                for qt in range(S // 512):
                    q0 = qt * 512
                    nkb = 4 * qt + 4
                    for m in range(2):
                        acc = self.pacc[m]
                        for kbi in range(nkb):
                            md = kbi - 4 * qt
                            c0 = 128 * md if md > 0 else 0
                            ps = self.next_ps()
                            kb.mm(ps, ps[:, c0:512], [(Kp[m][:, kbi * 128:(kbi + 1) * 128], Qp[m][:, q0 + c0:q0 + 512])],
                                  r=(Kp[m], Qp[m]))
                            P = pt[pi % 3]
                            pi += 1
                            kb.op('act', lambda e, P=P, ps=ps, c0=c0: e.activation(out=P[:, c0:512], in_=ps[:, c0:512], func=AF.Exp),
                                  r=(ps,), w=(P,))
                            if md >= 0:
                                kb.op('dve', lambda e, P=P, c0=c0: e.tensor_tensor(
                                    P[:, c0:c0 + 128], P[:, c0:c0 + 128], self.mUL[:, 0:128], ALU.mult), r=(P,), w=(P,))
                            kb.mm1(acc, acc[0:65, c0:512], Vp[:, kbi, :], P[:, c0:512], start=(kbi == 0), stop=(kbi == nkb - 1),
                                   r=(Vp, P))
                        kb.op('act', lambda e, m=m, acc=acc: e.copy(osb[m][:, :], acc[0:65, :]), r=(acc,), w=(osb[m],))
                    for m in range(2):
                        bc = self.next_ps()
                        kb.mm(bc, bc[0:64, :], [(self.cm[0:65, 512:576], osb[m][0:65, :])], r=(osb[m],))
                        kb.op('dve', lambda e, m=m, bc=bc: e.reciprocal(rb[m][:, :], bc[0:64, :]), r=(bc,), w=(rb[m],))
                        kb.op('dve', lambda e, m=m: e.tensor_tensor(yy[m][:, :], osb[m][0:64, :], rb[m][:, :], ALU.mult),
                              r=(osb[m], rb[m]), w=(yy[m],))
                    kb.op('dve', lambda e: e.scalar_tensor_tensor(att[:, :], yy[1][:, :], lsm[0:64, 5:6], yy[0][:, :], ALU.mult, ALU.add),
                          r=(yy[0], yy[1], lsm), w=(att,))
                    kb.op('act', lambda e: e.activation(out=sq[:, :], in_=att[:, :], func=AF.Square), r=(att,), w=(sq,))
                    ss = self.next_ps()
                    kb.mm(ss, ss[0:64, :], [(self.cm[0:64, 128:192], sq[:, :])], r=(sq,))
                    kb.op('act', lambda e, ss=ss: e.activation(out=rstd[:, :], in_=ss[0:64, :], func=AF.Sqrt, bias=self.epsc[0:64, 1:2],
                                                               scale=1.0 / 64), r=(ss,), w=(rstd,))
                    kb.op('dve', lambda e: e.reciprocal(rstd[:, :], rstd[:, :]), r=(rstd,), w=(rstd,))
                    o = ob[oi % 2]
                    oi += 1
                    kb.op('dve', lambda e, o=o: e.scalar_tensor_tensor(o[:, :], att[:, :], lsm[0:64, 6:7], rstd[:, :], ALU.mult, ALU.mult),
                          r=(att, rstd, lsm), w=(o,))
                    kb.dma('pool', self.Y[512 + h * 64:512 + (h + 1) * 64, tb + q0:tb + q0 + 512], o[:, :], r=(o,))

    def mixer_dil(self, l):
        kb = self.kb
        Qp = kb.sb("dl_q", [72, S], BF16)
        Kp = kb.sb("dl_k", [72, S], BF16)
        stg = kb.sb("dl_stg", [72, S], F32)
        vin = kb.sb("dl_vin", [64, S], BF16)
        Vp = kb.sb("dl_vp", [128, 32, 65], BF16)
        Acc = kb.sb("dl_acc", [65, S], F32)
        pt = [kb.sb(f"dl_pt{i}", [128, 256], BF16) for i in range(3)]
        rb = kb.sb("dl_rb", [64, 512], F32)
        ob = [kb.sb(f"dl_ob{i}", [64, 512], BF16) for i in range(2)]
        kb.op('dve', lambda e: e.memset(Vp[:, :, 64:65], 1.0), w=(Vp,))
        pi = 0
        oi = 0
        for si in range(self.NS):
            tb = si * S
            for h in range(4):
                for g, (win, d) in enumerate(DIL_PAT):
                    gh = g * 4 + h
                    nb = S // d // 128
                    self._load_qk(Qp, self.P_dq[gh * 64:(gh + 1) * 64], self.dlq[gh * 8:(gh + 1) * 8, :], 64, 8, stg, tb)
                    self._load_qk(Kp, self.P_dk[gh * 64:(gh + 1) * 64], self.dlk[gh * 8:(gh + 1) * 8, :], 64, 8, stg, tb)
                    kb.dma('sp', vin[:, :], self.P_dv[gh * 64:(gh + 1) * 64, tb:tb + S], w=(vin,))
                    blk = lambda r, c: slice(r + d * 128 * c, r + d * 128 * c + d * 127 + 1, d)
                    blocks = [blk(r, c) for r in range(d) for c in range(nb)]
                    self._make_vp(Vp, vin, blocks)
                    for r in range(d):
                        for c in range(nb):
                            bi = r * nb + c
                            sc = blocks[bi]
                            ps = self.next_ps()
                            kb.mm(ps, ps[:, 0:128], [(Kp[:, sc], Qp[:, sc])], r=(Kp, Qp))
                            w_ = 128
                            if c > 0:
                                kb.mm(ps, ps[:, 128:256], [(Kp[:, blocks[bi - 1]], Qp[:, sc])], r=(Kp, Qp))
                                w_ = 256
                            P = pt[pi % 3]
                            pi += 1
                            kb.op('act', lambda e, P=P, ps=ps, w_=w_: e.activation(out=P[:, 0:w_], in_=ps[:, 0:w_], func=AF.Exp),
                                  r=(ps,), w=(P,))
                            kb.op('dve', lambda e, P=P, w_=w_: e.tensor_tensor(P[:, 0:w_], P[:, 0:w_], self.mUL[:, 0:w_], ALU.mult),
                                  r=(P,), w=(P,))
                            po = self.next_ps()
                            pairs = [(Vp[:, bi, :], P[:, 0:128])]
                            if c > 0:
                                pairs.append((Vp[:, bi - 1, :], P[:, 128:256]))
                            kb.mm(po, po[0:65, 0:128], pairs, r=(Vp, P))
                            if g == 0:
                                kb.op('act', lambda e, po=po, sc=sc: e.copy(Acc[:, sc], po[0:65, 0:128]), r=(po,), w=(Acc,))
                            else:
                                kb.op('dve', lambda e, po=po, sc=sc: e.tensor_tensor(Acc[:, sc], Acc[:, sc], po[0:65, 0:128], ALU.add),
                                      r=(po, Acc), w=(Acc,))
                for n in range(S // 512):
                    ts_ = slice(n * 512, (n + 1) * 512)
                    bc = self.next_ps()
                    kb.mm(bc, bc[0:64, :], [(self.cm[0:65, 512:576], Acc[0:65, ts_])], r=(Acc,))
                    kb.op('dve', lambda e, bc=bc: e.reciprocal(rb[:, :], bc[0:64, :]), r=(bc,), w=(rb,))
                    o = ob[oi % 2]
                    oi += 1
                    kb.op('dve', lambda e, o=o, ts_=ts_: e.tensor_tensor(o[:, :], Acc[0:64, ts_], rb[:, :], ALU.mult), r=(Acc, rb), w=(o,))
                    kb.dma('pool', self.Y[256 + h * 64:256 + (h + 1) * 64, tb + n * 512:tb + (n + 1) * 512], o[:, :], r=(o,))

    def _post_norm_add(self, xt, y, gcol0, sq, rstd, tmp):
        kb = self.kb
        kb.op('act', lambda e: e.activation(out=sq[:, :, :], in_=y[:, :, :], func=AF.Square), r=(y,), w=(sq,))
        ps = self.next_ps()
        kb.mm(ps, ps[:, :], [(self.cm[:, 128:256], sq[:, kc, :]) for kc in range(KC)], r=(sq, self.cm))
        kb.op('act', lambda e: e.activation(out=rstd[:, :], in_=ps[:, :], func=AF.Sqrt, bias=self.epsc[:, 0:1], scale=1.0 / D),
              r=(ps,), w=(rstd,))
        kb.op('dve', lambda e: e.reciprocal(rstd[:, :], rstd[:, :]), r=(rstd,), w=(rstd,))
        for kc in range(KC):
            kb.op('dve', lambda e, kc=kc: e.scalar_tensor_tensor(
                tmp[:, kc, :], y[:, kc, :], self.cols[:, gcol0 + kc:gcol0 + kc + 1], rstd[:, :],
                ALU.mult, ALU.mult), r=(y, rstd), w=(tmp,))
        kb.op('pool', lambda e: e.tensor_tensor(xt[:, :, :], xt[:, :, :], tmp[:, :, :], ALU.add), r=(tmp, xt), w=(xt,))

    def stage_merge(self, l):
        kb, NT = self.kb, self.NT
        wbr = kb.sb("mg_wbr", [128, 8, D], BF16)
        wo = kb.sb("mg_wo", [128, KC, D], BF16)
        kb.dma('sp', wbr[:, :, :], self.wb_br[l].rearrange("(c p) d -> p c d", p=128), w=(wbr,))
        kb.dma('sp', wo[:, :, :], self.wb_out[l].rearrange("(c p) d -> p c d", p=128), w=(wo,))
        yt = [kb.sb(f"mg_y{i}", [128, 8, 512], BF16) for i in range(2)]
        gt = [kb.sb(f"mg_g{i}", [128, 32, 512], BF16) for i in range(2)]
        xt = [kb.sb(f"mg_x{i}", [128, KC, 512], F32) for i in range(2)]
        acc = [kb.sb(f"mg_acc{i}", [128, 512], F32) for i in range(2)]
        tm = [kb.sb(f"mg_tm{i}", [128, 512], F32) for i in range(2)]
        mT = kb.sb("mg_m", [128, KC, 512], BF16)
        yo = kb.sb("mg_yo", [128, KC, 512], F32)
        sq = kb.sb("mg_sq", [128, KC, 512], F32)
        rstd = kb.sb("mg_rstd", [128, 512], F32)
        Yv = self.Y.rearrange("(c p) t -> p c t", p=128)
        Gv = self.P_g.rearrange("(c p) t -> p c t", p=128)
        xTv = self.xT.rearrange("(kc p) t -> p kc t", p=128)
        g0 = self.cidx[('nmo', l)]
        for n in range(NT // 512):
            ts_ = slice(n * 512, (n + 1) * 512)
            y, g, x = yt[n % 2], gt[n % 2], xt[n % 2]
            kb.dma('sp', y[:, :, :], Yv[:, :, ts_], w=(y,))
            for gq_ in range(4):
                kb.dma('sp', g[:, gq_ * 8:(gq_ + 1) * 8, :], Gv[:, gq_ * 8:(gq_ + 1) * 8, ts_], w=(g,))
            kb.dma('sp', x[:, :, :], xTv[:, :, ts_], w=(x,))
            for dc in range(KC):
                a = acc[dc % 2]
                for br in range(4):
                    ps = self.next_ps()
                    kb.mm(ps, ps[:, :], [(wbr[:, br * 2 + c, dc * 128:(dc + 1) * 128], y[:, br * 2 + c, :]) for c in range(2)],
                          r=(wbr, y))
                    if br == 0:
                        kb.op('dve', lambda e, a=a, ps=ps, g=g, br=br, dc=dc: e.tensor_tensor(
                            a[:, :], ps[:, :], g[:, br * 8 + dc, :], ALU.mult), r=(ps, g), w=(a,))
                    else:
                        t = tm[br % 2]
                        kb.op('dve', lambda e, t=t, ps=ps, g=g, br=br, dc=dc: e.tensor_tensor(
                            t[:, :], ps[:, :], g[:, br * 8 + dc, :], ALU.mult), r=(ps, g), w=(t,))
                        if br < 3:
                            kb.op('pool', lambda e, a=a, t=t: e.tensor_tensor(a[:, :], a[:, :], t[:, :], ALU.add), r=(t, a), w=(a,))
                        else:
                            kb.op('pool', lambda e, a=a, t=t, dc=dc: e.tensor_tensor(mT[:, dc, :], a[:, :], t[:, :], ALU.add),
                                  r=(t, a), w=(mT,))
            for oc in range(KC):
                ps = self.next_ps()
                kb.mm(ps, ps[:, :], [(wo[:, kc, oc * 128:(oc + 1) * 128], mT[:, kc, :]) for kc in range(KC)], r=(wo, mT))
                kb.op('act', lambda e, ps=ps, oc=oc: e.copy(yo[:, oc, :], ps[:, :]), r=(ps,), w=(yo,))
            self._post_norm_add(x, yo, g0, sq, rstd, sq)
            kb.dma('pool', xTv[:, :, ts_], x[:, :, :], r=(x,))

    def stage_ffn(self, l):
        kb, NT = self.kb, self.NT
        xt = [kb.sb(f"ff_x{i}", [128, KC, 512], F32) for i in range(2)]
        hT = kb.sb("ff_h", [128, KC, 512], BF16)
        aT = kb.sb("ff_a", [128, FC, 512], BF16)
        sq = kb.sb("ff_sq", [128, KC, 512], F32)
        yo = kb.sb("ff_yo", [128, KC, 512], F32)
        rstd = kb.sb("ff_rstd", [128, 512], F32)
        sg = [kb.sb(f"ff_sg{i}", [128, 512], F32) for i in range(2)]
        NW = 3
        wg = [kb.sb(f"ff_wg{i}", [128, KC, 256], BF16) for i in range(NW)]
        wu = [kb.sb(f"ff_wu{i}", [128, KC, 256], BF16) for i in range(NW)]
        wd = [kb.sb(f"ff_wd{i}", [128, FC, 256], BF16) for i in range(2)]
        xTv = self.xT.rearrange("(kc p) t -> p kc t", p=128)
        wgv = self.wb_g[l].rearrange("(kc p) n -> p kc n", p=128)
        wuv = self.wb_u[l].rearrange("(kc p) n -> p kc n", p=128)
        wdv = self.wb_d[l].rearrange("(kc p) n -> p kc n", p=128)
        gpre, gpost = self.cidx[('nfp', l)], self.cidx[('nfo', l)]
        wi = 0
        di = 0
        for n in range(NT // 512):
            ts_ = slice(n * 512, (n + 1) * 512)
            x = xt[n % 2]
            kb.dma('sp', x[:, :, :], xTv[:, :, ts_], w=(x,))
            self._norm_into(x, hT, 0, hT, gpre, sq, rstd)
            for f2 in range(FC // 2):
                g_, u_ = wg[wi % NW], wu[wi % NW]
                wi += 1
                kb.dma('sp', g_[:, :, :], wgv[:, :, f2 * 256:(f2 + 1) * 256], w=(g_,))
                kb.dma('sp', u_[:, :, :], wuv[:, :, f2 * 256:(f2 + 1) * 256], w=(u_,))
                for j in range(2):
                    fc = f2 * 2 + j
                    pg = self.next_ps()
                    kb.mm(pg, pg[:, :], [(g_[:, kc, j * 128:(j + 1) * 128], hT[:, kc, :]) for kc in range(KC)], r=(g_, hT))
                    pu = self.next_ps()
                    kb.mm(pu, pu[:, :], [(u_[:, kc, j * 128:(j + 1) * 128], hT[:, kc, :]) for kc in range(KC)], r=(u_, hT))
                    sgt = sg[fc % 2]
                    kb.op('act', lambda e, sgt=sgt, pg=pg: e.activation(out=sgt[:, :], in_=pg[:, :], func=AF.Silu), r=(pg,), w=(sgt,))
                    kb.op('dve', lambda e, sgt=sgt, pu=pu, fc=fc: e.tensor_tensor(aT[:, fc, :], sgt[:, :], pu[:, :], ALU.mult),
                          r=(sgt, pu), w=(aT,))
            for o2 in range(KC // 2):
                d_ = wd[di % 2]
                di += 1
                for f0, f1 in ((0, 8), (8, 15), (15, 22)):
                    kb.dma('sp', d_[:, f0:f1, :], wdv[:, f0:f1, o2 * 256:(o2 + 1) * 256], w=(d_,))
                for j in range(2):
                    oc = o2 * 2 + j
                    ps = self.next_ps()
                    kb.mm(ps, ps[:, :], [(d_[:, fc, j * 128:(j + 1) * 128], aT[:, fc, :]) for fc in range(FC)], r=(d_, aT))
                    kb.op('act', lambda e, ps=ps, oc=oc: e.copy(yo[:, oc, :], ps[:, :]), r=(ps,), w=(yo,))
            self._post_norm_add(x, yo, gpost, sq, rstd, sq)
            kb.dma('pool', xTv[:, :, ts_], x[:, :, :], r=(x,))


DIL_PAT = ((128, 1), (512, 4), (2048, 16))


def _bf16_round(v):
    import ml_dtypes
    return np.asarray(v, np.float32).astype(ml_dtypes.bfloat16).astype(np.float32)


def pos_tables():
    pos = np.arange(S)
    hi_, lo_ = (pos // 128 * 128).astype(np.float32), (pos % 128).astype(np.float32)
    one = np.ones(S, np.float32)
    dsl = 2.0 ** (-8.0 * np.arange(1, 5) / 4)
    dxq = np.zeros((16, S), np.float32)
    dxk = np.zeros((16, S), np.float32)
    for h in range(4):
        sl = np.float32(dsl[h])
        dxk[h * 4:(h + 1) * 4] = np.stack([hi_, lo_, -sl * one, -sl * one])
        dxq[h * 4:(h + 1) * 4] = np.stack([sl * one, sl * one, hi_, lo_])
    asl = 2.0 ** (-8.0 * np.arange(1, 13) / 12)
    dlq = np.zeros((96, S), np.float32)
    dlk = np.zeros((96, S), np.float32)
    for g, (win, d) in enumerate(DIL_PAT):
        Tt = pos // d
        th, tl = (Tt // 128 * 128).astype(np.float32), (Tt % 128).astype(np.float32)
        for h in range(4):
            gh = g * 4 + h
            sd = np.float32(asl[gh]) * np.float32(d)
            shi = _bf16_round(sd)
            slo = _bf16_round(np.float32(sd) - shi)
            dlk[gh * 8:(gh + 1) * 8] = np.stack([th, th, tl, tl, -shi * one, -slo * one, -shi * one, -slo * one])
            dlq[gh * 8:(gh + 1) * 8] = np.stack([shi * one, slo * one, shi * one, slo * one, th, th, tl, tl])
    return dxq, dxk, dlq, dlk


def make_inmaps(inp, L, NS, ncores, l0=0, x=None):
    inp = dict(inp)
    for k_ in list(inp.keys()):
        if k_ != 'x':
            inp[k_] = np.asarray(inp[k_])[l0:l0 + L]
    cols = build_cols(inp, L, l0)
    ii = np.arange(128)
    U = (ii[:, None] <= ii[None, :]).astype(np.float32)
    Lm = (ii[:, None] >= ii[None, :]).astype(np.float32)
    sel = np.zeros((128, 128), np.float32)
    sel[64, :] = 1.0
    blk = np.zeros((128, 128), np.float32)
    blk[0:64, 0:64] = 1.0
    blk[64:128, 64:128] = 1.0
    cm = np.concatenate([np.eye(128, dtype=np.float32), np.ones((128, 128), np.float32), U, Lm, sel, blk], axis=1)
    if x is None:
        x = inp['x']
    x = np.ascontiguousarray(np.asarray(x, np.float32)).reshape(-1, D)
    dxq, dxk, dlq, dlk = pos_tables()
    lamv = np.stack([np.broadcast_to(np.concatenate([inp['diff_lam_q1'][l], inp['diff_lam_k1'][l],
                                                      inp['diff_lam_q2'][l], inp['diff_lam_k2'][l]])[None, :], (128, 128))
                     for l in range(L)]).astype(np.float32)
    lamv = np.ascontiguousarray(lamv)
    maps = []
    for c in range(ncores):
        m = {
            "x": np.ascontiguousarray(x[c * NS * S:(c + 1) * NS * S]),
            "w_in": np.ascontiguousarray(inp['w_in'][:L]),
            "w_branch": np.ascontiguousarray(inp['w_branch'][:L]),
            "w_out": np.ascontiguousarray(inp['w_out'][:L]),
            "ffn_w_gate": np.ascontiguousarray(inp['ffn_w_gate'][:L]),
            "ffn_w_up": np.ascontiguousarray(inp['ffn_w_up'][:L]),
            "ffn_w_down": np.ascontiguousarray(inp['ffn_w_down'][:L]),
            "cols": cols,
            "cmats": cm,
            "dxq": dxq, "dxk": dxk, "dlq": dlq, "dlk": dlk, "lamv": lamv,
            "wa_up": np.ascontiguousarray(np.concatenate([inp['rwkv_w_up'][:L], inp['rwkv_a_up'][:L]], axis=1).astype(np.float32)),
            "g_up": np.ascontiguousarray(inp['rwkv_g_up'][:L].astype(np.float32)),
        }
        maps.append(m)
    return maps


def kernel(**inputs):
    inp = {k: np.asarray(v) for k, v in inputs.items()}
    prog = Prog(L=DEPTH, NS=2)
    nc = prog.build()
    maps = make_inmaps(inp, DEPTH, 2, 8)
    res = run_bass_kernel_spmd(nc, maps, core_ids=list(range(8)))
    x = np.concatenate([r["out"] for r in res.results], axis=0)
    return x.reshape(16, S, D).astype(np.float32)
```

```python
import numpy as np
import concourse.bass as bass
import concourse.mybir as mybir
from concourse.bass_utils import run_bass_kernel_spmd

F32 = mybir.dt.float32
BF16 = mybir.dt.bfloat16
AF = mybir.ActivationFunctionType
ALU = mybir.AluOpType
AX = mybir.AxisListType

D = 1024
S = 4096
DEPTH = 4
KC = 8
IN_COLS = 8704
D_FF = 2816
FC = 22
RW0, DIL0, DIF0, CONV0, GATE0 = 0, 1024, 3328, 4096, 4608
SEM_LIMIT = 30000
NCM = 768


class T:
    def __init__(self, h, const=False):
        self.h = h
        self.w = None
        self.rd = {}
        self.const = const

    def __getitem__(self, idx):
        return self.h[idx]


class Eng:
    def __init__(self, kb, name, e, is_pe=False):
        self.kb, self.name, self.e, self.is_pe = kb, name, e, is_pe
        self.gen = 0
        self.sem = kb.nc.alloc_semaphore(f"s_{name}_0")
        self.cnt = 0
        self.seen = {}
        self.last = None
        self.dsems = []
        self.di = 0

    def signal(self, ins):
        if self.cnt >= SEM_LIMIT:
            self.gen += 1
            self.sem = self.kb.nc.alloc_semaphore(f"s_{self.name}_{self.gen}")
            self.cnt = 0
        self.cnt += 1
        ins.then_inc(self.sem, 1)
        ev = (self.sem, self.cnt, self)
        self.last = ev
        return ev


class KB:
    def __init__(self, nc, ndma_sems=12):
        self.nc = nc
        self.eng = {
            'pe': Eng(self, 'pe', nc.tensor, True),
            'act': Eng(self, 'act', nc.scalar),
            'dve': Eng(self, 'dve', nc.vector),
            'pool': Eng(self, 'pool', nc.gpsimd),
            'sp': Eng(self, 'sp', nc.sync),
        }
        self.ndma = ndma_sems
        self.dma_latest = {}
        self.n_ins = 0
        self.uid = 0

    def sb(self, name, shape, dt, const=False):
        self.uid += 1
        return T(self.nc.alloc_sbuf_tensor(f"{name}_{self.uid}", list(shape), dt), const)

    def ps(self, name, shape=(128, 512), dt=F32):
        self.uid += 1
        return T(self.nc.alloc_psum_tensor(f"{name}_{self.uid}", list(shape), dt))

    def _wait(self, E, deps):
        best = {}
        for d in deps:
            if d is None:
                continue
            sem, val, src = d
            if src is E and E.is_pe:
                continue
            k = id(sem)
            if E.seen.get(k, 0) >= val:
                continue
            if k not in best or best[k][1] < val:
                best[k] = (sem, val)
        for k, (sem, val) in best.items():
            E.e.wait_ge(sem, val)
            E.seen[k] = val
            self.n_ins += 1

    def _deps(self, r, w):
        deps = []
        for t in r:
            if t.w is not None:
                deps.append(t.w)
        for t in w:
            if t.w is not None:
                deps.append(t.w)
            deps.extend(t.rd.values())
        return deps

    def _mark(self, ev, r, w):
        for t in w:
            t.w = ev
            t.rd = {}
        for t in r:
            if t.const or t in w:
                continue
            t.rd[id(ev[0])] = ev

    def op(self, en, fn, r=(), w=(), extra=()):
        E = self.eng[en]
        self._wait(E, self._deps(r, w) + list(extra))
        ins = fn(E.e)
        ev = E.signal(ins)
        self._mark(ev, r, w)
        self.n_ins += 1
        return ev

    def mm(self, out_t, out_ap, pairs, r=(), extra=()):
        E = self.eng['pe']
        self._wait(E, self._deps(r, (out_t,)) + list(extra))
        n = len(pairs)
        ins = None
        for i, (l, rh) in enumerate(pairs):
            ins = E.e.matmul(out_ap, l, rh, start=(i == 0), stop=(i == n - 1))
            self.n_ins += 1
        ev = E.signal(ins)
        self._mark(ev, r, (out_t,))
        return ev

    def mm1(self, out_t, out_ap, l, rh, start, stop, r=(), sig=True):
        E = self.eng['pe']
        self._wait(E, self._deps(r, (out_t,)))
        ins = E.e.matmul(out_ap, l, rh, start=start, stop=stop)
        self.n_ins += 1
        if not sig:
            return None
        ev = E.signal(ins)
        self._mark(ev, r, (out_t,))
        return ev

    def dma(self, qn, out, in_, r=(), w=(), extra=(), **kw):
        E = self.eng[qn]
        if len(E.dsems) < self.ndma:
            E.dsems.append([self.nc.alloc_semaphore(f"d_{qn}_{len(E.dsems)}_0"), 0, 0])
        slot = E.dsems[E.di % self.ndma]
        E.di += 1
        if slot[1] * 16 >= SEM_LIMIT:
            slot[2] += 1
            slot[0] = self.nc.alloc_semaphore(f"d_{qn}_{E.di % self.ndma}_{slot[2]}")
            slot[1] = 0
        prev = (slot[0], slot[1] * 16, None) if slot[1] > 0 else None
        self._wait(E, self._deps(r, w) + list(extra) + [prev])
        ins = E.e.dma_start(out=out, in_=in_, **kw)
        slot[1] += 1
        ins.then_inc(slot[0], 16)
        ev = (slot[0], slot[1] * 16, None)
        self.dma_latest[id(slot[0])] = ev
        self._mark(ev, r, w)
        self.n_ins += 1
        return ev

    def barrier(self):
        evs = [E.last for E in self.eng.values() if E.last is not None]
        evs += list(self.dma_latest.values())
        for E in self.eng.values():
            self._wait(E, [e for e in evs if not (e[2] is E and E.is_pe)])
        self.dma_latest = {}


def colblock(v):
    v = np.asarray(v, np.float32).reshape(-1)
    n = v.size // 128
    return np.ascontiguousarray(v.reshape(n, 128).T)


class ColTable:
    def __init__(self):
        self.parts, self.idx, self.n = [], {}, 0

    def add(self, name, v):
        b = colblock(v)
        self.idx[name] = self.n
        self.parts.append(b)
        self.n += b.shape[1]

    def build(self):
        return np.ascontiguousarray(np.concatenate(self.parts, axis=1))


def col_layout(L):
    idx, n = {}, 0
    for l in range(L):
        for nm, w in (('nmp', 8), ('nmo', 8), ('nfp', 8), ('nfo', 8), ('gb', 32), ('mu', 8), ('cw', 62), ('cb', 2), ('cg', 2), ('cbb', 2), ('slg', 1), ('w0', 2), ('a0', 2), ('kk', 2), ('ka', 2), ('rk', 2), ('lng', 2), ('lnb', 2), ('lmi', 1), ('oml', 1)):
            idx[(nm, l)] = n
            n += w
    return idx, n


def build_cols(inp, L, l0=0):
    import math
    ct = ColTable()
    for l in range(L):
        ct.add(('nmp', l), inp['norm_mix_pre'][l])
        ct.add(('nmo', l), inp['norm_mix_post'][l])
        ct.add(('nfp', l), inp['norm_ffn_pre'][l])
        ct.add(('nfo', l), inp['norm_ffn_post'][l])
        ct.add(('gb', l), inp['gate_bias'][l])
        ct.add(('mu', l), inp['rwkv_mu'][l])
        ct.add(('cw', l), inp['conv_dw_w'][l].reshape(-1))
        ct.add(('cb', l), inp['conv_dw_b'][l])
        ct.add(('cg', l), inp['conv_ln_g'][l])
        ct.add(('cbb', l), inp['conv_ln_b'][l])
        ct.add(('slg', l), np.concatenate([inp['diff_subln_g'][l], np.zeros(64, np.float32)]))
        ct.add(('w0', l), inp['rwkv_w0'][l])
        ct.add(('a0', l), inp['rwkv_a0'][l])
        ct.add(('kk', l), inp['rwkv_k_k'][l])
        ct.add(('ka', l), inp['rwkv_k_a'][l])
        ct.add(('rk', l), inp['rwkv_r_k'][l].reshape(-1))
        ct.add(('lng', l), inp['rwkv_ln_g'][l])
        ct.add(('lnb', l), inp['rwkv_ln_b'][l])
        li = 0.8 - 0.6 * math.exp(-0.3 * (l0 + l))
        ct.add(('lmi', l), np.full(128, -li, np.float32))
        ct.add(('oml', l), np.full(128, 1.0 - li, np.float32))
    return ct.build()


class Prog:
    def __init__(self, L=DEPTH, NS=2, debug=()):
        self.L, self.NS, self.NT = L, NS, NS * S
        self.debug = set(debug)
        nc = bass.Bass("TRN2", target_bir_lowering=False)
        self.nc = nc
        self.kb = KB(nc)
        NT = self.NT
        ein = lambda n, s, d=F32: nc.dram_tensor(n, list(s), d, kind="ExternalInput").ap()
        self.x = ein("x", [NT, D])
        self.w_in = ein("w_in", [L, D, IN_COLS])
        self.w_branch = ein("w_branch", [L, 4, 256, D])
        self.w_out = ein("w_out", [L, D, D])
        self.w_g = ein("ffn_w_gate", [L, D, D_FF])
        self.w_u = ein("ffn_w_up", [L, D, D_FF])
        self.w_d = ein("ffn_w_down", [L, D_FF, D])
        self.cidx, ncols = col_layout(L)
        self.cols_d = ein("cols", [128, ncols])
        self.cm_d = ein("cmats", [128, NCM])
        self.dxq = ein("dxq", [16, S])
        self.dxk = ein("dxk", [16, S])
        self.dlq = ein("dlq", [96, S])
        self.dlk = ein("dlk", [96, S])
        self.lamv = ein("lamv", [L, 128, 128])
        self.wa_up = ein("wa_up", [L, 128, 256])
        self.g_up = ein("g_up", [L, 128, 256])
        self.out = nc.dram_tensor("out", [NT, D], F32, kind="ExternalOutput").ap()
        self.ncols = ncols

    def scratch(self, name, shape, dt):
        kind = "ExternalOutput" if name in self.debug else "Internal"
        return self.nc.dram_tensor(name, list(shape), dt, kind=kind).ap()

    def build(self):
        nc, kb, L, NT = self.nc, self.kb, self.L, self.NT
        self.cols = kb.sb("cols", [128, self.ncols], F32, const=True)
        self.cm = kb.sb("cmats", [128, NCM], F32, const=True)
        self.identb = kb.sb("identb", [128, 128], BF16, const=True)
        kb.dma('sp', self.cols[:, :], self.cols_d[:, :], w=(self.cols,))
        kb.dma('sp', self.cm[:, :], self.cm_d[:, :], w=(self.cm,))
        kb.op('dve', lambda e: e.tensor_copy(self.identb[:, :], self.cm[:, 0:128]), r=(self.cm,), w=(self.identb,))
        self.ident = self.cm
        self.epsc = kb.sb("epsc", [128, 4], F32, const=True)
        kb.op('dve', lambda e: e.memset(self.epsc[:, 0:1], 1e-6), w=(self.epsc,))
        kb.op('dve', lambda e: e.memset(self.epsc[:, 1:2], 1e-5), w=(self.epsc,))
        kb.op('dve', lambda e: e.memset(self.epsc[:, 2:3], 64e-5), w=(self.epsc,))
        kb.op('dve', lambda e: e.memset(self.epsc[:, 3:4], 0.0), w=(self.epsc,))
        self.psb = [kb.ps(f"psb{i}") for i in range(4)]
        self.psi = 0
        self.pacc = [kb.ps(f"pacc{i}") for i in range(3)]
        self.psT = kb.ps("psT", (128, 1024), BF16)
        self.mUL = kb.sb("mUL", [128, 256], BF16, const=True)
        kb.op('dve', lambda e: e.tensor_copy(self.mUL[:, :], self.cm[:, 256:512]), r=(self.cm,), w=(self.mUL,))
        self.xT = self.scratch("xT", [D, NT], F32)
        self.wb_in = self.scratch("wb_in", [L, D, IN_COLS], BF16)
        self.wb_br = self.scratch("wb_br", [L, 1024, D], BF16)
        self.wb_out = self.scratch("wb_out", [L, D, D], BF16)
        self.wb_g = self.scratch("wb_g", [L, D, D_FF], BF16)
        self.wb_u = self.scratch("wb_u", [L, D, D_FF], BF16)
        self.wb_d = self.scratch("wb_d", [L, D_FF, D], BF16)
        self.P_rw = self.scratch("P_rw", [1024, NT], F32)
        self.P_dq = self.scratch("P_dq", [768, NT], BF16)
        self.P_dk = self.scratch("P_dk", [768, NT], BF16)
        self.P_dv = self.scratch("P_dv", [768, NT], BF16)
        self.P_cq = self.scratch("P_cq", [256, NT], BF16)
        self.P_ck = self.scratch("P_ck", [256, NT], BF16)
        self.P_cv = self.scratch("P_cv", [256, NT], BF16)
        self.P_cn = self.scratch("P_cn", [512, NT], F32)
        self.P_g = self.scratch("P_g", [4096, NT], BF16)
        self.Y = self.scratch("Y", [1024, NT], BF16)
        self.ROWS = self.scratch("ROWS", [S, 5, 2, 256], F32)
        self.Vf = self.scratch("Vf", [256, NT], F32)
        self.Gf = self.scratch("Gf", [256, NT], F32)
        self.BNVf = self.scratch("BNVf", [256, NT], F32)
        self.Of = self.scratch("Of", [256, NT], F32)

        sb0 = nc.sbuf_base
        self.stage_cast()
        kb.barrier()
        nc.sbuf_base = sb0
        self.stage_xin()
        kb.barrier()
        for l in range(L):
            nc.sbuf_base = sb0
            self.stage_proj(l)
            kb.barrier()
            nc.sbuf_base = sb0
            self.stage_mixers(l)
            kb.barrier()
            nc.sbuf_base = sb0
            self.stage_merge(l)
            kb.barrier()
            nc.sbuf_base = sb0
            self.stage_ffn(l)
            kb.barrier()
        nc.sbuf_base = sb0
        self.stage_xout()
        kb.barrier()
        return nc

    def next_ps(self):
        p = self.psb[self.psi % 4]
        self.psi += 1
        return p

    def stage_cast(self):
        kb, L = self.kb, self.L
        CW = 2176
        NB = 3
        fin = [kb.sb(f"cast_in{i}", [128, CW], F32) for i in range(NB)]
        fout = [kb.sb(f"cast_out{i}", [128, CW], BF16) for i in range(NB)]
        engs = ['act', 'dve', 'pool']
        jobs = []
        for l in range(L):
            jobs.append((self.wb_in[l], self.w_in[l], D, IN_COLS))
            jobs.append((self.wb_br[l], self.w_branch[l].rearrange("n c d -> (n c) d"), 1024, D))
            jobs.append((self.wb_out[l], self.w_out[l], D, D))
            jobs.append((self.wb_g[l], self.w_g[l], D, D_FF))
            jobs.append((self.wb_u[l], self.w_u[l], D, D_FF))
            jobs.append((self.wb_d[l], self.w_d[l], D_FF, D))
        i = 0
        for dst, src, rows, cols in jobs:
            for r0 in range(0, rows, 128):
                for c0 in range(0, cols, CW):
                    cw = min(CW, cols - c0)
                    a, b = fin[i % NB], fout[i % NB]
                    kb.dma('sp', a[:, 0:cw], src[r0:r0 + 128, c0:c0 + cw], w=(a,))
                    en = engs[i % 3]
                    if en == 'act':
                        kb.op('act', lambda e, a=a, b=b, cw=cw: e.copy(b[:, 0:cw], a[:, 0:cw]), r=(a,), w=(b,))
                    else:
                        kb.op(en, lambda e, a=a, b=b, cw=cw: e.tensor_copy(b[:, 0:cw], a[:, 0:cw]), r=(a,), w=(b,))
                    kb.dma('pool', dst[r0:r0 + 128, c0:c0 + cw], b[:, 0:cw], r=(b,))
                    i += 1

    def stage_xin(self):
        kb, NT = self.kb, self.NT
        NB = 2
        xin = [kb.sb(f"xin{i}", [128, 4, D], F32) for i in range(NB)]
        xo = [kb.sb(f"xo{i}", [128, KC, 512], F32) for i in range(NB)]
        xv = self.x.rearrange("(n j p) d -> n p j d", p=128, j=4)
        xTv = self.xT.rearrange("(kc p) t -> p kc t", p=128)
        for n in range(NT // 512):
            a, b = xin[n % NB], xo[n % NB]
            kb.dma('sp', a[:, :, :], xv[n], w=(a,))
            for kc in range(KC):
                ps = self.next_ps()
                for j in range(4):
                    kb.op('pe', lambda e, ps=ps, a=a, j=j, kc=kc: e.transpose(
                        ps[:, j * 128:(j + 1) * 128], a[:, j, kc * 128:(kc + 1) * 128], self.ident[:, 0:128]),
                        r=(a,), w=(ps,))
                en = 'act' if kc % 2 == 0 else 'dve'
                if en == 'act':
                    kb.op('act', lambda e, ps=ps, b=b, kc=kc: e.copy(b[:, kc, :], ps[:, :]), r=(ps,), w=(b,))
                else:
                    kb.op('dve', lambda e, ps=ps, b=b, kc=kc: e.tensor_copy(b[:, kc, :], ps[:, :]), r=(ps,), w=(b,))
            kb.dma('pool', xTv[:, :, n * 512:(n + 1) * 512], b[:, :, :], r=(b,))

    def stage_xout(self):
        kb, NT = self.kb, self.NT
        NB = 2
        xi = [kb.sb(f"xoi{i}", [128, KC, 512], F32) for i in range(NB)]
        xo = [kb.sb(f"xoo{i}", [128, 4, D], F32) for i in range(NB)]
        ov = self.out.rearrange("(n j p) d -> n p j d", p=128, j=4)
        xTv = self.xT.rearrange("(kc p) t -> p kc t", p=128)
        for n in range(NT // 512):
            a, b = xi[n % NB], xo[n % NB]
            kb.dma('sp', a[:, :, :], xTv[:, :, n * 512:(n + 1) * 512], w=(a,))
            for j in range(4):
                for h in range(2):
                    ps = self.next_ps()
                    for q in range(4):
                        kc = h * 4 + q
                        kb.op('pe', lambda e, ps=ps, a=a, j=j, kc=kc, q=q: e.transpose(
                            ps[:, q * 128:(q + 1) * 128], a[:, kc, j * 128:(j + 1) * 128], self.ident[:, 0:128]),
                            r=(a,), w=(ps,))
                    if h == 0:
                        kb.op('act', lambda e, ps=ps, b=b, j=j: e.copy(b[:, j, 0:512], ps[:, :]), r=(ps,), w=(b,))
                    else:
                        kb.op('dve', lambda e, ps=ps, b=b, j=j: e.tensor_copy(b[:, j, 512:1024], ps[:, :]), r=(ps,), w=(b,))
            kb.dma('pool', ov[n], b[:, :, :], r=(b,))

    def stage_proj(self, l):
        kb, NT = self.kb, self.NT
        TT = 2048
        NSUB = TT // 512
        xt = [kb.sb(f"pj_x{i}", [128, KC, 512], F32) for i in range(2)]
        sq = kb.sb("pj_sq", [128, KC, 512], F32)
        rstd = kb.sb("pj_rstd", [128, 512], F32)
        hT = kb.sb("pj_h", [128, KC, TT], BF16)
        hsub = [T(hT.h) for _ in range(NSUB)]
        NW = 3
        wt = [kb.sb(f"pj_w{i}", [128, KC, 128], BF16) for i in range(NW)]
        NO = 4
        ot32 = [kb.sb(f"pj_o32_{i}", [128, 512], F32) for i in range(NO)]
        ot16 = [kb.sb(f"pj_o16_{i}", [128, 512], BF16) for i in range(NO)]
        xTv = self.xT.rearrange("(kc p) t -> p kc t", p=128)
        wv = self.wb_in[l].rearrange("(kc p) n -> p kc n", p=128)
        gb0 = self.cidx[('gb', l)]
        oi = 0
        wi = 0
        for st in range(NT // TT):
            t0 = st * TT
            for s in range(NSUB):
                a = xt[s % 2]
                kb.dma('sp', a[:, :, :], xTv[:, :, t0 + s * 512:t0 + (s + 1) * 512], w=(a,))
                hs = hsub[s]
                self._norm_into(a, hT, s * 512, hs, self.cidx[('nmp', l)], sq, rstd)
            for oc in range(IN_COLS // 128):
                w = wt[wi % NW]
                wi += 1
                kb.dma('sp', w[:, :, :], wv[:, :, oc * 128:(oc + 1) * 128], w=(w,))
                c0 = oc * 128
                for s in range(NSUB):
                    ps = self.next_ps()
                    kb.mm(ps, ps[:, :], [(w[:, kc, :], hT[:, kc, s * 512:(s + 1) * 512]) for kc in range(KC)],
                          r=(w, hsub[s]))
                    tsl = slice(t0 + s * 512, t0 + (s + 1) * 512)
                    en = 'act' if oi % 2 == 0 else 'dve'
                    if c0 < DIL0:
                        o = ot32[oi % NO]
                        self._evac(en, o, ps, None)
                        dst = self.P_rw[c0:c0 + 128, tsl]
                    elif c0 < DIF0:
                        o = ot16[oi % NO]
                        cc = c0 - DIL0
                        if cc < 768:
                            self._evac(en, o, ps, 0.125)
                            dst = self.P_dq[cc:cc + 128, tsl]
                        elif cc < 1536:
                            self._evac(en, o, ps, None)
                            dst = self.P_dk[cc - 768:cc - 640, tsl]
                        else:
                            self._evac(en, o, ps, None)
                            dst = self.P_dv[cc - 1536:cc - 1408, tsl]
                    elif c0 < CONV0:
                        o = ot16[oi % NO]
                        cc = c0 - DIF0
                        if cc < 256:
                            self._evac(en, o, ps, 32.0 ** -0.5)
                            dst = self.P_cq[cc:cc + 128, tsl]
                        elif cc < 512:
                            self._evac(en, o, ps, None)
                            dst = self.P_ck[cc - 256:cc - 128, tsl]
                        else:
                            self._evac(en, o, ps, None)
                            dst = self.P_cv[cc - 512:cc - 384, tsl]
                    elif c0 < GATE0:
                        o = ot32[oi % NO]
                        self._evac(en, o, ps, None)
                        dst = self.P_cn[c0 - CONV0:c0 - CONV0 + 128, tsl]
                    else:
                        o = ot16[oi % NO]
                        gc = (c0 - GATE0) // 128
                        kb.op('act', lambda e, o=o, ps=ps, gc=gc: e.activation(
                            out=o[:, :], in_=ps[:, :], func=AF.Sigmoid, bias=self.cols[:, gb0 + gc:gb0 + gc + 1]),
                            r=(ps,), w=(o,))
                        dst = self.P_g[c0 - GATE0:c0 - GATE0 + 128, tsl]
                    kb.dma('pool', dst, o[:, :], r=(o,))
                    oi += 1

    def _evac(self, en, o, ps, scale):
        kb = self.kb
        if en == 'act':
            if scale is None:
                kb.op('act', lambda e: e.copy(o[:, :], ps[:, :]), r=(ps,), w=(o,))
            else:
                kb.op('act', lambda e: e.mul(o[:, :], ps[:, :], scale), r=(ps,), w=(o,))
        else:
            if scale is None:
                kb.op('dve', lambda e: e.tensor_copy(o[:, :], ps[:, :]), r=(ps,), w=(o,))
            else:
                kb.op('dve', lambda e: e.tensor_scalar(o[:, :], ps[:, :], scale, None, ALU.mult), r=(ps,), w=(o,))

    def _norm_into(self, xt, hT, off, htrk, gcol0, sq, rstd, n=512):
        kb = self.kb
        kb.op('act', lambda e: e.activation(out=sq[:, :, 0:n], in_=xt[:, :, 0:n], func=AF.Square), r=(xt,), w=(sq,))
        ps = self.next_ps()
        kb.mm(ps, ps[:, 0:n], [(self.cm[:, 128:256], sq[:, kc, 0:n]) for kc in range(KC)], r=(sq, self.cm))
        kb.op('act', lambda e: e.activation(out=rstd[:, 0:n], in_=ps[:, 0:n], func=AF.Sqrt, bias=self.epsc[:, 0:1], scale=1.0 / D),
              r=(ps,), w=(rstd,))
        kb.op('dve', lambda e: e.reciprocal(rstd[:, 0:n], rstd[:, 0:n]), r=(rstd,), w=(rstd,))
        for kc in range(KC):
            kb.op('dve', lambda e, kc=kc: e.scalar_tensor_tensor(
                hT[:, kc, off:off + n], xt[:, kc, 0:n], self.cols[:, gcol0 + kc:gcol0 + kc + 1], rstd[:, 0:n],
                ALU.mult, ALU.mult), r=(xt, rstd), w=(htrk,))

    def stage_mixers(self, l):
        nc = self.nc
        sb0 = nc.sbuf_base
        for fn in (self.mixer_conv, self.mixer_diff, self.mixer_dil, self.mixer_rwkv):
            nc.sbuf_base = sb0
            fn(l)
            self.kb.barrier()

    def mixer_rwkv(self, l):
        nc, kb = self.nc, self.kb
        sb0 = nc.sbuf_base
        self.rwkv_prelude(l)
        kb.barrier()
        nc.sbuf_base = sb0
        self.rwkv_scan(l)
        kb.barrier()
        nc.sbuf_base = sb0
        self.rwkv_post(l)

    def rwkv_prelude(self, l):
        kb = self.kb
        ci = lambda k: self.cidx[(k, l)]
        col = lambda i: self.cols[:, i:i + 1]
        blk = self.cm[:, 640:768]
        wa = kb.sb("rw_wa", [128, 256], F32)
        gup = kb.sb("rw_gup", [128, 256], F32)
        kb.dma('sp', wa[:, :], self.wa_up[l], w=(wa,))
        kb.dma('sp', gup[:, :], self.g_up[l], w=(gup,))
        pin = [kb.sb(f"rw_pin{i}", [128, 8, 513], F32) for i in range(2)]
        pm = kb.sb("rw_pm", [128, 8, 512], F32)
        tz = kb.sb("rw_tz", [128, 512], F32)
        sg = kb.sb("rw_sg", [128, 512], F32)
        Q = [kb.sb(f"rw_q{i}", [128, 2, 512], F32) for i in range(5)]
        Wq, NKK, Bq, K2, Rq = Q
        aq = kb.sb("rw_a", [128, 2, 512], F32)
        gq = kb.sb("rw_g", [128, 2, 512], F32)
        kkq = kb.sb("rw_kk", [128, 2, 512], F32)
        sq = kb.sb("rw_sq", [128, 2, 512], F32)
        rn = kb.sb("rw_rn", [128, 2, 512], F32)
        tq = kb.sb("rw_t", [128, 2, 512], F32)
        bnv = kb.sb("rw_bnv", [128, 2, 512], F32)
        vq = kb.sb("rw_v", [128, 2, 512], F32)
        rowb = [kb.sb(f"rw_rowb{i}", [128, 5, 256], F32) for i in range(2)]
        Pv = self.P_rw.rearrange("(c p) t -> p c t", p=128)
        fview = lambda dr: dr.rearrange("(c p) t -> p c t", p=128)
        ri = 0
        for si in range(self.NS):
            tb = si * S
            for n in range(S // 512):
                t0 = tb + n * 512
                p_ = pin[n % 2]
                if n == 0:
                    kb.op('dve', lambda e, p_=p_: e.memset(p_[:, :, 0:1], 0.0), w=(p_,))
                    kb.dma('sp', p_[:, :, 1:513], Pv[:, :, t0:t0 + 512], w=(p_,))
                else:
                    kb.dma('sp', p_[:, :, 0:513], Pv[:, :, t0 - 1:t0 + 512], w=(p_,))
                kb.op('pool', lambda e, p_=p_: e.tensor_tensor(pm[:, :, :], p_[:, :, 0:512], p_[:, :, 1:513], ALU.subtract),
                      r=(p_,), w=(pm,))
                for c in range(8):
                    kb.op('dve', lambda e, p_=p_, c=c: e.scalar_tensor_tensor(
                        pm[:, c, :], pm[:, c, :], col(ci('mu') + c), p_[:, c, 1:513], ALU.mult, ALU.add), r=(pm, p_), w=(pm,))
                kb.op('act', lambda e: e.copy(Rq[:, :, :], pm[:, 0:2, :]), r=(pm,), w=(Rq,))
                kb.op('act', lambda e: e.copy(vq[:, :, :], pm[:, 4:6, :]), r=(pm,), w=(vq,))
                kb.op('act', lambda e: e.activation(out=tz[0:64, :], in_=pm[0:64, 6, :], func=AF.Tanh), r=(pm,), w=(tz,))
                for c in range(2):
                    ps = self.next_ps()
                    kb.mm(ps, ps[:, :], [(wa[0:64, c * 128:(c + 1) * 128], tz[0:64, :])], r=(wa, tz))
                    kb.op('act', lambda e, ps=ps, c=c: e.activation(out=Wq[:, c, :], in_=ps[:, :], func=AF.Sigmoid, bias=col(ci('w0') + c)),
                          r=(ps,), w=(Wq,))
                kb.op('act', lambda e: e.activation(out=Wq[:, :, :], in_=Wq[:, :, :], func=AF.Exp, scale=-0.606531), r=(Wq,), w=(Wq,))
                for c in range(2):
                    ps = self.next_ps()
                    kb.mm(ps, ps[:, :], [(wa[64:128, c * 128:(c + 1) * 128], pm[64:128, 6, :])], r=(wa, pm))
                    kb.op('act', lambda e, ps=ps, c=c: e.activation(out=aq[:, c, :], in_=ps[:, :], func=AF.Sigmoid, bias=col(ci('a0') + c)),
                          r=(ps,), w=(aq,))
                kb.op('act', lambda e: e.activation(out=sg[:, :], in_=pm[:, 7, :], func=AF.Sigmoid), r=(pm,), w=(sg,))
                for c in range(2):
                    ps = self.next_ps()
                    kb.mm(ps, ps[:, :], [(gup[:, c * 128:(c + 1) * 128], sg[:, :])], r=(gup, sg))
                    kb.op('act', lambda e, ps=ps, c=c: e.copy(gq[:, c, :], ps[:, :]), r=(ps,), w=(gq,))
                for c in range(2):
                    kb.op('dve', lambda e, c=c: e.tensor_scalar(kkq[:, c, :], pm[:, 2 + c, :], col(ci('kk') + c), None, ALU.mult),
                          r=(pm,), w=(kkq,))
                kb.op('act', lambda e: e.activation(out=sq[:, :, :], in_=kkq[:, :, :], func=AF.Square), r=(kkq,), w=(sq,))
                for c in range(2):
                    ps = self.next_ps()
                    kb.mm(ps, ps[:, :], [(blk, sq[:, c, :])], r=(sq,))
                    kb.op('act', lambda e, ps=ps, c=c: e.activation(out=rn[:, c, :], in_=ps[:, :], func=AF.Sqrt), r=(ps,), w=(rn,))
                kb.op('dve', lambda e: e.tensor_scalar(rn[:, :, :], rn[:, :, :], 1e-12, None, ALU.max), r=(rn,), w=(rn,))
                kb.op('dve', lambda e: e.reciprocal(rn[:, :, :], rn[:, :, :]), r=(rn,), w=(rn,))
                kb.op('dve', lambda e: e.tensor_tensor(kkq[:, :, :], kkq[:, :, :], rn[:, :, :], ALU.mult), r=(kkq, rn), w=(kkq,))
                kb.op('act', lambda e: e.mul(NKK[:, :, :], kkq[:, :, :], -1.0), r=(kkq,), w=(NKK,))
                kb.op('dve', lambda e: e.tensor_tensor(Bq[:, :, :], kkq[:, :, :], aq[:, :, :], ALU.mult), r=(kkq, aq), w=(Bq,))
                for c in range(2):
                    kb.op('dve', lambda e, c=c: e.tensor_scalar(tq[:, c, :], aq[:, c, :], -1.0, col(ci('ka') + c), ALU.add, ALU.mult),
                          r=(aq,), w=(tq,))
                kb.op('dve', lambda e: e.scalar_tensor_tensor(K2[:, :, :], tq[:, :, :], 1.0, pm[:, 2:4, :], ALU.add, ALU.mult),
                      r=(tq, pm), w=(K2,))
                for c in range(2):
                    kb.op('dve', lambda e, c=c: e.scalar_tensor_tensor(tq[:, c, :], Rq[:, c, :], col(ci('rk') + c), K2[:, c, :],
                                                                       ALU.mult, ALU.mult), r=(Rq, K2, tq), w=(tq,))
                for c in range(2):
                    ps = self.next_ps()
                    kb.mm(ps, ps[:, :], [(blk, tq[:, c, :])], r=(tq,))
                    kb.op('dve', lambda e, ps=ps, c=c: e.tensor_tensor(bnv[:, c, :], ps[:, :], vq[:, c, :], ALU.mult),
                          r=(ps, vq), w=(bnv,))
                kb.dma('pool', fview(self.Vf)[:, :, t0:t0 + 512], vq[:, :, :], r=(vq,))
                kb.dma('pool', fview(self.Gf)[:, :, t0:t0 + 512], gq[:, :, :], r=(gq,))
                kb.dma('pool', fview(self.BNVf)[:, :, t0:t0 + 512], bnv[:, :, :], r=(bnv,))
                for j in range(4):
                    rb = rowb[ri % 2]
                    ri += 1
                    for q0 in range(0, 5, 2):
                        ps = self.next_ps()
                        nq = min(2, 5 - q0)
                        for qq in range(nq):
                            for c in range(2):
                                kb.op('pe', lambda e, ps=ps, qq=qq, c=c, q0=q0, j=j: e.transpose(
                                    ps[:, (qq * 2 + c) * 128:(qq * 2 + c + 1) * 128], Q[q0 + qq][:, c, j * 128:(j + 1) * 128],
                                    self.ident[:, 0:128]), r=(Q[q0 + qq],), w=(ps,))
                        kb.op('act' if q0 != 2 else 'dve',
                              (lambda e, ps=ps, rb=rb, q0=q0, nq=nq: e.copy(
                                  rb[:, q0:q0 + nq, :], ps[:, 0:nq * 256].rearrange("p (q f) -> p q f", f=256))) if q0 != 2 else
                              (lambda e, ps=ps, rb=rb, q0=q0, nq=nq: e.tensor_copy(
                                  rb[:, q0:q0 + nq, :], ps[:, 0:nq * 256].rearrange("p (q f) -> p q f", f=256))),
                              r=(ps,), w=(rb,))
                    tl = n * 512 + j * 128
                    kb.dma('pool', self.ROWS[tl:tl + 128, :, si, :], rb[:, :, :], r=(rb,))

    def rwkv_scan(self, l):
        kb = self.kb
        NS = self.NS
        P = 64 * NS
        St = kb.sb("rs_S", [P, 256], F32)
        tmp = [kb.sb(f"rs_tmp{i}", [P, 256], F32) for i in range(2)]
        tmp2 = [kb.sb(f"rs_tp{i}", [P, 256], F32) for i in range(2)]
        sa = [kb.sb(f"rs_sa{i}", [P, 4], F32) for i in range(2)]
        swb = [kb.sb(f"rs_sw{i}", [P, 256], F32) for i in range(2)]
        NBB = 8
        bt = [kb.sb(f"rs_bt{i}", [P, NBB, 5, 256], F32) for i in range(2)]
        VB = 64
        vv = [kb.sb(f"rs_vv{i}", [P, 4, VB], F32) for i in range(2)]
        oo = [kb.sb(f"rs_oo{i}", [P, 4, VB], F32) for i in range(2)]
        kb.op('dve', lambda e: e.memset(St[:, :], 0.0), w=(St,))
        v3 = lambda ap: ap.rearrange("p (h k) -> p h k", k=64)
        hview = lambda dr: dr.rearrange("(h p) t -> p h t", p=64)
        bi = 0
        for blk in range(S // VB):
            tB = blk * VB
            V_, O_ = vv[blk % 2], oo[blk % 2]
            for si in range(NS):
                kb.dma('sp', V_[si * 64:(si + 1) * 64, :, :], hview(self.Vf)[:, :, si * S + tB:si * S + tB + VB], w=(V_,))
            for sb_ in range(VB // NBB):
                t0 = tB + sb_ * NBB
                B_ = bt[bi % 2]
                bi += 1
                for si in range(NS):
                    kb.dma('sp', B_[si * 64:(si + 1) * 64, :, :, :], self.ROWS[t0:t0 + NBB, :, si, :].partition_broadcast(64), w=(B_,))
                for j in range(NBB):
                    tt = sb_ * NBB + j
                    X = lambda q: B_[:, j, q, :]
                    X3 = lambda q: B_[:, j, q, :].rearrange("p (h k) -> p h k", k=64)
                    tm, t2, s_ = tmp[tt % 2], tmp2[tt % 2], sa[tt % 2]
                    kb.op('pool', lambda e, t2=t2, X3=X3, V_=V_, tt=tt: e.tensor_tensor(
                        v3(t2[:, :]), X3(3), V_[:, :, tt:tt + 1].to_broadcast([P, 4, 64]), ALU.mult), r=(B_, V_), w=(t2,))
                    sw_ = swb[tt % 2]
                    kb.op('pool', lambda e, sw_=sw_, X=X: e.tensor_tensor(sw_[:, :], St[:, :], X(0), ALU.mult), r=(St, B_), w=(sw_,))
                    kb.op('pool', lambda e, sw_=sw_, t2=t2: e.tensor_tensor(sw_[:, :], sw_[:, :], t2[:, :], ALU.add), r=(sw_, t2), w=(sw_,))
                    kb.op('dve', lambda e, tm=tm, X=X: e.tensor_tensor(tm[:, :], St[:, :], X(1), ALU.mult), r=(St, B_), w=(tm,))
                    kb.op('dve', lambda e, tm=tm, s_=s_: e.tensor_reduce(s_[:, :], v3(tm[:, :]), AX.X, ALU.add), r=(tm,), w=(s_,))
                    kb.op('dve', lambda e, tm=tm, X3=X3, s_=s_: e.tensor_tensor(
                        v3(tm[:, :]), X3(2), s_[:, :].unsqueeze(2).to_broadcast([P, 4, 64]), ALU.mult), r=(B_, s_), w=(tm,))
                    kb.op('dve', lambda e, tm=tm, sw_=sw_: e.tensor_tensor(St[:, :], sw_[:, :], tm[:, :], ALU.add), r=(sw_, tm), w=(St,))
                    kb.op('dve', lambda e, tm=tm, X=X: e.tensor_tensor(tm[:, :], St[:, :], X(4), ALU.mult), r=(St, B_), w=(tm,))
                    kb.op('dve', lambda e, tm=tm, O_=O_, tt=tt: e.tensor_reduce(O_[:, :, tt], v3(tm[:, :]), AX.X, ALU.add),
                          r=(tm,), w=(O_,))
            for si in range(NS):
                kb.dma('pool', hview(self.Of)[:, :, si * S + tB:si * S + tB + VB], O_[si * 64:(si + 1) * 64, :, :], r=(O_,))

    def rwkv_post(self, l):
        kb = self.kb
        ci = lambda k: self.cidx[(k, l)]
        col = lambda i: self.cols[:, i:i + 1]
        blk = self.cm[:, 640:768]
        ot = [kb.sb(f"rp_o{i}", [128, 512], F32) for i in range(2)]
        bt_ = [kb.sb(f"rp_b{i}", [128, 512], F32) for i in range(2)]
        gt = [kb.sb(f"rp_g{i}", [128, 512], F32) for i in range(2)]
        sq = kb.sb("rp_sq", [128, 512], F32)
        mean = kb.sb("rp_mean", [128, 512], F32)
        msq = kb.sb("rp_msq", [128, 512], F32)
        rstd = kb.sb("rp_rstd", [128, 512], F32)
        t1 = kb.sb("rp_t1", [128, 512], F32)
        ob = [kb.sb(f"rp_ob{i}", [128, 512], BF16) for i in range(2)]
        i = 0
        for c in range(2):
            for n in range(self.NT // 512):
                ts_ = slice(n * 512, (n + 1) * 512)
                o, b_, g_ = ot[i % 2], bt_[i % 2], gt[i % 2]
                rows = slice(c * 128, (c + 1) * 128)
                kb.dma('sp', o[:, :], self.Of[rows, ts_], w=(o,))
                kb.dma('sp', b_[:, :], self.BNVf[rows, ts_], w=(b_,))
                kb.dma('sp', g_[:, :], self.Gf[rows, ts_], w=(g_,))
                ps1 = self.next_ps()
                kb.mm(ps1, ps1[:, :], [(blk, o[:, :])], r=(o,))
                kb.op('act', lambda e, o=o: e.activation(out=sq[:, :], in_=o[:, :], func=AF.Square), r=(o,), w=(sq,))
                ps2 = self.next_ps()
                kb.mm(ps2, ps2[:, :], [(blk, sq[:, :])], r=(sq,))
                kb.op('dve', lambda e, ps1=ps1: e.tensor_scalar(mean[:, :], ps1[:, :], 1.0 / 64, None, ALU.mult), r=(ps1,), w=(mean,))
                kb.op('dve', lambda e: e.tensor_tensor(msq[:, :], mean[:, :], mean[:, :], ALU.mult), r=(mean,), w=(msq,))
                kb.op('dve', lambda e, ps2=ps2: e.scalar_tensor_tensor(rstd[:, :], ps2[:, :], 1.0 / 64, msq[:, :], ALU.mult, ALU.subtract),
                      r=(ps2, msq), w=(rstd,))
                kb.op('act', lambda e: e.activation(out=rstd[:, :], in_=rstd[:, :], func=AF.Sqrt, bias=self.epsc[:, 2:3], scale=1.0),
                      r=(rstd,), w=(rstd,))
                kb.op('dve', lambda e: e.reciprocal(rstd[:, :], rstd[:, :]), r=(rstd,), w=(rstd,))
                kb.op('dve', lambda e, o=o: e.tensor_tensor(t1[:, :], o[:, :], mean[:, :], ALU.subtract), r=(o, mean), w=(t1,))
                kb.op('dve', lambda e: e.tensor_tensor(t1[:, :], t1[:, :], rstd[:, :], ALU.mult), r=(t1, rstd), w=(t1,))
                kb.op('dve', lambda e, c=c: e.tensor_scalar(t1[:, :], t1[:, :], col(ci('lng') + c), col(ci('lnb') + c), ALU.mult, ALU.add),
                      r=(t1,), w=(t1,))
                kb.op('dve', lambda e, b_=b_: e.tensor_tensor(t1[:, :], t1[:, :], b_[:, :], ALU.add), r=(t1, b_), w=(t1,))
                ob_ = ob[i % 2]
                kb.op('dve', lambda e, g_=g_, ob_=ob_: e.tensor_tensor(ob_[:, :], t1[:, :], g_[:, :], ALU.mult), r=(t1, g_), w=(ob_,))
                kb.dma('pool', self.Y[rows, ts_], ob_[:, :], r=(ob_,))
                i += 1

    def mixer_conv(self, l):
        kb = self.kb
        cw, cb, cg, cbb = (self.cidx[(k, l)] for k in ('cw', 'cb', 'cg', 'cbb'))
        col = lambda i: self.cols[:, i:i + 1]
        a_t = kb.sb("cv_a", [128, S], F32)
        b_t = kb.sb("cv_b", [128, S], F32)
        u = kb.sb("cv_u", [128, S + 32], F32)
        yc = [kb.sb(f"cv_y{i}", [128, S], F32) for i in range(2)]
        sq = kb.sb("cv_sq", [128, 2, 512], F32)
        mean = kb.sb("cv_mean", [128, 512], F32)
        msq = kb.sb("cv_msq", [128, 512], F32)
        rstd = kb.sb("cv_rstd", [128, 512], F32)
        t1 = [kb.sb(f"cv_t{i}", [128, 512], F32) for i in range(2)]
        ob = [kb.sb(f"cv_o{i}", [128, 512], BF16) for i in range(2)]
        kb.op('dve', lambda e: e.memset(u[:, 0:32], 0.0), w=(u,))
        for sq_i in range(self.NS):
            tb = sq_i * S
            for c in range(2):
                kb.dma('sp', a_t[:, :], self.P_cn[c * 128:(c + 1) * 128, tb:tb + S], w=(a_t,))
                kb.dma('sp', b_t[:, :], self.P_cn[256 + c * 128:256 + (c + 1) * 128, tb:tb + S], w=(b_t,))
                kb.op('act', lambda e: e.activation(out=b_t[:, :], in_=b_t[:, :], func=AF.Sigmoid), r=(b_t,), w=(b_t,))
                kb.op('dve', lambda e: e.tensor_tensor(u[:, 30:30 + S], a_t[:, :], b_t[:, :], ALU.mult), r=(a_t, b_t), w=(u,))
                y = yc[c]
                kb.op('dve', lambda e, y=y, c=c: e.tensor_scalar(y[:, :], u[:, 0:S], col(cw + c), col(cb + c), ALU.mult, ALU.add),
                      r=(u,), w=(y,))
                for j in range(1, 31):
                    kb.op('dve', lambda e, y=y, c=c, j=j: e.scalar_tensor_tensor(
                        y[:, :], u[:, j:j + S], col(cw + j * 2 + c), y[:, :], ALU.mult, ALU.add), r=(u, y), w=(y,))
            for n in range(S // 512):
                ts_ = slice(n * 512, (n + 1) * 512)
                ps1 = self.next_ps()
                kb.mm(ps1, ps1[:, :], [(self.cm[:, 128:256], yc[c][:, ts_]) for c in range(2)], r=(yc[0], yc[1]))
                kb.op('act', lambda e: e.activation(out=sq[:, 0, :], in_=yc[0][:, ts_], func=AF.Square), r=(yc[0],), w=(sq,))
                kb.op('act', lambda e: e.activation(out=sq[:, 1, :], in_=yc[1][:, ts_], func=AF.Square), r=(yc[1],), w=(sq,))
                ps2 = self.next_ps()
                kb.mm(ps2, ps2[:, :], [(self.cm[:, 128:256], sq[:, c, :]) for c in range(2)], r=(sq,))
                kb.op('dve', lambda e: e.tensor_scalar(mean[:, :], ps1[:, :], 1.0 / 256, None, ALU.mult), r=(ps1,), w=(mean,))
                kb.op('dve', lambda e: e.tensor_tensor(msq[:, :], mean[:, :], mean[:, :], ALU.mult), r=(mean,), w=(msq,))
                kb.op('dve', lambda e: e.scalar_tensor_tensor(rstd[:, :], ps2[:, :], 1.0 / 256, msq[:, :], ALU.mult, ALU.subtract),
                      r=(ps2, msq), w=(rstd,))
                kb.op('act', lambda e: e.activation(out=rstd[:, :], in_=rstd[:, :], func=AF.Sqrt, bias=self.epsc[:, 1:2], scale=1.0),
                      r=(rstd,), w=(rstd,))
                kb.op('dve', lambda e: e.reciprocal(rstd[:, :], rstd[:, :]), r=(rstd,), w=(rstd,))
                for c in range(2):
                    t, o = t1[c], ob[c]
                    kb.op('dve', lambda e, t=t, c=c: e.tensor_tensor(t[:, :], yc[c][:, ts_], mean[:, :], ALU.subtract),
                          r=(yc[c], mean), w=(t,))
                    kb.op('dve', lambda e, t=t, c=c: e.scalar_tensor_tensor(t[:, :], t[:, :], col(cg + c), rstd[:, :], ALU.mult, ALU.mult),
                          r=(t, rstd), w=(t,))
                    kb.op('act', lambda e, t=t, o=o, c=c: e.activation(out=o[:, :], in_=t[:, :], func=AF.Silu, bias=col(cbb + c)),
                          r=(t,), w=(o,))
                    kb.dma('pool', self.Y[768 + c * 128:768 + (c + 1) * 128, tb + n * 512:tb + (n + 1) * 512], o[:, :], r=(o,))

    def _load_qk(self, dst, src_rows, xtab_rows, nd, nx, stage, tb):
        kb = self.kb
        kb.dma('sp', dst[0:nd, :], src_rows[:, tb:tb + S], w=(dst,))
        kb.dma('sp', stage[nd:nd + nx, :], xtab_rows, w=(stage,))
        kb.op('dve', lambda e: e.tensor_copy(dst[nd:nd + nx, :], stage[nd:nd + nx, :]), r=(stage,), w=(dst,))

    def _make_vp(self, Vp, vin, blocks):
        kb = self.kb
        nb = len(blocks)
        for b0 in range(0, nb, 16):
            for j in range(16):
                kb.op('pe', lambda e, j=j, sl=blocks[b0 + j]: e.transpose(
                    self.psT[:, j * 64:(j + 1) * 64], vin[0:64, sl], self.identb[0:64, 0:64]), r=(vin,), w=(self.psT,))
            kb.op('dve', lambda e, b0=b0: e.tensor_copy(
                Vp[:, b0:b0 + 16, 0:64], self.psT[:, :].rearrange("p (b d) -> p b d", d=64)), r=(self.psT,), w=(Vp,))

    def mixer_diff(self, l):
        import math
        kb = self.kb
        slg = self.cidx[('slg', l)]
        Qp = [kb.sb(f"df_q{m}", [36, S], BF16) for m in range(2)]
        Kp = [kb.sb(f"df_k{m}", [36, S], BF16) for m in range(2)]
        stg = kb.sb("df_stg", [36, S], F32)
        vin = kb.sb("df_vin", [64, S], BF16)
        Vp = kb.sb("df_vp", [128, 32, 65], BF16)
        pt = [kb.sb(f"df_pt{i}", [128, 512], BF16) for i in range(3)]
        osb = [kb.sb(f"df_o{m}", [65, 512], F32) for m in range(2)]
        rb = [kb.sb(f"df_rb{m}", [64, 512], F32) for m in range(2)]
        yy = [kb.sb(f"df_y{m}", [64, 512], F32) for m in range(2)]
        att = kb.sb("df_att", [64, 512], F32)
        sq = kb.sb("df_sq", [64, 512], F32)
        rstd = kb.sb("df_rstd", [64, 512], F32)
        ob = [kb.sb(f"df_ob{i}", [64, 512], BF16) for i in range(2)]
        lamt = kb.sb("df_lamt", [128, 128], F32)
        lsm = kb.sb("df_lsm", [128, 8], F32)
        kb.op('dve', lambda e: e.memset(Vp[:, :, 64:65], 1.0), w=(Vp,))
        kb.dma('sp', lamt[:, :], self.lamv[l], w=(lamt,))
        kb.op('dve', lambda e: e.tensor_tensor(lamt[:, 0:32], lamt[:, 0:32], lamt[:, 32:64], ALU.mult), r=(lamt,), w=(lamt,))
        kb.op('dve', lambda e: e.tensor_tensor(lamt[:, 64:96], lamt[:, 64:96], lamt[:, 96:128], ALU.mult), r=(lamt,), w=(lamt,))
        kb.op('dve', lambda e: e.tensor_reduce(lsm[:, 0:1], lamt[:, 0:32], AX.X, ALU.add), r=(lamt,), w=(lsm,))
        kb.op('dve', lambda e: e.tensor_reduce(lsm[:, 1:2], lamt[:, 64:96], AX.X, ALU.add), r=(lamt,), w=(lsm,))
        kb.op('act', lambda e: e.activation(out=lsm[:, 2:4], in_=lsm[:, 0:2], func=AF.Exp), r=(lsm,), w=(lsm,))
        kb.op('dve', lambda e: e.tensor_tensor(lsm[:, 4:5], lsm[:, 3:4], lsm[:, 2:3], ALU.subtract), r=(lsm,), w=(lsm,))
        lmi, oml = self.cidx[('lmi', l)], self.cidx[('oml', l)]
        kb.op('dve', lambda e: e.tensor_tensor(lsm[:, 5:6], lsm[:, 4:5], self.cols[:, lmi:lmi + 1], ALU.add), r=(lsm,), w=(lsm,))
        kb.op('dve', lambda e: e.tensor_tensor(lsm[:, 6:7], self.cols[:, slg:slg + 1], self.cols[:, oml:oml + 1], ALU.mult),
              r=(lsm,), w=(lsm,))
        pi = 0
        oi = 0
        for si in range(self.NS):
            tb = si * S
            for h in range(4):
                for m in range(2):
                    r0 = (h * 2 + m) * 32
                    self._load_qk(Qp[m], self.P_cq[r0:r0 + 32], self.dxq[h * 4:(h + 1) * 4, :], 32, 4, stg, tb)
                    self._load_qk(Kp[m], self.P_ck[r0:r0 + 32], self.dxk[h * 4:(h + 1) * 4, :], 32, 4, stg, tb)
                kb.dma('sp', vin[:, :], self.P_cv[h * 64:(h + 1) * 64, tb:tb + S], w=(vin,))
                self._make_vp(Vp, vin, [slice(b * 128, (b + 1) * 128) for b in range(32)])
                for qt in range(S // 512):
                    q0 = qt * 512
                    nkb = 4 * qt + 4
                    for m in range(2):
                        acc = self.pacc[m]
                        for kbi in range(nkb):
                            md = kbi - 4 * qt
                            c0 = 128 * md if md > 0 else 0
                            ps = self.next_ps()
                            kb.mm(ps, ps[:, c0:512], [(Kp[m][:, kbi * 128:(kbi + 1) * 128], Qp[m][:, q0 + c0:q0 + 512])],
                                  r=(Kp[m], Qp[m]))
                            P = pt[pi % 3]
                            pi += 1
                            kb.op('act', lambda e, P=P, ps=ps, c0=c0: e.activation(out=P[:, c0:512], in_=ps[:, c0:512], func=AF.Exp),
                                  r=(ps,), w=(P,))
                            if md >= 0:
                                kb.op('dve', lambda e, P=P, c0=c0: e.tensor_tensor(
                                    P[:, c0:c0 + 128], P[:, c0:c0 + 128], self.mUL[:, 0:128], ALU.mult), r=(P,), w=(P,))
                            kb.mm1(acc, acc[0:65, c0:512], Vp[:, kbi, :], P[:, c0:512], start=(kbi == 0), stop=(kbi == nkb - 1),
                                   r=(Vp, P))
                        kb.op('act', lambda e, m=m, acc=acc: e.copy(osb[m][:, :], acc[0:65, :]), r=(acc,), w=(osb[m],))
                    for m in range(2):
                        bc = self.next_ps()
                        kb.mm(bc, bc[0:64, :], [(self.cm[0:65, 512:576], osb[m][0:65, :])], r=(osb[m],))
                        kb.op('dve', lambda e, m=m, bc=bc: e.reciprocal(rb[m][:, :], bc[0:64, :]), r=(bc,), w=(rb[m],))
                        kb.op('dve', lambda e, m=m: e.tensor_tensor(yy[m][:, :], osb[m][0:64, :], rb[m][:, :], ALU.mult),
                              r=(osb[m], rb[m]), w=(yy[m],))
                    kb.op('dve', lambda e: e.scalar_tensor_tensor(att[:, :], yy[1][:, :], lsm[0:64, 5:6], yy[0][:, :], ALU.mult, ALU.add),
                          r=(yy[0], yy[1], lsm), w=(att,))
                    kb.op('act', lambda e: e.activation(out=sq[:, :], in_=att[:, :], func=AF.Square), r=(att,), w=(sq,))
                    ss = self.next_ps()
                    kb.mm(ss, ss[0:64, :], [(self.cm[0:64, 128:192], sq[:, :])], r=(sq,))
                    kb.op('act', lambda e, ss=ss: e.activation(out=rstd[:, :], in_=ss[0:64, :], func=AF.Sqrt, bias=self.epsc[0:64, 1:2],
                                                               scale=1.0 / 64), r=(ss,), w=(rstd,))
                    kb.op('dve', lambda e: e.reciprocal(rstd[:, :], rstd[:, :]), r=(rstd,), w=(rstd,))
                    o = ob[oi % 2]
                    oi += 1
                    kb.op('dve', lambda e, o=o: e.scalar_tensor_tensor(o[:, :], att[:, :], lsm[0:64, 6:7], rstd[:, :], ALU.mult, ALU.mult),
                          r=(att, rstd, lsm), w=(o,))
                    kb.dma('pool', self.Y[512 + h * 64:512 + (h + 1) * 64, tb + q0:tb + q0 + 512], o[:, :], r=(o,))

    def mixer_dil(self, l):
        kb = self.kb
        Qp = kb.sb("dl_q", [72, S], BF16)
        Kp = kb.sb("dl_k", [72, S], BF16)
        stg = kb.sb("dl_stg", [72, S], F32)
        vin = kb.sb("dl_vin", [64, S], BF16)
        Vp = kb.sb("dl_vp", [128, 32, 65], BF16)
        Acc = kb.sb("dl_acc", [65, S], F32)
        pt = [kb.sb(f"dl_pt{i}", [128, 256], BF16) for i in range(3)]
        rb = kb.sb("dl_rb", [64, 512], F32)
        ob = [kb.sb(f"dl_ob{i}", [64, 512], BF16) for i in range(2)]
        kb.op('dve', lambda e: e.memset(Vp[:, :, 64:65], 1.0), w=(Vp,))
        pi = 0
        oi = 0
        for si in range(self.NS):
            tb = si * S
            for h in range(4):
                for g, (win, d) in enumerate(DIL_PAT):
                    gh = g * 4 + h
                    nb = S // d // 128
                    self._load_qk(Qp, self.P_dq[gh * 64:(gh + 1) * 64], self.dlq[gh * 8:(gh + 1) * 8, :], 64, 8, stg, tb)
                    self._load_qk(Kp, self.P_dk[gh * 64:(gh + 1) * 64], self.dlk[gh * 8:(gh + 1) * 8, :], 64, 8, stg, tb)
                    kb.dma('sp', vin[:, :], self.P_dv[gh * 64:(gh + 1) * 64, tb:tb + S], w=(vin,))
                    blk = lambda r, c: slice(r + d * 128 * c, r + d * 128 * c + d * 127 + 1, d)
                    blocks = [blk(r, c) for r in range(d) for c in range(nb)]
                    self._make_vp(Vp, vin, blocks)
                    for r in range(d):
                        for c in range(nb):
                            bi = r * nb + c
                            sc = blocks[bi]
                            ps = self.next_ps()
                            kb.mm(ps, ps[:, 0:128], [(Kp[:, sc], Qp[:, sc])], r=(Kp, Qp))
                            w_ = 128
                            if c > 0:
                                kb.mm(ps, ps[:, 128:256], [(Kp[:, blocks[bi - 1]], Qp[:, sc])], r=(Kp, Qp))
                                w_ = 256
                            P = pt[pi % 3]
                            pi += 1
                            kb.op('act', lambda e, P=P, ps=ps, w_=w_: e.activation(out=P[:, 0:w_], in_=ps[:, 0:w_], func=AF.Exp),
                                  r=(ps,), w=(P,))
                            kb.op('dve', lambda e, P=P, w_=w_: e.tensor_tensor(P[:, 0:w_], P[:, 0:w_], self.mUL[:, 0:w_], ALU.mult),
                                  r=(P,), w=(P,))
                            po = self.next_ps()
                            pairs = [(Vp[:, bi, :], P[:, 0:128])]
                            if c > 0:
                                pairs.append((Vp[:, bi - 1, :], P[:, 128:256]))
                            kb.mm(po, po[0:65, 0:128], pairs, r=(Vp, P))
                            if g == 0:
                                kb.op('act', lambda e, po=po, sc=sc: e.copy(Acc[:, sc], po[0:65, 0:128]), r=(po,), w=(Acc,))
                            else:
                                kb.op('dve', lambda e, po=po, sc=sc: e.tensor_tensor(Acc[:, sc], Acc[:, sc], po[0:65, 0:128], ALU.add),
                                      r=(po, Acc), w=(Acc,))
                for n in range(S // 512):
                    ts_ = slice(n * 512, (n + 1) * 512)
                    bc = self.next_ps()
                    kb.mm(bc, bc[0:64, :], [(self.cm[0:65, 512:576], Acc[0:65, ts_])], r=(Acc,))
                    kb.op('dve', lambda e, bc=bc: e.reciprocal(rb[:, :], bc[0:64, :]), r=(bc,), w=(rb,))
                    o = ob[oi % 2]
                    oi += 1
                    kb.op('dve', lambda e, o=o, ts_=ts_: e.tensor_tensor(o[:, :], Acc[0:64, ts_], rb[:, :], ALU.mult), r=(Acc, rb), w=(o,))
                    kb.dma('pool', self.Y[256 + h * 64:256 + (h + 1) * 64, tb + n * 512:tb + (n + 1) * 512], o[:, :], r=(o,))

    def _post_norm_add(self, xt, y, gcol0, sq, rstd, tmp):
        kb = self.kb
        kb.op('act', lambda e: e.activation(out=sq[:, :, :], in_=y[:, :, :], func=AF.Square), r=(y,), w=(sq,))
        ps = self.next_ps()
        kb.mm(ps, ps[:, :], [(self.cm[:, 128:256], sq[:, kc, :]) for kc in range(KC)], r=(sq, self.cm))
        kb.op('act', lambda e: e.activation(out=rstd[:, :], in_=ps[:, :], func=AF.Sqrt, bias=self.epsc[:, 0:1], scale=1.0 / D),
              r=(ps,), w=(rstd,))
        kb.op('dve', lambda e: e.reciprocal(rstd[:, :], rstd[:, :]), r=(rstd,), w=(rstd,))
        for kc in range(KC):
            kb.op('dve', lambda e, kc=kc: e.scalar_tensor_tensor(
                tmp[:, kc, :], y[:, kc, :], self.cols[:, gcol0 + kc:gcol0 + kc + 1], rstd[:, :],
                ALU.mult, ALU.mult), r=(y, rstd), w=(tmp,))
        kb.op('pool', lambda e: e.tensor_tensor(xt[:, :, :], xt[:, :, :], tmp[:, :, :], ALU.add), r=(tmp, xt), w=(xt,))

    def stage_merge(self, l):
        kb, NT = self.kb, self.NT
        wbr = kb.sb("mg_wbr", [128, 8, D], BF16)
        wo = kb.sb("mg_wo", [128, KC, D], BF16)
        kb.dma('sp', wbr[:, :, :], self.wb_br[l].rearrange("(c p) d -> p c d", p=128), w=(wbr,))
        kb.dma('sp', wo[:, :, :], self.wb_out[l].rearrange("(c p) d -> p c d", p=128), w=(wo,))
        yt = [kb.sb(f"mg_y{i}", [128, 8, 512], BF16) for i in range(2)]
        gt = [kb.sb(f"mg_g{i}", [128, 32, 512], BF16) for i in range(2)]
        xt = [kb.sb(f"mg_x{i}", [128, KC, 512], F32) for i in range(2)]
        acc = [kb.sb(f"mg_acc{i}", [128, 512], F32) for i in range(2)]
        tm = [kb.sb(f"mg_tm{i}", [128, 512], F32) for i in range(2)]
        mT = kb.sb("mg_m", [128, KC, 512], BF16)
        yo = kb.sb("mg_yo", [128, KC, 512], F32)
        sq = kb.sb("mg_sq", [128, KC, 512], F32)
        rstd = kb.sb("mg_rstd", [128, 512], F32)
        Yv = self.Y.rearrange("(c p) t -> p c t", p=128)
        Gv = self.P_g.rearrange("(c p) t -> p c t", p=128)
        xTv = self.xT.rearrange("(kc p) t -> p kc t", p=128)
        g0 = self.cidx[('nmo', l)]
        for n in range(NT // 512):
            ts_ = slice(n * 512, (n + 1) * 512)
            y, g, x = yt[n % 2], gt[n % 2], xt[n % 2]
            kb.dma('sp', y[:, :, :], Yv[:, :, ts_], w=(y,))
            for gq_ in range(4):
                kb.dma('sp', g[:, gq_ * 8:(gq_ + 1) * 8, :], Gv[:, gq_ * 8:(gq_ + 1) * 8, ts_], w=(g,))
            kb.dma('sp', x[:, :, :], xTv[:, :, ts_], w=(x,))
            for dc in range(KC):
                a = acc[dc % 2]
                for br in range(4):
                    ps = self.next_ps()
                    kb.mm(ps, ps[:, :], [(wbr[:, br * 2 + c, dc * 128:(dc + 1) * 128], y[:, br * 2 + c, :]) for c in range(2)],
                          r=(wbr, y))
                    if br == 0:
                        kb.op('dve', lambda e, a=a, ps=ps, g=g, br=br, dc=dc: e.tensor_tensor(
                            a[:, :], ps[:, :], g[:, br * 8 + dc, :], ALU.mult), r=(ps, g), w=(a,))
                    else:
                        t = tm[br % 2]
                        kb.op('dve', lambda e, t=t, ps=ps, g=g, br=br, dc=dc: e.tensor_tensor(
                            t[:, :], ps[:, :], g[:, br * 8 + dc, :], ALU.mult), r=(ps, g), w=(t,))
                        if br < 3:
                            kb.op('pool', lambda e, a=a, t=t: e.tensor_tensor(a[:, :], a[:, :], t[:, :], ALU.add), r=(t, a), w=(a,))
                        else:
                            kb.op('pool', lambda e, a=a, t=t, dc=dc: e.tensor_tensor(mT[:, dc, :], a[:, :], t[:, :], ALU.add),
                                  r=(t, a), w=(mT,))
            for oc in range(KC):
                ps = self.next_ps()
                kb.mm(ps, ps[:, :], [(wo[:, kc, oc * 128:(oc + 1) * 128], mT[:, kc, :]) for kc in range(KC)], r=(wo, mT))
                kb.op('act', lambda e, ps=ps, oc=oc: e.copy(yo[:, oc, :], ps[:, :]), r=(ps,), w=(yo,))
            self._post_norm_add(x, yo, g0, sq, rstd, sq)
            kb.dma('pool', xTv[:, :, ts_], x[:, :, :], r=(x,))

    def stage_ffn(self, l):
        kb, NT = self.kb, self.NT
        xt = [kb.sb(f"ff_x{i}", [128, KC, 512], F32) for i in range(2)]
        hT = kb.sb("ff_h", [128, KC, 512], BF16)
        aT = kb.sb("ff_a", [128, FC, 512], BF16)
        sq = kb.sb("ff_sq", [128, KC, 512], F32)
        yo = kb.sb("ff_yo", [128, KC, 512], F32)
        rstd = kb.sb("ff_rstd", [128, 512], F32)
        sg = [kb.sb(f"ff_sg{i}", [128, 512], F32) for i in range(2)]
        NW = 3
        wg = [kb.sb(f"ff_wg{i}", [128, KC, 256], BF16) for i in range(NW)]
        wu = [kb.sb(f"ff_wu{i}", [128, KC, 256], BF16) for i in range(NW)]
        wd = [kb.sb(f"ff_wd{i}", [128, FC, 256], BF16) for i in range(2)]
        xTv = self.xT.rearrange("(kc p) t -> p kc t", p=128)
        wgv = self.wb_g[l].rearrange("(kc p) n -> p kc n", p=128)
        wuv = self.wb_u[l].rearrange("(kc p) n -> p kc n", p=128)
        wdv = self.wb_d[l].rearrange("(kc p) n -> p kc n", p=128)
        gpre, gpost = self.cidx[('nfp', l)], self.cidx[('nfo', l)]
        wi = 0
        di = 0
        for n in range(NT // 512):
            ts_ = slice(n * 512, (n + 1) * 512)
            x = xt[n % 2]
            kb.dma('sp', x[:, :, :], xTv[:, :, ts_], w=(x,))
            self._norm_into(x, hT, 0, hT, gpre, sq, rstd)
            for f2 in range(FC // 2):
                g_, u_ = wg[wi % NW], wu[wi % NW]
                wi += 1
                kb.dma('sp', g_[:, :, :], wgv[:, :, f2 * 256:(f2 + 1) * 256], w=(g_,))
                kb.dma('sp', u_[:, :, :], wuv[:, :, f2 * 256:(f2 + 1) * 256], w=(u_,))
                for j in range(2):
                    fc = f2 * 2 + j
                    pg = self.next_ps()
                    kb.mm(pg, pg[:, :], [(g_[:, kc, j * 128:(j + 1) * 128], hT[:, kc, :]) for kc in range(KC)], r=(g_, hT))
                    pu = self.next_ps()
                    kb.mm(pu, pu[:, :], [(u_[:, kc, j * 128:(j + 1) * 128], hT[:, kc, :]) for kc in range(KC)], r=(u_, hT))
                    sgt = sg[fc % 2]
                    kb.op('act', lambda e, sgt=sgt, pg=pg: e.activation(out=sgt[:, :], in_=pg[:, :], func=AF.Silu), r=(pg,), w=(sgt,))
                    kb.op('dve', lambda e, sgt=sgt, pu=pu, fc=fc: e.tensor_tensor(aT[:, fc, :], sgt[:, :], pu[:, :], ALU.mult),
                          r=(sgt, pu), w=(aT,))
            for o2 in range(KC // 2):
                d_ = wd[di % 2]
                di += 1
                for f0, f1 in ((0, 8), (8, 15), (15, 22)):
                    kb.dma('sp', d_[:, f0:f1, :], wdv[:, f0:f1, o2 * 256:(o2 + 1) * 256], w=(d_,))
                for j in range(2):
                    oc = o2 * 2 + j
                    ps = self.next_ps()
                    kb.mm(ps, ps[:, :], [(d_[:, fc, j * 128:(j + 1) * 128], aT[:, fc, :]) for fc in range(FC)], r=(d_, aT))
                    kb.op('act', lambda e, ps=ps, oc=oc: e.copy(yo[:, oc, :], ps[:, :]), r=(ps,), w=(yo,))
            self._post_norm_add(x, yo, gpost, sq, rstd, sq)
            kb.dma('pool', xTv[:, :, ts_], x[:, :, :], r=(x,))


DIL_PAT = ((128, 1), (512, 4), (2048, 16))


def _bf16_round(v):
    import ml_dtypes
    return np.asarray(v, np.float32).astype(ml_dtypes.bfloat16).astype(np.float32)


def pos_tables():
    pos = np.arange(S)
    hi_, lo_ = (pos // 128 * 128).astype(np.float32), (pos % 128).astype(np.float32)
    one = np.ones(S, np.float32)
    dsl = 2.0 ** (-8.0 * np.arange(1, 5) / 4)
    dxq = np.zeros((16, S), np.float32)
    dxk = np.zeros((16, S), np.float32)
    for h in range(4):
        sl = np.float32(dsl[h])
        dxk[h * 4:(h + 1) * 4] = np.stack([hi_, lo_, -sl * one, -sl * one])
        dxq[h * 4:(h + 1) * 4] = np.stack([sl * one, sl * one, hi_, lo_])
    asl = 2.0 ** (-8.0 * np.arange(1, 13) / 12)
    dlq = np.zeros((96, S), np.float32)
    dlk = np.zeros((96, S), np.float32)
    for g, (win, d) in enumerate(DIL_PAT):
        Tt = pos // d
        th, tl = (Tt // 128 * 128).astype(np.float32), (Tt % 128).astype(np.float32)
        for h in range(4):
            gh = g * 4 + h
            sd = np.float32(asl[gh]) * np.float32(d)
            shi = _bf16_round(sd)
            slo = _bf16_round(np.float32(sd) - shi)
            dlk[gh * 8:(gh + 1) * 8] = np.stack([th, th, tl, tl, -shi * one, -slo * one, -shi * one, -slo * one])
            dlq[gh * 8:(gh + 1) * 8] = np.stack([shi * one, slo * one, shi * one, slo * one, th, th, tl, tl])
    return dxq, dxk, dlq, dlk


def make_inmaps(inp, L, NS, ncores, l0=0, x=None):
    inp = dict(inp)
    for k_ in list(inp.keys()):
        if k_ != 'x':
            inp[k_] = np.asarray(inp[k_])[l0:l0 + L]
    cols = build_cols(inp, L, l0)
    ii = np.arange(128)
    U = (ii[:, None] <= ii[None, :]).astype(np.float32)
    Lm = (ii[:, None] >= ii[None, :]).astype(np.float32)
    sel = np.zeros((128, 128), np.float32)
    sel[64, :] = 1.0
    blk = np.zeros((128, 128), np.float32)
    blk[0:64, 0:64] = 1.0
    blk[64:128, 64:128] = 1.0
    cm = np.concatenate([np.eye(128, dtype=np.float32), np.ones((128, 128), np.float32), U, Lm, sel, blk], axis=1)
    if x is None:
        x = inp['x']
    x = np.ascontiguousarray(np.asarray(x, np.float32)).reshape(-1, D)
    dxq, dxk, dlq, dlk = pos_tables()
    lamv = np.stack([np.broadcast_to(np.concatenate([inp['diff_lam_q1'][l], inp['diff_lam_k1'][l],
                                                      inp['diff_lam_q2'][l], inp['diff_lam_k2'][l]])[None, :], (128, 128))
                     for l in range(L)]).astype(np.float32)
    lamv = np.ascontiguousarray(lamv)
    maps = []
    for c in range(ncores):
        m = {
            "x": np.ascontiguousarray(x[c * NS * S:(c + 1) * NS * S]),
            "w_in": np.ascontiguousarray(inp['w_in'][:L]),
            "w_branch": np.ascontiguousarray(inp['w_branch'][:L]),
            "w_out": np.ascontiguousarray(inp['w_out'][:L]),
            "ffn_w_gate": np.ascontiguousarray(inp['ffn_w_gate'][:L]),
            "ffn_w_up": np.ascontiguousarray(inp['ffn_w_up'][:L]),
            "ffn_w_down": np.ascontiguousarray(inp['ffn_w_down'][:L]),
            "cols": cols,
            "cmats": cm,
            "dxq": dxq, "dxk": dxk, "dlq": dlq, "dlk": dlk, "lamv": lamv,
            "wa_up": np.ascontiguousarray(np.concatenate([inp['rwkv_w_up'][:L], inp['rwkv_a_up'][:L]], axis=1).astype(np.float32)),
            "g_up": np.ascontiguousarray(inp['rwkv_g_up'][:L].astype(np.float32)),
        }
        maps.append(m)
    return maps


def kernel(**inputs):
    inp = {k: np.asarray(v) for k, v in inputs.items()}
    prog = Prog(L=DEPTH, NS=2)
    nc = prog.build()
    maps = make_inmaps(inp, DEPTH, 2, 8)
    res = run_bass_kernel_spmd(nc, maps, core_ids=list(range(8)))
    x = np.concatenate([r["out"] for r in res.results], axis=0)
    return x.reshape(16, S, D).astype(np.float32)
```
